# Optimizing a Trainium2 kernel written in Bass

```python
import jax, jax.numpy as jnp
from jax import lax
import numpy as np

D_MODEL = 1024
BATCH = 4
SEQ = 8192
DEPTH = 1

GRID_W = 64
MEM_LEN = 256
MEM_HEADS = 4
MEM_HEAD_DIM = 128
MEM_DIM = MEM_HEADS * MEM_HEAD_DIM
N_FOURIER_GROUPS = 4
FOURIER_GROUP_DIM = 128
FOURIER_DIM = N_FOURIER_GROUPS * FOURIER_GROUP_DIM
NA_HEADS = 8
NA_HEAD_DIM = 64
NA_DIM = NA_HEADS * NA_HEAD_DIM
NA_ROW_WIN = 8
NA_COL_WIN = 16
N_BRANCHES = 3
IN_PROJ_DIM = FOURIER_DIM + 3 * NA_DIM + MEM_DIM
N_GROUPS = 8
EXPERTS_PER_GROUP = 8
N_EXPERTS = N_GROUPS * EXPERTS_PER_GROUP
TOP_K = 2
D_EXPERT = 512
MOE_BLOCK = 128
ALPHA = (2.0 * DEPTH) ** 0.25
BETA = (8.0 * DEPTH) ** -0.25
LN_EPS = 1e-5

kernel_name = 'hybrid_fnet_natten_memory_hmoe_encoder'


def layer_norm(x, g, b):
    xf = x.astype(jnp.float32)
    mu = xf.mean(-1, keepdims=True)
    var = jnp.square(xf - mu).mean(-1, keepdims=True)
    y = (xf - mu) * lax.rsqrt(var + LN_EPS) * g.astype(jnp.float32) + b.astype(jnp.float32)
    return y.astype(x.dtype)


def fourier_mix(u):
    b, s, _ = u.shape
    ug = u.astype(jnp.float32).reshape(b, s, N_FOURIER_GROUPS, FOURIER_GROUP_DIM)
    y = jnp.fft.fft2(ug, axes=(1, 3), norm='ortho').real
    return y.reshape(b, s, FOURIER_DIM).astype(u.dtype)


def neighbourhood_attention(q, k, v, rpb):
    b, s = q.shape[0], q.shape[1]
    rows = s // GRID_W
    kr = min(NA_ROW_WIN, rows)

    def to_grid(t):
        return t.reshape(b, rows, GRID_W, NA_HEADS, NA_HEAD_DIM).transpose(0, 3, 1, 2, 4)

    qg, kg, vg = to_grid(q), to_grid(k), to_grid(v)
    cols = np.arange(GRID_W)
    col_start = np.clip(cols - NA_COL_WIN // 2, 0, GRID_W - NA_COL_WIN)
    col_idx = col_start[:, None] + np.arange(NA_COL_WIN)[None, :]
    dc_idx = col_idx - cols[:, None] + (NA_COL_WIN - 1)
    bias_c = rpb[:, :, dc_idx]
    scale = NA_HEAD_DIM ** -0.5

    def one_row(r):
        rs = jnp.clip(r - kr // 2, 0, rows - kr)
        q_r = lax.dynamic_index_in_dim(qg, r, axis=2, keepdims=False)
        k_rows = lax.dynamic_slice_in_dim(kg, rs, kr, axis=2)
        v_rows = lax.dynamic_slice_in_dim(vg, rs, kr, axis=2)
        k_win = k_rows[:, :, :, col_idx]
        v_win = v_rows[:, :, :, col_idx]
        dr_idx = rs + jnp.arange(kr) - r + (NA_ROW_WIN - 1)
        bias = bias_c[:, dr_idx].transpose(0, 2, 1, 3)
        sc = jnp.einsum('bhcd,bhrckd->bhcrk', q_r, k_win).astype(jnp.float32) * scale
        sc = sc + bias[None].astype(jnp.float32)
        p = jax.nn.softmax(sc.reshape(b, NA_HEADS, GRID_W, kr * NA_COL_WIN), axis=-1)
        p = p.reshape(b, NA_HEADS, GRID_W, kr, NA_COL_WIN).astype(v.dtype)
        return jnp.einsum('bhcrk,bhrckd->bhcd', p, v_win)

    out = lax.map(one_row, jnp.arange(rows))
    return out.transpose(1, 0, 3, 2, 4).reshape(b, s, NA_DIM)


def memory_attention(q, k, v):
    b, s = q.shape[0], q.shape[1]
    sc = jnp.einsum('bshd,bmhd->bhsm', q, k).astype(jnp.float32) * (MEM_HEAD_DIM ** -0.5)
    p = jax.nn.softmax(sc, axis=-1).astype(v.dtype)
    return jnp.einsum('bhsm,bmhd->bshd', p, v).reshape(b, s, MEM_DIM)


def token_mixing(x, mem, w_in, w_gate, b_gate, w_mem_kv, rpb, w_fourier_o, w_na_o, w_mem_o, w_out):
    b, s, d = x.shape
    proj = x @ w_in
    o1 = FOURIER_DIM
    o2 = o1 + NA_DIM
    o3 = o2 + NA_DIM
    o4 = o3 + NA_DIM
    u_f = proj[..., :o1]
    q_na = proj[..., o1:o2].reshape(b, s, NA_HEADS, NA_HEAD_DIM)
    k_na = proj[..., o2:o3].reshape(b, s, NA_HEADS, NA_HEAD_DIM)
    v_na = proj[..., o3:o4].reshape(b, s, NA_HEADS, NA_HEAD_DIM)
    q_mem = proj[..., o4:].reshape(b, s, MEM_HEADS, MEM_HEAD_DIM)
    kv = mem @ w_mem_kv
    m = mem.shape[1]
    k_mem = kv[..., :MEM_DIM].reshape(b, m, MEM_HEADS, MEM_HEAD_DIM)
    v_mem = kv[..., MEM_DIM:].reshape(b, m, MEM_HEADS, MEM_HEAD_DIM)
    br_f = fourier_mix(u_f) @ w_fourier_o
    br_na = neighbourhood_attention(q_na, k_na, v_na, rpb) @ w_na_o
    br_mem = memory_attention(q_mem, k_mem, v_mem) @ w_mem_o
    gates = jax.nn.sigmoid(x @ w_gate + b_gate).reshape(b, s, N_BRANCHES, d)
    merged = gates[:, :, 0] * br_f + gates[:, :, 1] * br_na + gates[:, :, 2] * br_mem
    return merged @ w_out


def hierarchical_moe(h, w_rg, b_rg, w_re, b_re, w_eg, w_eu, w_ed):
    b, s, d = h.shape
    t = b * s
    tok = h.reshape(t, d)
    tf = tok.astype(jnp.float32)
    group_logits = tf @ w_rg.astype(jnp.float32) + b_rg.astype(jnp.float32)
    group_prob = jax.nn.softmax(group_logits, axis=-1)
    g_idx = jnp.argmax(group_logits, axis=-1).astype(jnp.int32)
    p_group = jnp.take_along_axis(group_prob, g_idx[:, None], axis=1)
    exp_logits = (tf @ w_re.astype(jnp.float32) + b_re.astype(jnp.float32)).reshape(t, N_GROUPS, EXPERTS_PER_GROUP)
    exp_logits = jnp.take_along_axis(exp_logits, g_idx[:, None, None], axis=1)[:, 0]
    top_val, top_idx = lax.top_k(exp_logits, TOP_K)
    weights = (jax.nn.softmax(top_val, axis=-1) * p_group).reshape(-1)
    expert = (g_idx[:, None] * EXPERTS_PER_GROUP + top_idx).reshape(-1).astype(jnp.int32)
    token = jnp.repeat(jnp.arange(t, dtype=jnp.int32), TOP_K)
    n_assign = t * TOP_K
    order = jnp.argsort(expert)
    e_sorted = expert[order]
    counts = jnp.bincount(expert, length=N_EXPERTS)
    start = jnp.cumsum(counts) - counts
    padded = (counts + MOE_BLOCK - 1) // MOE_BLOCK * MOE_BLOCK
    pad_end = jnp.cumsum(padded)
    pad_start = pad_end - padded
    dest = pad_start[e_sorted] + jnp.arange(n_assign, dtype=jnp.int32) - start[e_sorted]
    n_pad = n_assign + N_EXPERTS * MOE_BLOCK
    n_blocks = n_pad // MOE_BLOCK
    pad_tok = jnp.full((n_pad,), t, jnp.int32).at[dest].set(token[order])
    pad_w = jnp.zeros((n_pad,), jnp.float32).at[dest].set(weights[order])
    block_start = jnp.arange(n_blocks, dtype=jnp.int32) * MOE_BLOCK
    block_expert = jnp.clip(jnp.searchsorted(pad_end, block_start, side='right'), 0, N_EXPERTS - 1)
    tok_pad = jnp.concatenate([tok, jnp.zeros((1, d), tok.dtype)], axis=0)
    xg = tok_pad[pad_tok].reshape(n_blocks, MOE_BLOCK, d)

    def expert_block(args):
        xb, e = args
        return (jax.nn.silu(xb @ w_eg[e]) * (xb @ w_eu[e])) @ w_ed[e]

    yb = lax.map(expert_block, (xg, block_expert)).reshape(n_pad, d)
    y = jax.ops.segment_sum(yb * pad_w[:, None].astype(yb.dtype), pad_tok, num_segments=t + 1)[:t]
    return y.reshape(b, s, d)


def setup_inputs(seed: int = 0) -> dict:
    key = jax.random.key(seed)
    ks = jax.random.split(key, 32)
    L = DEPTH
    sd = D_MODEL ** -0.5

    def nrm(k, shape, scale):
        return jax.random.normal(k, shape, jnp.float32) * scale

    x = nrm(ks[0], (BATCH, SEQ, D_MODEL), 1.0)
    mem = nrm(ks[1], (BATCH, MEM_LEN, D_MODEL), 1.0)
    w_in = jnp.concatenate([
        nrm(ks[2], (L, D_MODEL, FOURIER_DIM + 2 * NA_DIM), sd),
        nrm(ks[3], (L, D_MODEL, NA_DIM), sd * BETA),
        nrm(ks[4], (L, D_MODEL, MEM_DIM), sd)], axis=-1)
    w_gate = nrm(ks[5], (L, D_MODEL, N_BRANCHES * D_MODEL), sd)
    b_gate = nrm(ks[6], (L, N_BRANCHES * D_MODEL), 0.02)
    w_mem_kv = jnp.concatenate([
        nrm(ks[7], (L, D_MODEL, MEM_DIM), sd),
        nrm(ks[8], (L, D_MODEL, MEM_DIM), sd * BETA)], axis=-1)
    rpb = nrm(ks[9], (L, NA_HEADS, 2 * NA_ROW_WIN - 1, 2 * NA_COL_WIN - 1), 0.02)
    w_fourier_o = nrm(ks[10], (L, FOURIER_DIM, D_MODEL), FOURIER_DIM ** -0.5 * BETA)
    w_na_o = nrm(ks[11], (L, NA_DIM, D_MODEL), NA_DIM ** -0.5 * BETA)
    w_mem_o = nrm(ks[12], (L, MEM_DIM, D_MODEL), MEM_DIM ** -0.5 * BETA)
    w_out = nrm(ks[13], (L, D_MODEL, D_MODEL), sd * BETA)
    ln1_g = 1.0 + nrm(ks[14], (L, D_MODEL), 0.02)
    ln1_b = nrm(ks[15], (L, D_MODEL), 0.02)
    w_router_group = nrm(ks[16], (L, D_MODEL, N_GROUPS), sd)
    b_router_group = nrm(ks[17], (L, N_GROUPS), 0.01)
    w_router_expert = nrm(ks[18], (L, D_MODEL, N_EXPERTS), sd)
    b_router_expert = nrm(ks[19], (L, N_EXPERTS), 0.01)
    w_exp_gate = nrm(ks[20], (L, N_EXPERTS, D_MODEL, D_EXPERT), sd)
    w_exp_up = nrm(ks[21], (L, N_EXPERTS, D_MODEL, D_EXPERT), sd * BETA)
    w_exp_down = nrm(ks[22], (L, N_EXPERTS, D_EXPERT, D_MODEL), D_EXPERT ** -0.5 * BETA)
    ln2_g = 1.0 + nrm(ks[23], (L, D_MODEL), 0.02)
    ln2_b = nrm(ks[24], (L, D_MODEL), 0.02)
    return {'x': x, 'mem': mem, 'w_in': w_in, 'w_gate': w_gate, 'b_gate': b_gate,
            'w_mem_kv': w_mem_kv, 'rpb': rpb, 'w_fourier_o': w_fourier_o, 'w_na_o': w_na_o,
            'w_mem_o': w_mem_o, 'w_out': w_out, 'ln1_g': ln1_g, 'ln1_b': ln1_b,
            'w_router_group': w_router_group, 'b_router_group': b_router_group,
            'w_router_expert': w_router_expert, 'b_router_expert': b_router_expert,
            'w_exp_gate': w_exp_gate, 'w_exp_up': w_exp_up, 'w_exp_down': w_exp_down,
            'ln2_g': ln2_g, 'ln2_b': ln2_b}


def reference(x, mem, w_in, w_gate, b_gate, w_mem_kv, rpb, w_fourier_o, w_na_o, w_mem_o,
              w_out, ln1_g, ln1_b, w_router_group, b_router_group, w_router_expert,
              b_router_expert, w_exp_gate, w_exp_up, w_exp_down, ln2_g, ln2_b):
    for l in range(DEPTH):
        mix = token_mixing(x, mem, w_in[l], w_gate[l], b_gate[l], w_mem_kv[l], rpb[l],
                           w_fourier_o[l], w_na_o[l], w_mem_o[l], w_out[l])
        h = layer_norm(ALPHA * x + mix, ln1_g[l], ln1_b[l])
        ffn = hierarchical_moe(h, w_router_group[l], b_router_group[l], w_router_expert[l],
                               b_router_expert[l], w_exp_gate[l], w_exp_up[l], w_exp_down[l])
        x = layer_norm(ALPHA * h + ffn, ln2_g[l], ln2_b[l])
    return x
```

```python
import contextlib
import math
import numpy as np
import ml_dtypes
import concourse.bass as bass
import concourse.mybir as mybir
from concourse.bass_utils import run_bass_kernel_spmd

F32 = mybir.dt.float32
BF16 = mybir.dt.bfloat16
I32 = mybir.dt.int32
U8 = mybir.dt.uint8
ALU = mybir.AluOpType
AF = mybir.ActivationFunctionType
AX = mybir.AxisListType

ENGS = ("tensor", "vector", "scalar", "gpsimd", "sync")
NCORES = 8
D = 1024
CAP = 256
NEXP = 64
NSLOT = NEXP * CAP
ALPHA = 2.0 ** 0.25
LN_EPS = 1e-5
MASKV = -30000.0
ARENA = 206 * 1024


class Op:
    __slots__ = ("eng", "fn", "dma", "deps", "sig", "sigval", "key", "dval", "slot")

    def __init__(self, eng, fn, dma):
        self.eng = eng
        self.fn = fn
        self.dma = dma
        self.deps = []
        self.sig = False
        self.sigval = 0
        self.key = None
        self.dval = 0
        self.slot = None


class Prog:
    def __init__(self, nc):
        self.nc = nc
        self.ops = {e: [] for e in ENGS}
        self.writers = {}
        self.readers = {}
        self.key_slot = {}
        self.slot_count = []
        self.nops = 0

    def op(self, eng, fn, reads=(), writes=(), dma=False, key=None, partial=False):
        o = Op(eng, fn, dma)
        self.nops += 1
        deps = {}
        banks = set()
        for b in list(reads) + list(writes):
            if isinstance(b, tuple) and b and b[0] == "ps":
                banks.add(b[1])
        for bk in banks:
            nm = ("psbank", bk)
            for w in self.writers.get(nm, ()):
                deps[id(w)] = w
            self.writers[nm] = [o]
        for b in reads:
            for w in self.writers.get(b, ()):
                deps[id(w)] = w
        for b in writes:
            if not partial:
                for w in self.writers.get(b, ()):
                    deps[id(w)] = w
            for r in self.readers.get(b, ()):
                deps[id(r)] = r
        for b in reads:
            self.readers.setdefault(b, []).append(o)
        for b in writes:
            if partial:
                self.writers.setdefault(b, []).append(o)
            else:
                self.writers[b] = [o]
                self.readers[b] = []
        deps.pop(id(o), None)
        o.deps = list(deps.values())
        if dma:
            if key is None:
                key = writes[0] if writes else reads[0]
            if key not in self.key_slot:
                self.key_slot[key] = len(self.key_slot)
                if len(self.key_slot) > len(self.slot_count):
                    self.slot_count.append(0)
            s = self.key_slot[key]
            self.slot_count[s] += 1
            o.slot = s
            o.dval = 16 * self.slot_count[s]
        self.ops[eng].append(o)
        return o

    def barrier(self):
        lasts = []
        for e in ENGS:
            nd = [o for o in self.ops[e] if not o.dma and o.fn is not None]
            if nd:
                lasts.append(nd[-1])
        dmas = {}
        for e in ENGS:
            for o in self.ops[e]:
                if o.dma and (o.slot not in dmas or dmas[o.slot].dval < o.dval):
                    dmas[o.slot] = o
        for e in ENGS:
            o = Op(e, None, False)
            o.deps = [l for l in lasts if l.eng != e] + list(dmas.values())
            self.ops[e].append(o)
        self.writers = {}
        self.readers = {}
        self.key_slot = {}

    def emit(self):
        nc = self.nc
        for e in ENGS:
            for o in self.ops[e]:
                for d in o.deps:
                    if not d.dma and not (d.eng == "tensor" and o.eng == "tensor"):
                        d.sig = True
        for e in ENGS:
            c = 0
            for o in self.ops[e]:
                if o.sig:
                    c += 1
                    o.sigval = c
        nslots = len(self.slot_count)
        with contextlib.ExitStack() as st:
            esem = {e: st.enter_context(nc.semaphore("e_" + e)) for e in ENGS}
            ksem = [st.enter_context(nc.semaphore("d%d" % i)) for i in range(nslots)]
            block = st.enter_context(nc.Block())

            def body(e):
                def _f(eng):
                    waited = {}
                    for o in self.ops[e]:
                        for d in o.deps:
                            if d.dma:
                                s, v = ksem[d.slot], d.dval
                            elif d.sig:
                                s, v = esem[d.eng], d.sigval
                            else:
                                continue
                            if waited.get(id(s), 0) >= v:
                                continue
                            waited[id(s)] = v
                            eng.wait_ge(s, v)
                        if o.fn is None:
                            continue
                        ins = o.fn(eng)
                        if o.dma:
                            ins.then_inc(ksem[o.slot], 16)
                        elif o.sig:
                            ins.then_inc(esem[e], 1)
                    if e == "sync":
                        for i in range(nslots):
                            v = 16 * self.slot_count[i]
                            if waited.get(id(ksem[i]), 0) < v:
                                eng.wait_ge(ksem[i], v)
                return _f

            block.sync(body("sync"))
            block.tensor(body("tensor"))
            block.vector(body("vector"))
            block.scalar(body("scalar"))
            block.gpsimd(body("gpsimd"))


def _isz(dt):
    return {F32: 4, BF16: 2, I32: 4, U8: 1}[dt]


class Arena:
    def __init__(self, ap, size):
        self.ap = ap
        self.size = size
        self.off = 0

    def alloc(self, shape, dt):
        n = int(np.prod(shape))
        nb = n * _isz(dt)
        off = self.off
        self.off += (nb + 63) // 64 * 64
        assert self.off <= self.size, ("arena overflow", self.off, self.size)
        v = self.ap[:, off:off + nb].bitcast(dt)
        if len(shape) == 2:
            v = v.rearrange("p (a b) -> p a b", a=shape[0], b=shape[1])
        elif len(shape) == 3:
            v = v.rearrange("p (a b c) -> p a b c", a=shape[0], b=shape[1], c=shape[2])
        elif len(shape) == 4:
            v = v.rearrange("p (a b c d) -> p a b c d", a=shape[0], b=shape[1], c=shape[2], d=shape[3])
        return v

    def mark(self):
        return self.off

    def reset(self, m):
        self.off = m


class K:
    def __init__(self, nc, P):
        self.nc = nc
        self.P = P
        self.ev = 0

    def mm(self, out, lhsT, rhs, start, stop, reads, writes):
        self.P.op("tensor", lambda e: e.matmul(out, lhsT, rhs, start=start, stop=stop),
                  reads=reads, writes=writes, partial=not start)

    def mmi(self, groups):
        n = max(len(g) for g in groups)
        for i in range(n):
            for g in groups:
                if i < len(g):
                    out, lhsT, rhs, reads, writes = g[i]
                    self.mm(out, lhsT, rhs, i == 0, i == len(g) - 1, reads, writes)

    def tri(self, groups):
        n = max(len(g) for g in groups)
        for i in range(n):
            for g in groups:
                if i < len(g):
                    out, in_, ident, reads, writes = g[i]
                    self.tr(out, in_, ident, reads, writes, partial=(i > 0))

    def tr(self, out, in_, ident, reads, writes, partial=True):
        self.P.op("tensor", lambda e: e.transpose(out, in_, ident), reads=reads, writes=writes,
                  partial=partial)

    def act(self, out, in_, func, reads, writes, bias=None, scale=None, partial=False):
        def f(e):
            kw = {}
            if bias is not None:
                kw["bias"] = bias
            if scale is not None:
                kw["scale"] = scale
            return e.activation(out, in_, func, **kw)
        self.P.op("scalar", f, reads=reads, writes=writes, partial=partial)

    def evac(self, out, in_, reads, writes, eng=None, partial=False):
        if eng is None:
            self.ev += 1
            eng = "vector" if self.ev % 2 else "scalar"
        if eng == "vector":
            self.P.op("vector", lambda e: e.tensor_copy(out, in_), reads=reads, writes=writes, partial=partial)
        else:
            self.P.op("scalar", lambda e: e.activation(out, in_, AF.Copy), reads=reads, writes=writes,
                      partial=partial)

    def v(self, eng, name, args, reads, writes, kw=None, partial=False):
        kw = kw or {}
        self.P.op(eng, lambda e: getattr(e, name)(*args, **kw), reads=reads, writes=writes, partial=partial)

    def dma(self, q, out, in_, reads, writes, key=None, partial=False):
        self.P.op(q, lambda e: e.dma_start(out=out, in_=in_), reads=reads, writes=writes, dma=True,
                  key=key, partial=partial)


def build_program(debug=False):
    nc = bass.Bass("TRN2", target_bir_lowering=False)
    _bc = {}

    def _bc_reg(e):
        if "r" not in _bc:
            _bc["r"] = e.to_reg(NSLOT - 1)
        return _bc["r"]
    dr = {}

    def din(name, shape, dt=F32):
        dr[name] = nc.dram_tensor(name, list(shape), dt, kind="ExternalInput").ap()

    def dscr(name, shape, dt):
        dr[name] = nc.dram_tensor(name, list(shape), dt, kind="Internal").ap()

    din("xf", [8192, D]); din("xo", [4608, D]); din("mem", [256, D])
    din("w_in", [D, 2560]); din("w_gate", [D, 3072]); din("bg", [128, 24])
    din("w_kv", [D, 1024]); din("w_fo", [512, D]); din("w_nao", [512, D]); din("w_memo", [512, D])
    din("w_out", [D, D]); din("ln1_g", [1, D]); din("ln1_b", [1, D]); din("ln2_g", [1, D]); din("ln2_b", [1, D])
    din("wr", [D, 72]); din("br", [1, 72])
    din("w_eg", [NEXP, D, 512]); din("w_eu", [NEXP, D, 512]); din("w_ed", [NEXP, 512, D])
    din("biasT", [5, 8, 128, 768])
    din("c_identf", [128, 128]); din("c_identb", [128, 128], BF16); din("c_ones", [128, 128], BF16)
    din("c_ustrict", [128, 128], BF16); din("c_edft", [64, 128, 256], BF16)
    din("c_fca", [128, 256], BF16); din("c_fcb", [128, 256], BF16)
    din("c_bdc", [128, 64], BF16); din("c_bds", [128, 64], BF16)
    din("c_ec", [128, 64]); din("c_mhalf", [128, 1]); din("c_onesrow", [1, 128])
    dr["out"] = nc.dram_tensor("out", [4096, D], F32, kind="ExternalOutput").ap()
    dscr("yfT_d", [128, 4, 4096], BF16); dscr("ynaT_d", [128, 4, 4096], BF16); dscr("ymemT_d", [128, 4, 4096], BF16)
    dscr("xT_d", [8, 128, 8, 512], BF16); dscr("qmT_d", [8, 128, 4, 512], BF16)
    dscr("h_d", [4096, D], F32); dscr("xg_d", [NSLOT, D], BF16); dscr("yb_d", [NSLOT, D], BF16)
    if debug:
        dr["dbg_h"] = nc.dram_tensor("dbg_h", [4096, D], F32, kind="ExternalOutput").ap()
        dr["dbg_y3"] = nc.dram_tensor("dbg_y3", [3, 128, 4, 4096], BF16, kind="ExternalOutput").ap()
        dr["dbg_rt"] = nc.dram_tensor("dbg_rt", [128, 32, 4], F32, kind="ExternalOutput").ap()

    P = Prog(nc)
    k = K(nc, P)
    with contextlib.ExitStack() as st:
        arena_t = st.enter_context(nc.sbuf_tensor("arena", [128, ARENA], U8))
        A = Arena(arena_t, ARENA)
        ps = [st.enter_context(nc.psum_tensor("ps%d" % i, [128, 512], F32)) for i in range(8)]
        psb = [p[:].bitcast(BF16) for p in ps]

        identf = A.alloc([128], F32); identb = A.alloc([128], BF16); ones_b = A.alloc([128], BF16)
        ustrict = A.alloc([128], BF16)
        slots_i = A.alloc([32, 2], I32); rw = A.alloc([32, 2], F32)
        k.dma("sync", identf, dr["c_identf"], [], ["identf"])
        k.dma("sync", identb, dr["c_identb"], [], ["identb"])
        k.dma("sync", ones_b, dr["c_ones"], [], ["ones_b"])
        k.dma("sync", ustrict, dr["c_ustrict"], [], ["ustrict"])
        base_mark = A.mark()

        wf = A.alloc([8, 512], BF16)
        fca = A.alloc([256], BF16); fcb = A.alloc([256], BF16); bdc = A.alloc([64], BF16); bds = A.alloc([64], BF16)
        AT = [A.alloc([2, 64, 64, 2], BF16) for _ in range(4)]
        xt = [A.alloc([1024], F32) for _ in range(4)]
        xT = [A.alloc([8, 128], BF16) for _ in range(2)]
        ub = [A.alloc([512], BF16) for _ in range(2)]
        Ej = [A.alloc([256], BF16) for _ in range(4)]
        Bt = [A.alloc([256], BF16) for _ in range(4)]
        yfs = [A.alloc([4096], BF16) for _ in range(2)]
        k.dma("gpsimd", wf, dr["w_in"][:, 0:512].rearrange("(c p) n -> p c n", p=128), [], ["wf"])
        k.dma("sync", fca, dr["c_fca"], [], ["fca"]); k.dma("sync", fcb, dr["c_fcb"], [], ["fcb"])
        k.dma("sync", bdc, dr["c_bdc"], [], ["bdc"]); k.dma("sync", bds, dr["c_bds"], [], ["bds"])
        xf_v = dr["xf"].rearrange("(r c) f -> c r f", c=64)

        def a_load(j):
            k.dma("sync", xt[j % 4], xf_v[j], [], [("xt", j % 4)])
            k.dma("sync", Ej[j % 4], dr["c_edft"][j], [], [("Ej", j % 4)])
        a_load(0); a_load(1)
        for j0 in range(0, 64, 2):
            for j in (j0, j0 + 1):
                if j + 2 < 64:
                    a_load(j + 2)
            tg = []
            for j in (j0, j0 + 1):
                for hb in range(2):
                    bank = 2 * (j % 2) + hb
                    tg.append([(ps[bank][:, q * 128:(q + 1) * 128], xt[j % 4][:, (hb * 4 + q) * 128:(hb * 4 + q + 1) * 128],
                                identf, [("xt", j % 4), "identf"], [("ps", bank)]) for q in range(4)])
            k.tri(tg)
            for j in (j0, j0 + 1):
                for hb in range(2):
                    bank = 2 * (j % 2) + hb
                    k.evac(xT[j % 2][:, hb * 4:(hb + 1) * 4, :], ps[bank][:].rearrange("p (a b) -> p a b", a=4),
                           [("ps", bank)], [("xT", j % 2, hb)])
            k.mmi([[(ps[4 + (j % 2)][:], xT[j % 2][:, fc, :], wf[:, fc, :],
                     [("xT", j % 2, 0), ("xT", j % 2, 1), "wf"], [("ps", 4 + (j % 2))]) for fc in range(8)]
                   for j in (j0, j0 + 1)])
            for j in (j0, j0 + 1):
                k.evac(ub[j % 2], ps[4 + (j % 2)][:], [("ps", 4 + (j % 2))], [("ub", j % 2)])
            for j in (j0, j0 + 1):
                for g in range(4):
                    pa = 6 + (g % 2)
                    k.mm(ps[pa][:, 0:256], ub[j % 2][:, g * 128:(g + 1) * 128], Ej[j % 4], True, True,
                         [("ub", j % 2), ("Ej", j % 4)], [("ps", pa)])
                    k.evac(AT[g][:, :, :, j, :], ps[pa][:, 0:256].rearrange("p (r k a) -> p r k a", r=2, k=64, a=2),
                           [("ps", pa)], [("AT", g)], partial=True)
        it = 0
        for g in range(4):
            ys = yfs[g % 2]
            ysv = ys.rearrange("p (k q) -> p k q", k=32)
            for kk0 in range(0, 64, 2):
                kks = (kk0, kk0 + 1)
                k.mmi([[(ps[kk % 2][:, 0:256], AT[g][:, 0, kk, :, :].rearrange("p s a -> p (s a)"), fca,
                         [("AT", g), "fca"], [("ps", kk % 2)]),
                        (ps[kk % 2][:, 0:256], AT[g][:, 1, kk, :, :].rearrange("p s a -> p (s a)"), fcb,
                         [("AT", g), "fcb"], [("ps", kk % 2)])] for kk in kks])
                bts = {}
                for kk in kks:
                    bts[kk] = it % 4
                    k.evac(Bt[it % 4], ps[kk % 2][:, 0:256], [("ps", kk % 2)], [("Bt", it % 4)])
                    it += 1
                k.mmi([[(ps[2 + kk % 2][:, 0:64], Bt[bts[kk]][:, 0:128], bdc, [("Bt", bts[kk]), "bdc"], [("ps", 2 + kk % 2)]),
                        (ps[2 + kk % 2][:, 0:64], Bt[bts[kk]][:, 128:256], bds, [("Bt", bts[kk]), "bds"], [("ps", 2 + kk % 2)])]
                       for kk in kks])
                for kk in kks:
                    k.evac(ysv[:, :, 2 * kk:2 * kk + 2].rearrange("p k a -> p a k"),
                           ps[2 + kk % 2][:, 0:64].rearrange("p (a k) -> p a k", a=2),
                           [("ps", 2 + kk % 2)], [("yfs", g % 2)], partial=True)
            k.dma("sync", dr["yfT_d"][:, g, :], ys, [("yfs", g % 2)], [("yfT_d", g)])
        P.barrier()
        A.reset(base_mark)

        kT = A.alloc([4, 4608], BF16)
        Vext = A.alloc([36, 8, 65], BF16)
        qT = A.alloc([4, 4096], BF16)
        b1_mark = A.mark()
        W = A.alloc([8, 2048], BF16)
        xt = [A.alloc([1024], F32) for _ in range(4)]
        xTq = [A.alloc([8, 512], BF16) for _ in range(2)]
        qmq = [A.alloc([4, 512], BF16) for _ in range(2)]
        for cb in range(4):
            k.dma("gpsimd", W[:, :, cb * 512:(cb + 1) * 512],
                  dr["w_in"][:, 512 + cb * 512:512 + (cb + 1) * 512].rearrange("(c p) n -> p c n", p=128),
                  [], [("W", cb)])
        Wr = [("W", cb) for cb in range(4)]
        k.v("vector", "memset", (Vext[:, :, :, 64:65], 1.0), [], ["Vext1"])
        xo_t = dr["xo"].rearrange("(t p) f -> t p f", p=128)

        def b_load(t):
            k.dma("sync", xt[t % 4], xo_t[t], [], [("xt", t % 4)])
        b_load(0); b_load(1)
        for Q in range(9):
            xq = xTq[Q % 2]
            for tt0 in range(0, 4, 2):
                tts = (tt0, tt0 + 1)
                for tt in tts:
                    t = 4 * Q + tt
                    if t + 2 < 36:
                        b_load(t + 2)
                tg = []
                for tt in tts:
                    t = 4 * Q + tt
                    for hb in range(2):
                        bank = 2 * (t % 2) + hb
                        tg.append([(ps[bank][:, q * 128:(q + 1) * 128],
                                    xt[t % 4][:, (hb * 4 + q) * 128:(hb * 4 + q + 1) * 128], identf,
                                    [("xt", t % 4), "identf"], [("ps", bank)]) for q in range(4)])
                k.tri(tg)
                for tt in tts:
                    t = 4 * Q + tt
                    for hb in range(2):
                        bank = 2 * (t % 2) + hb
                        k.evac(xq[:, hb * 4:(hb + 1) * 4, tt * 128:(tt + 1) * 128],
                               ps[bank][:].rearrange("p (a b) -> p a b", a=4),
                               [("ps", bank)], [("xTq", Q % 2)], partial=True)
            xr = [("xTq", Q % 2)] + Wr

            def proj_pair(col0, cs, banks):
                k.mmi([[(ps[banks[i]][:], W[:, fc, col0 + c * 128:col0 + (c + 1) * 128], xq[:, fc, :], xr,
                         [("ps", banks[i])]) for fc in range(8)] for i, c in enumerate(cs)])
            for cs, banks in (((0, 1), (4, 5)), ((2, 3), (6, 7))):
                proj_pair(512, cs, banks)
                for i, c in enumerate(cs):
                    k.evac(kT[:, c, Q * 512:(Q + 1) * 512], ps[banks[i]][:], [("ps", banks[i])], [("kT", Q)],
                           partial=True)
            if Q < 8:
                for cs, banks in (((0, 1), (4, 5)), ((2, 3), (6, 7))):
                    proj_pair(0, cs, banks)
                    for i, c in enumerate(cs):
                        k.act(qT[:, c, Q * 512:(Q + 1) * 512], ps[banks[i]][:], AF.Copy, [("ps", banks[i])],
                              [("qT", Q)], scale=0.125, partial=True)
                for cs, banks in (((0, 1), (4, 5)), ((2, 3), (6, 7))):
                    proj_pair(1536, cs, banks)
                    for i, c in enumerate(cs):
                        k.evac(qmq[Q % 2][:, c, :], ps[banks[i]][:], [("ps", banks[i])], [("qmq", Q % 2)],
                               partial=True)
                k.dma("sync", dr["qmT_d"][Q], qmq[Q % 2], [("qmq", Q % 2)], [("qmT_d", Q)])
                k.dma("sync", dr["xT_d"][Q], xq, [("xTq", Q % 2)], [("xT_d", Q)])
            for tts, banks in (((0, 1), (4, 5)), ((2, 3), (6, 7))):
                k.mmi([[(ps[banks[i]][:], xq[:, fc, tt * 128:(tt + 1) * 128], W[:, fc, 1024:1536], xr,
                         [("ps", banks[i])]) for fc in range(8)] for i, tt in enumerate(tts)])
                for i, tt in enumerate(tts):
                    t = 4 * Q + tt
                    k.evac(Vext[:, t, :, 0:64], ps[banks[i]][:].rearrange("p (h d) -> p h d", h=8),
                           [("ps", banks[i])], [("Vext", t)])
        P.barrier()
        A.reset(b1_mark)

        bias_i = A.alloc([8, 768], BF16)
        bias_s = [A.alloc([8, 768], BF16) for _ in range(2)]
        eS = [A.alloc([768], BF16) for _ in range(3)]
        yna = [A.alloc([512], BF16) for _ in range(2)]
        rc = [A.alloc([8], F32) for _ in range(2)]
        ynaTq = [A.alloc([4, 512], BF16) for _ in range(2)]
        for hh in range(8):
            k.dma("gpsimd", bias_i[:, hh, :], dr["biasT"][0, hh], [], [("bias_i", hh)])
        special = {0: 1, 1: 2, 30: 3, 31: 4}

        def tile_of_pair(lp):
            if 0 <= lp < 32:
                return lp
            return {-2: 32, -1: 33, 32: 34, 33: 35}[lp]
        nsp = 0

        def offs_of(qp):
            if qp == 0:
                return [-2, -1, 0, 1, 2, 3]
            if qp == 31:
                return [-3, -2, -1, 0, 1, 2]
            return [-2, -1, 0, 1, 2]
        bias_of = {}

        def na_finalize(qp):
            yq = yna[qp % 2]
            rcq = rc[qp % 2]
            for h2 in range(8):
                po2 = 4 + (qp % 2) * 2 + (h2 // 4)
                oc2 = (h2 % 4) * 65
                k.v("vector", "reciprocal", (rcq[:, h2:h2 + 1], ps[po2][:, oc2 + 64:oc2 + 65]),
                    [("ps", po2, h2 % 4)], [("rc", qp % 2, h2)])
                k.v("vector", "tensor_scalar", (yq[:, h2 * 64:(h2 + 1) * 64], ps[po2][:, oc2:oc2 + 64],
                                                 rcq[:, h2:h2 + 1], None, ALU.mult),
                    [("ps", po2, h2 % 4), ("rc", qp % 2, h2)], [("yna", qp % 2)], partial=True)

        def na_transposes(qp):
            yq = yna[qp % 2]
            Q = qp // 4
            pT = 4 + (qp % 2) * 2 + 1
            tv = psb[pT][:, 640:1024]
            pT2 = 4 + (qp % 2) * 2
            tv2 = psb[pT2][:, 640:768]
            k.tr(tv[:, 0:128], yq[:, 0:128], identb, [("yna", qp % 2), "identb"], [("ps", pT, "T")], partial=False)
            k.tr(tv2, yq[:, 384:512], identb, [("yna", qp % 2), "identb"], [("ps", pT2, "T")], partial=False)
            for cc in range(1, 3):
                k.tr(tv[:, cc * 128:(cc + 1) * 128], yq[:, cc * 128:(cc + 1) * 128], identb,
                     [("yna", qp % 2), "identb"], [("ps", pT, "T")], partial=True)
            yt = ynaTq[Q % 2]
            k.evac(yt[:, 0:3, (qp % 4) * 128:(qp % 4 + 1) * 128], tv.rearrange("p (a b) -> p a b", a=3),
                   [("ps", pT, "T")], [("ynaTq", Q % 2)], partial=True)
            k.evac(yt[:, 3, (qp % 4) * 128:(qp % 4 + 1) * 128], tv2, [("ps", pT2, "T")], [("ynaTq", Q % 2)],
                   partial=True)
            if qp % 4 == 3:
                k.dma("sync", dr["ynaT_d"][:, :, Q * 512:(Q + 1) * 512], yt, [("ynaTq", Q % 2)], [("ynaT_d", Q)])

        def pv_list(n):
            qp, h = n // 8, n % 8
            offs = offs_of(qp); ns = len(offs)
            es = eS[n % 3]; en = ("eS", n % 3)
            po = 4 + (qp % 2) * 2 + (h // 4)
            oc = (h % 4) * 65
            out = []
            for j, off in enumerate(offs):
                kt = tile_of_pair(qp + off)
                out.append((ps[po][:, oc:oc + 65], es[:, j * 128:(j + 1) * 128], Vext[:, kt, h, :], j == 0, j == ns - 1,
                            [en, ("Vext", kt), "Vext1"], [("ps", po, h % 4)]))
            return out
        NH = 256
        for n in range(NH + 1):
            qk = []
            if n < NH:
                qp, h = n // 8, n % 8
                offs = offs_of(qp); ns = len(offs)
                if h == 0:
                    if qp in special:
                        bt_ = bias_s[nsp % 2]; bname = ("bias_s", nsp % 2)
                        for hh in range(8):
                            k.dma("gpsimd", bt_[:, hh, :], dr["biasT"][special[qp], hh], [], [bname], key=bname,
                                  partial=(hh > 0))
                        bias_of[qp] = (bt_, [bname])
                        nsp += 1
                    else:
                        bias_of[qp] = (bias_i, None)
                bt_, breads = bias_of[qp]
                c = h // 2; pb0 = 64 * (h % 2)
                sX = (n % 2) * 2; sY = sX + 1
                br_ = breads if breads is not None else [("bias_i", h)]
                k.mm(ps[sX][:], identb, bt_[:, h, 0:512], True, False, br_ + ["identb"], [("ps", sX)])
                k.mm(ps[sY][:, 0:(ns - 4) * 128], identb, bt_[:, h, 512:512 + (ns - 4) * 128], True, False,
                     br_ + ["identb"], [("ps", sY)])
                for j, off in enumerate(offs):
                    kt = tile_of_pair(qp + off)
                    bank = sX if j < 4 else sY
                    last = (j == 3) or (j == ns - 1)
                    qk.append((ps[bank][:, (j % 4) * 128:(j % 4 + 1) * 128],
                               kT[pb0:pb0 + 64, c, kt * 128:(kt + 1) * 128],
                               qT[pb0:pb0 + 64, c, qp * 128:(qp + 1) * 128], False, last,
                               [("kT", kt // 4), ("qT", qp // 4)], [("ps", bank)]))
            pv = pv_list(n - 1) if n >= 1 else []
            for i in range(max(len(qk), len(pv))):
                if i < len(qk):
                    k.mm(*qk[i])
                if i < len(pv):
                    k.mm(*pv[i])
            if n < NH:
                es = eS[n % 3]; en = ("eS", n % 3)
                k.act(es[:, 0:512], ps[sX][:], AF.Exp, [("ps", sX)], [en])
                k.act(es[:, 512:ns * 128], ps[sY][:, 0:(ns - 4) * 128], AF.Exp, [("ps", sY)], [en], partial=True)
            if n >= 1 and (n - 1) % 8 == 7:
                na_finalize((n - 1) // 8)
            if n >= 1 and (n - 1) % 8 == 3 and (n - 1) // 8 >= 1:
                na_transposes((n - 1) // 8 - 1)
        na_transposes(31)
        P.barrier()
        A.reset(base_mark)

        wkv = A.alloc([8, 1024], BF16)
        memt = [A.alloc([1024], F32) for _ in range(2)]
        memT = A.alloc([8, 256], BF16)
        kmT = A.alloc([4, 256], BF16)
        Vm = A.alloc([2, 4, 129], BF16)
        qmq = [A.alloc([4, 512], BF16) for _ in range(2)]
        eSm = [A.alloc([512], BF16) for _ in range(4)]
        ym = [A.alloc([512], BF16) for _ in range(8)]
        rcm = [A.alloc([4], F32) for _ in range(8)]
        ymTq = [A.alloc([4, 512], BF16) for _ in range(2)]
        for cb in range(2):
            k.dma("gpsimd", wkv[:, :, cb * 512:(cb + 1) * 512],
                  dr["w_kv"][:, cb * 512:(cb + 1) * 512].rearrange("(c p) n -> p c n", p=128), [], [("wkv", cb)])
        k.v("vector", "memset", (Vm[:, :, :, 128:129], 1.0), [], ["Vm1"])
        for mc in range(2):
            k.dma("sync", memt[mc], dr["mem"][mc * 128:(mc + 1) * 128, :], [], [("memt", mc)])
            for hb in range(2):
                bank = 2 * mc + hb
                for q in range(4):
                    fc = hb * 4 + q
                    k.tr(ps[bank][:, q * 128:(q + 1) * 128], memt[mc][:, fc * 128:(fc + 1) * 128], identf,
                         [("memt", mc), "identf"], [("ps", bank)], partial=(q > 0))
                k.evac(memT[:, hb * 4:(hb + 1) * 4, mc * 128:(mc + 1) * 128],
                       ps[bank][:].rearrange("p (a b) -> p a b", a=4), [("ps", bank)], ["memT"], partial=True)
        for h in range(4):
            pb_ = 4 + h % 2
            for fc in range(8):
                k.mm(ps[pb_][:, 0:256], wkv[:, fc, h * 128:(h + 1) * 128], memT[:, fc, :], fc == 0, fc == 7,
                     ["memT", ("wkv", 0)], [("ps", pb_)])
            k.evac(kmT[:, h, :], ps[pb_][:, 0:256], [("ps", pb_)], ["kmT"], partial=True)
        for mc in range(2):
            pb_ = 6 + mc
            for fc in range(8):
                k.mm(ps[pb_][:], memT[:, fc, mc * 128:(mc + 1) * 128], wkv[:, fc, 512:1024], fc == 0, fc == 7,
                     ["memT", ("wkv", 1)], [("ps", pb_)])
            k.evac(Vm[:, mc, :, 0:128], ps[pb_][:].rearrange("p (h d) -> p h d", h=4), [("ps", pb_)], ["Vm"],
                   partial=True)
        msc = 128.0 ** -0.5
        k.dma("sync", qmq[0], dr["qmT_d"][0], [], [("qmq", 0)])
        ecnt = 0
        for Q in range(8):
            if Q + 1 < 8:
                k.dma("sync", qmq[(Q + 1) % 2], dr["qmT_d"][Q + 1], [], [("qmq", (Q + 1) % 2)])
            qq = qmq[Q % 2]
            for h in range(4):
                ebufs = []
                for mc in range(2):
                    bank = (ecnt % 2) * 2 + mc
                    k.mm(ps[bank][:], kmT[:, h, mc * 128:(mc + 1) * 128], qq[:, h, :], True, True,
                         ["kmT", ("qmq", Q % 2)], [("ps", bank)])
                    eb = (ecnt % 2) * 2 + mc
                    k.act(eSm[eb], ps[bank][:], AF.Exp, [("ps", bank)], [("eSm", eb)], scale=msc)
                    ebufs.append(eb)
                for tts in ((0, 2), (1, 3)):
                    k.mmi([[(ps[4 + (tt // 2) + 2 * (ecnt % 2)][:, (tt % 2) * 129:(tt % 2) * 129 + 129],
                             eSm[ebufs[mc]][:, tt * 128:(tt + 1) * 128], Vm[:, mc, h, :],
                             [("eSm", ebufs[mc]), "Vm", "Vm1"], [("ps", 4 + (tt // 2) + 2 * (ecnt % 2), tt % 2)])
                            for mc in range(2)] for tt in tts])
                for tt in range(4):
                    po = 4 + (tt // 2) + 2 * (ecnt % 2)
                    oc = (tt % 2) * 129
                    yb_ = (Q % 2) * 4 + tt
                    k.v("vector", "reciprocal", (rcm[yb_][:, h:h + 1], ps[po][:, oc + 128:oc + 129]),
                        [("ps", po, tt % 2)], [("rcm", yb_, h)])
                    k.v("vector", "tensor_scalar", (ym[yb_][:, h * 128:(h + 1) * 128], ps[po][:, oc:oc + 128],
                                                     rcm[yb_][:, h:h + 1], None, ALU.mult),
                        [("ps", po, tt % 2), ("rcm", yb_, h)], [("ym", yb_)], partial=True)
                ecnt += 1
            yt = ymTq[Q % 2]
            for tt in range(4):
                yb_ = (Q % 2) * 4 + tt
                bank = (tt % 2) * 2 + (ecnt % 2)
                tvm = psb[bank][:, 0:512]
                for cc in range(4):
                    k.tr(tvm[:, cc * 128:(cc + 1) * 128], ym[yb_][:, cc * 128:(cc + 1) * 128], identb,
                         [("ym", yb_), "identb"], [("ps", bank)], partial=(cc > 0))
                k.evac(yt[:, :, tt * 128:(tt + 1) * 128], tvm.rearrange("p (a b) -> p a b", a=4), [("ps", bank)],
                       [("ymTq", Q % 2)], partial=True)
            k.dma("sync", dr["ymemT_d"][:, :, Q * 512:(Q + 1) * 512], yt, [("ymTq", Q % 2)], [("ymemT_d", Q)])
        P.barrier()
        A.reset(base_mark)

        wg = A.alloc([8, 3072], BF16)
        wo3 = [A.alloc([4, 1024], BF16) for _ in range(3)]
        wout = A.alloc([8, 1024], BF16)
        wrt = A.alloc([8, 72], F32)
        bgt = A.alloc([24], F32)
        brt = A.alloc([72], F32); onesrow = A.alloc([128], F32)
        lng = A.alloc([1024], F32); lnb = A.alloc([1024], F32)
        ect = A.alloc([64], F32); mhalf = A.alloc([1], F32)
        atot = A.alloc([64], BF16)
        xTq = [A.alloc([8, 512], BF16) for _ in range(2)]
        ybq = [[A.alloc([4, 512], BF16) for _ in range(2)] for _ in range(3)]
        G = [[A.alloc([512], BF16) for _ in range(2)] for _ in range(3)]
        tm = [A.alloc([512], F32) for _ in range(4)]
        mT = A.alloc([8, 512], BF16)
        xt = [A.alloc([1024], F32) for _ in range(2)]
        rt = [A.alloc([1024], F32) for _ in range(2)]
        ht = [A.alloc([1024], F32) for _ in range(2)]
        hl = [A.alloc([1024], F32) for _ in range(2)]
        hb = [A.alloc([1024], BF16) for _ in range(2)]
        hT = A.alloc([8, 128], F32)
        sm = A.alloc([256], F32)
        k.A1 = A.alloc([64], F32); k.A2 = A.alloc([64], F32); k.Ab = A.alloc([64], BF16)
        for cb in range(6):
            k.dma("gpsimd", wg[:, :, cb * 512:(cb + 1) * 512],
                  dr["w_gate"][:, cb * 512:(cb + 1) * 512].rearrange("(c p) n -> p c n", p=128), [], [("wg", cb)])
        for i, nm in enumerate(("w_fo", "w_nao", "w_memo")):
            k.dma("gpsimd", wo3[i], dr[nm].rearrange("(c p) n -> p c n", p=128), [], [("wo3", i)])
        for cb in range(2):
            k.dma("gpsimd", wout[:, :, cb * 512:(cb + 1) * 512],
                  dr["w_out"][:, cb * 512:(cb + 1) * 512].rearrange("(c p) n -> p c n", p=128), [], [("wout", cb)])
        k.dma("sync", wrt, dr["wr"].rearrange("(c p) n -> p c n", p=128), [], ["wrt"])
        k.dma("sync", bgt, dr["bg"], [], ["bgt"])
        k.dma("sync", brt[0:1, :], dr["br"], [], ["brt"])
        k.dma("sync", onesrow[0:1, :], dr["c_onesrow"], [], ["onesrow"])
        k.dma("sync", lng, dr["ln1_g"].partition_broadcast(128)[:, 0, :], [], ["lng"])
        k.dma("sync", lnb, dr["ln1_b"].partition_broadcast(128)[:, 0, :], [], ["lnb"])
        k.dma("sync", ect, dr["c_ec"], [], ["ect"])
        k.dma("sync", mhalf, dr["c_mhalf"], [], ["mhalf"])
        k.v("vector", "memset", (atot, 0.0), [], ["atot"])
        ysrc = ("yfT_d", "ynaT_d", "ymemT_d")

        def c_load(Q):
            k.dma("sync", xTq[Q % 2], dr["xT_d"][Q], [], [("xTq", Q % 2)])
            for i in range(3):
                k.dma("sync", ybq[i][Q % 2], dr[ysrc[i]][:, :, Q * 512:(Q + 1) * 512], [], [("ybq", i, Q % 2)])
        c_load(0)
        wgr = [("wg", cb) for cb in range(6)]

        def r1(ti):
            b2 = ti % 2
            k.dma("sync", hl[b2], dr["h_d"][ti * 128:(ti + 1) * 128, :], [("h_d", ti)], [("hl", b2)])
            k.act(hb[b2], hl[b2], AF.Copy, [("hl", b2)], [("hb", b2)])
            for hh in range(2):
                for q in range(4):
                    k.tr(ps[6][:, q * 128:(q + 1) * 128], hl[b2][:, (hh * 4 + q) * 128:(hh * 4 + q + 1) * 128], identf,
                         [("hl", b2), "identf"], [("ps", 6)], partial=(q > 0))
                k.evac(hT[:, hh * 4:(hh + 1) * 4, :], ps[6][:].rearrange("p (a b) -> p a b", a=4),
                       [("ps", 6)], [("hT", hh)])

        def r2(ti):
            for fc in range(8):
                k.mm(ps[7][:, 0:72], hT[:, fc, :], wrt[:, fc, :], fc == 0, False,
                     [("hT", 0), ("hT", 1), "wrt"], [("ps", 7)])
            k.mm(ps[7][:, 0:72], onesrow[0:1, :], brt[0:1, :], False, True, ["onesrow", "brt"], [("ps", 7)])

        def r3(ti):
            emit_route(k, ps, sm, ti, slots_i, rw, atot, ustrict, ones_b, ect, 7, 1)

        def r4(ti):
            b2 = ti % 2
            emit_route(k, ps, sm, ti, slots_i, rw, atot, ustrict, ones_b, ect, 7, 2)
            for kk in range(2):
                P.op("gpsimd", (lambda e, ti=ti, kk=kk, b2=b2: e.indirect_dma_start(
                    out=dr["xg_d"][:, :],
                    out_offset=bass.IndirectOffsetOnAxis(ap=slots_i[:, ti, kk:kk + 1], axis=0),
                    in_=hb[b2][:, :], in_offset=None, bounds_check=_bc_reg(e), oob_is_err=False)),
                    reads=[("hb", b2), ("slots", ti)], writes=["xg_d"], dma=True, key=("xg_sc", b2), partial=True)

        def route_sched(Qp):
            t = [Qp * 4 + i for i in range(4)]
            return [[lambda: r1(t[0])], [lambda: r2(t[0]), lambda: r3(t[0])],
                    [lambda: r4(t[0]), lambda: r1(t[1])], [lambda: r2(t[1]), lambda: r3(t[1])],
                    [lambda: r4(t[1]), lambda: r1(t[2])], [lambda: r2(t[2]), lambda: r3(t[2])],
                    [lambda: r4(t[2]), lambda: r1(t[3])], [lambda: r2(t[3]), lambda: r3(t[3])],
                    [lambda: r4(t[3])]]
        for Q in range(9):
            sched = route_sched(Q - 1) if Q >= 1 else [[] for _ in range(9)]
            if Q < 8:
                if Q + 1 < 8:
                    c_load(Q + 1)
                xq = xTq[Q % 2]
            for dc in range(8):
                if Q < 8:
                    k.mmi([[(ps[i][:], wg[:, fc, i * 1024 + dc * 128:i * 1024 + (dc + 1) * 128], xq[:, fc, :],
                             wgr + [("xTq", Q % 2)], [("ps", i)]) for fc in range(8)] for i in range(3)])
                    for i in range(3):
                        k.act(G[i][dc % 2], ps[i][:], AF.Sigmoid, [("ps", i), "bgt"], [("G", i, dc % 2)],
                              bias=bgt[:, i * 8 + dc:i * 8 + dc + 1])
                    k.mmi([[(ps[3 + i][:], wo3[i][:, cc, dc * 128:(dc + 1) * 128], ybq[i][Q % 2][:, cc, :],
                             [("wo3", i), ("ybq", i, Q % 2)], [("ps", 3 + i)]) for cc in range(4)] for i in range(3)])
                for f_ in sched[dc]:
                    f_()
                if Q < 8:
                    for i in range(3):
                        k.v("vector", "tensor_tensor", (tm[i], G[i][dc % 2], ps[3 + i][:], ALU.mult),
                            [("G", i, dc % 2), ("ps", 3 + i)], [("tm", i)])
                    k.v("gpsimd", "tensor_tensor", (tm[3], tm[0], tm[1], ALU.add), [("tm", 0), ("tm", 1)], [("tm", 3)])
                    k.v("gpsimd", "tensor_tensor", (mT[:, dc, :], tm[3], tm[2], ALU.add), [("tm", 3), ("tm", 2)],
                        [("mT", dc)])
            if Q == 8:
                for f_ in sched[8]:
                    f_()
                break
            mTr = [("mT", dc) for dc in range(8)]
            k.dma("sync", xt[0], xo_t[Q * 4], [], [("xt", 0)])
            for tt in range(4):
                ti = Q * 4 + tt
                b2 = ti % 2
                if tt + 1 < 4:
                    k.dma("sync", xt[(tt + 1) % 2], xo_t[ti + 1], [], [("xt", (tt + 1) % 2)])
                pbk = (6, 7) if tt % 2 == 0 else (4, 5)
                k.mmi([[(ps[pbk[half]][:], mT[:, dc, tt * 128:(tt + 1) * 128], wout[:, dc, half * 512:(half + 1) * 512],
                         mTr + [("wout", half)], [("ps", pbk[half])]) for dc in range(8)] for half in range(2)])
                for half in range(2):
                    pm = pbk[half]
                    k.v("vector", "scalar_tensor_tensor",
                        (rt[b2][:, half * 512:(half + 1) * 512], xt[tt % 2][:, half * 512:(half + 1) * 512], ALPHA,
                         ps[pm][:], ALU.mult, ALU.add),
                        [("xt", tt % 2), ("ps", pm)], [("rt", b2, half)])
                emit_ln(k, rt[b2], [("rt", b2, 0), ("rt", b2, 1)], ht[b2], ("ht", b2), lng, lnb, ["lng", "lnb"],
                        sm, ("smln",), mhalf)
                k.dma("sync", dr["h_d"][ti * 128:(ti + 1) * 128, :], ht[b2], [("ht", b2)], [("h_d", ti)])
                if tt == 1:
                    for f_ in sched[8]:
                        f_()
        P.barrier()
        A.reset(base_mark)
        if debug:
            dbgt = A.alloc([32, 4], F32)
            k.v("vector", "tensor_copy", (dbgt[:, :, 0:2], slots_i), [], ["dbgt0"])
            k.v("vector", "tensor_copy", (dbgt[:, :, 2:4], rw), [], ["dbgt1"])
            k.dma("sync", dr["dbg_rt"], dbgt, ["dbgt0", "dbgt1"], ["dbg_rt"])
            k.dma("sync", dr["dbg_h"], dr["h_d"], [], ["dbg_h"])
            for i_, nm_ in enumerate(("yfT_d", "ynaT_d", "ymemT_d")):
                k.dma("sync", dr["dbg_y3"][i_], dr[nm_], [], [("dbg_y3", i_)])

        weg = [A.alloc([8, 512], BF16) for _ in range(3)]
        weu = [A.alloc([8, 512], BF16) for _ in range(3)]
        wed = [A.alloc([4, 1024], BF16) for _ in range(3)]
        xg = [A.alloc([2, 1024], BF16) for _ in range(3)]
        xgT = [A.alloc([8, 256], BF16) for _ in range(2)]
        sg = [A.alloc([256], F32) for _ in range(2)]
        aT = [A.alloc([4, 256], BF16) for _ in range(2)]
        yo = [A.alloc([1024], BF16) for _ in range(2)]

        def e_load(e_):
            s = e_ % 3
            k.dma("gpsimd", weg[s], dr["w_eg"][e_].rearrange("(c p) n -> p c n", p=128), [], [("weg", s)])
            k.dma("gpsimd", weu[s], dr["w_eu"][e_].rearrange("(c p) n -> p c n", p=128), [], [("weu", s)])
            k.dma("gpsimd", wed[s], dr["w_ed"][e_].rearrange("(c p) n -> p c n", p=128), [], [("wed", s)])
            k.dma("sync", xg[e_ % 3], dr["xg_d"][e_ * CAP:(e_ + 1) * CAP, :].rearrange("(t p) f -> p t f", p=128),
                  ["xg_d"], [("xg", e_ % 3)])
        e_load(0); e_load(1)

        def e_transposes(ee):
            xgt_ = xgT[ee % 2]
            k.tri([[(psb[tt][:, fc * 128:(fc + 1) * 128], xg[ee % 3][:, tt, fc * 128:(fc + 1) * 128], identb,
                     [("xg", ee % 3), "identb"], [("ps", tt)]) for fc in range(8)] for tt in range(2)])
            for tt in range(2):
                k.evac(xgt_[:, :, tt * 128:(tt + 1) * 128], psb[tt][:].rearrange("p (a b) -> p a b", a=8),
                       [("ps", tt)], [("xgT", ee % 2)], partial=True)
        yc = 0
        for e_ in range(NEXP):
            if e_ + 2 < NEXP:
                e_load(e_ + 2)
            s = e_ % 3
            xgt = xgT[e_ % 2]
            if e_ == 0:
                e_transposes(0)
            at = aT[e_ % 2]
            for dcn in range(4):
                pg = 2 + (dcn % 2) * 2; pu = pg + 1
                k.mmi([[(ps[pg][:, 0:256], weg[s][:, fc, dcn * 128:(dcn + 1) * 128], xgt[:, fc, :],
                         [("weg", s), ("xgT", e_ % 2)], [("ps", pg)]) for fc in range(8)],
                       [(ps[pu][:, 0:256], weu[s][:, fc, dcn * 128:(dcn + 1) * 128], xgt[:, fc, :],
                         [("weu", s), ("xgT", e_ % 2)], [("ps", pu)]) for fc in range(8)]])
                k.act(sg[dcn % 2], ps[pg][:, 0:256], AF.Silu, [("ps", pg)], [("sg", dcn % 2)])
                k.v("vector", "tensor_tensor", (at[:, dcn, :], sg[dcn % 2], ps[pu][:, 0:256], ALU.mult),
                    [("sg", dcn % 2), ("ps", pu)], [("aT", e_ % 2, dcn)])
            atr = [("aT", e_ % 2, dcn) for dcn in range(4)]
            if e_ + 1 < NEXP:
                e_transposes(e_ + 1)
            for tt in range(2):
                yt = yo[yc % 2]
                k.mmi([[(ps[6 + half][:], at[:, dcn, tt * 128:(tt + 1) * 128], wed[s][:, dcn, half * 512:(half + 1) * 512],
                         atr + [("wed", s)], [("ps", 6 + half)]) for dcn in range(4)] for half in range(2)])
                for half in range(2):
                    pd = 6 + half
                    k.evac(yt[:, half * 512:(half + 1) * 512], ps[pd][:], [("ps", pd)], [("yo", yc % 2, half)])
                k.dma("sync", dr["yb_d"][e_ * CAP + tt * 128:e_ * CAP + (tt + 1) * 128, :], yt,
                      [("yo", yc % 2, 0), ("yo", yc % 2, 1)], ["yb_d"], key=("yo_st", yc % 2), partial=True)
                yc += 1
        P.barrier()
        A.reset(base_mark)

        lng = A.alloc([1024], F32); lnb = A.alloc([1024], F32); mhalf = A.alloc([1], F32)
        sm = A.alloc([256], F32)
        NB = 4
        hd = [A.alloc([1024], F32) for _ in range(NB)]
        g1 = [A.alloc([1024], BF16) for _ in range(NB)]
        g2 = [A.alloc([1024], BF16) for _ in range(NB)]
        acc = [A.alloc([1024], F32) for _ in range(NB)]
        ot = [A.alloc([1024], F32) for _ in range(NB)]
        smd = [A.alloc([32], F32) for _ in range(NB)]
        k.dma("sync", lng, dr["ln2_g"].partition_broadcast(128)[:, 0, :], [], ["lng"])
        k.dma("sync", lnb, dr["ln2_b"].partition_broadcast(128)[:, 0, :], [], ["lnb"])
        k.dma("sync", mhalf, dr["c_mhalf"], [], ["mhalf"])

        def d_load(ti):
            b2 = ti % NB
            k.dma("sync", hd[b2], dr["h_d"][ti * 128:(ti + 1) * 128, :], [], [("hd", b2)])
            for kk, gt in enumerate((g1, g2)):
                P.op("gpsimd", (lambda e, ti=ti, kk=kk, gt=gt, b2=b2: e.indirect_dma_start(
                    out=gt[b2][:, :], out_offset=None, in_=dr["yb_d"][:, :],
                    in_offset=bass.IndirectOffsetOnAxis(ap=slots_i[:, ti, kk:kk + 1], axis=0),
                    bounds_check=_bc_reg(e), oob_is_err=False)),
                    reads=[], writes=[("g", kk, b2)], dma=True)
        d_load(0); d_load(1)
        for ti in range(32):
            if ti + 2 < 32:
                d_load(ti + 2)
            b2 = ti % NB
            k.act(acc[b2], hd[b2], AF.Copy, [("hd", b2)], [("acc", b2)], scale=ALPHA)
            k.v("vector", "scalar_tensor_tensor", (acc[b2], g1[b2], rw[:, ti, 0:1], acc[b2], ALU.mult, ALU.add),
                [("g", 0, b2), ("acc", b2)], [("acc", b2)])
            k.v("vector", "scalar_tensor_tensor", (acc[b2], g2[b2], rw[:, ti, 1:2], acc[b2], ALU.mult, ALU.add),
                [("g", 1, b2), ("acc", b2)], [("acc", b2)])
            emit_ln(k, acc[b2], [("acc", b2)], ot[b2], ("ot", b2), lng, lnb, ["lng", "lnb"], smd[b2], ("smln", b2),
                    mhalf, tag=b2)
            k.dma("sync", dr["out"][ti * 128:(ti + 1) * 128, :], ot[b2], [("ot", b2)], [("out", ti)])
        P.emit()
    return nc


def emit_ln(k, src, src_names, dst, dst_name, g, b, gb_names, sm, smn, mhalf, tag=0):
    st = sm[:, 0:12].rearrange("p (a b) -> p a b", a=2)
    mv = sm[:, 12:14]
    rs = sm[:, 14:15]
    nb = sm[:, 15:16]
    T = lambda x: (x, tag)
    for half in range(2):
        k.v("vector", "bn_stats", (st[:, half, :], src[:, half * 512:(half + 1) * 512]), src_names,
            [T("lnst%d" % half)])
    k.v("vector", "bn_aggr", (mv, st), [T("lnst0"), T("lnst1")], [T("lnmv")])
    k.v("gpsimd", "tensor_scalar", (rs, mv[:, 1:2], LN_EPS, 1.0, ALU.add, ALU.mult), [T("lnmv")], [T("lnrs0")])
    k.v("gpsimd", "tensor_tensor", (rs, rs, mhalf, ALU.pow), [T("lnrs0"), "mhalf"], [T("lnrs")])
    k.v("vector", "scalar_tensor_tensor", (nb, mv[:, 0:1], -1.0, rs, ALU.mult, ALU.mult), [T("lnmv"), T("lnrs")],
        [T("lnnb")])
    k.act(dst, src, AF.Identity, src_names + [T("lnrs"), T("lnnb")], [dst_name + ("n",), dst_name], bias=nb, scale=rs)
    k.v("gpsimd", "tensor_tensor", (dst[:, 0:512], dst[:, 0:512], g[:, 0:512], ALU.mult),
        [dst_name + ("n",), gb_names[0]], [dst_name + ("g0",)])
    k.v("vector", "tensor_tensor", (dst[:, 512:1024], dst[:, 512:1024], g[:, 512:1024], ALU.mult),
        [dst_name + ("n",), gb_names[0]], [dst_name + ("g1",)])
    k.v("vector", "tensor_tensor", (dst, dst, b, ALU.add), [dst_name + ("g0",), dst_name + ("g1",), gb_names[1]],
        [dst_name])


def emit_route(k, ps, sm, ti, slots_i, rw, atot, ustrict, ones_b, ect, pl=2, stage=0):
    L = sm[:, 16:88]
    gm8 = sm[:, 88:96]
    ohg = sm[:, 96:104]
    dd = sm[:, 104:112]
    pg = sm[:, 112:113]
    el = sm[:, 113:121]
    tm8 = sm[:, 121:129]
    oh1 = sm[:, 129:137]
    oh2 = sm[:, 137:145]
    dv = sm[:, 145:146]
    t64 = sm[:, 146:210]
    sl = sm[:, 210:212]
    n = lambda s: ("rt_" + s,)
    V = lambda name, args, r, w: k.v("vector", name, args, r, w)
    if stage == 2:
        return _route_p2(k, ps, sm, ti, slots_i, atot, ustrict, ones_b, ect, pl)
    V("tensor_copy", (L, ps[pl][:, 0:72]), [("ps", pl)], [n("L")])
    V("max", (gm8, L[:, 0:8]), [n("L")], [n("gm8")])
    V("tensor_scalar", (ohg, L[:, 0:8], gm8[:, 0:1], None, ALU.is_equal), [n("L"), n("gm8")], [n("ohg")])
    V("tensor_scalar", (dd, L[:, 0:8], gm8[:, 0:1], None, ALU.subtract), [n("L"), n("gm8")], [n("dd")])
    k.act(dd, dd, AF.Sigmoid, [n("dd")], [n("dd")])
    V("tensor_scalar", (tm8, dd, -1.0, 1.0, ALU.mult, ALU.add), [n("dd")], [n("tm8")])
    V("reciprocal", (tm8, tm8), [n("tm8")], [n("tm8")])
    V("tensor_tensor", (dd, dd, tm8, ALU.mult), [n("dd"), n("tm8")], [n("dd")])
    V("tensor_reduce", (pg, dd, AX.X, ALU.add), [n("dd")], [n("pg")])
    V("reciprocal", (pg, pg), [n("pg")], [n("pg")])
    t3 = t64.rearrange("p (g e) -> p g e", g=8)
    L3 = L[:, 8:72].rearrange("p (g e) -> p g e", g=8)
    V("tensor_tensor", (t3, L3, ohg.unsqueeze(2).to_broadcast([128, 8, 8]), ALU.mult), [n("L"), n("ohg")], [n("t64")])
    V("tensor_reduce", (el, t64.rearrange("p (g e) -> p e g", g=8), AX.X, ALU.add), [n("t64")], [n("el")])
    V("max", (tm8, el), [n("el"), n("tm8")], [n("tm8")])
    V("tensor_scalar", (oh1, el, tm8[:, 0:1], None, ALU.is_equal), [n("el"), n("tm8")], [n("oh1")])
    V("tensor_scalar", (oh2, el, tm8[:, 1:2], None, ALU.is_equal), [n("el"), n("tm8")], [n("oh2")])
    V("tensor_tensor", (dv, tm8[:, 0:1], tm8[:, 1:2], ALU.subtract), [n("tm8")], [n("dv")])
    k.act(dv, dv, AF.Sigmoid, [n("dv")], [n("dv")])
    V("tensor_tensor", (rw[:, ti, 0:1], dv, pg, ALU.mult), [n("dv"), n("pg")], [("rw", ti, 0)])
    V("tensor_tensor", (rw[:, ti, 1:2], pg, rw[:, ti, 0:1], ALU.subtract), [n("pg"), ("rw", ti, 0)], [("rw", ti, 1)])
    A1 = k.A1
    A2 = k.A2
    Ab = k.Ab
    A13 = A1.rearrange("p (g e) -> p g e", g=8)
    A23 = A2.rearrange("p (g e) -> p g e", g=8)
    gb = ohg.unsqueeze(2).to_broadcast([128, 8, 8])
    V("tensor_tensor", (A13, gb, oh1.unsqueeze(1).to_broadcast([128, 8, 8]), ALU.mult), [n("ohg"), n("oh1")], [n("A1")])
    V("tensor_tensor", (A23, gb, oh2.unsqueeze(1).to_broadcast([128, 8, 8]), ALU.mult), [n("ohg"), n("oh2")], [n("A2")])
    V("tensor_tensor", (Ab, A1, A2, ALU.add), [n("A1"), n("A2")], [n("Ab")])
    if stage == 1:
        return
    _route_p2(k, ps, sm, ti, slots_i, atot, ustrict, ones_b, ect, pl)


def _route_p2(k, ps, sm, ti, slots_i, atot, ustrict, ones_b, ect, pl):
    t64 = sm[:, 146:210]
    sl = sm[:, 210:212]
    n = lambda s: ("rt_" + s,)
    V = lambda name, args, r, w: k.v("vector", name, args, r, w)
    A1 = k.A1
    A2 = k.A2
    Ab = k.Ab
    k.mm(ps[pl][:, 128:192], ustrict, Ab, True, False, ["ustrict", n("Ab")], [("ps", pl)])
    k.mm(ps[pl][:, 128:192], ones_b, atot, False, True, ["ones_b", "atot"], [("ps", pl)])
    V("tensor_tensor", (t64, ps[pl][:, 128:192], ect, ALU.add), [("ps", pl), "ect"], [n("t64")])
    V("tensor_tensor", (atot, atot, Ab, ALU.add), [n("Ab"), "atot"], ["atot"])
    V("tensor_tensor", (A1, A1, t64, ALU.mult), [n("A1"), n("t64")], [n("A1")])
    V("tensor_tensor", (A2, A2, t64, ALU.mult), [n("A2"), n("t64")], [n("A2")])
    V("tensor_reduce", (sl[:, 0:1], A1, AX.X, ALU.add), [n("A1")], [n("sl0")])
    V("tensor_reduce", (sl[:, 1:2], A2, AX.X, ALU.add), [n("A2")], [n("sl1")])
    V("tensor_copy", (slots_i[:, ti, :], sl), [n("sl0"), n("sl1")], [("slots", ti)])


_NC_CACHE = {}


def _consts(hf):
    bf = ml_dtypes.bfloat16
    c = {}
    c["c_identf"] = np.eye(128, dtype=np.float32)
    c["c_identb"] = np.eye(128, dtype=np.float32).astype(bf)
    c["c_ones"] = np.ones((128, 128), np.float32).astype(bf)
    c["c_ustrict"] = np.triu(np.ones((128, 128), np.float32), 1).astype(bf)
    row = np.arange(128)
    k2 = np.arange(128)
    ed = np.zeros((64, 128, 256), np.float32)
    for j in range(64):
        s = j + 64 * row
        th = 2.0 * np.pi * ((s[:, None] * k2[None, :]) % 8192) / 8192.0
        ed[j, :, 0:128] = np.cos(th) / 32.0
        ed[j, :, 128:256] = np.sin(th) / 32.0
    c["c_edft"] = ed.astype(bf)
    ph = 2.0 * np.pi * ((row[:, None] * row[None, :]) % 128) / 128.0
    C = np.cos(ph); S = np.sin(ph)
    c["c_fca"] = np.concatenate([C, -S], axis=1).astype(np.float32).astype(bf)
    c["c_fcb"] = np.concatenate([-S, -C], axis=1).astype(np.float32).astype(bf)
    s1 = np.arange(64)
    k1 = 32 * hf + np.arange(32)
    psi = 2.0 * np.pi * ((s1[:, None] * k1[None, :]) % 64) / 64.0
    bdc = np.zeros((128, 64), np.float32); bds = np.zeros((128, 64), np.float32)
    for a in range(2):
        bdc[a::2, a * 32:(a + 1) * 32] = np.cos(psi) / 32.0
        bds[a::2, a * 32:(a + 1) * 32] = np.sin(psi) / 32.0
    c["c_bdc"] = bdc.astype(bf); c["c_bds"] = bds.astype(bf)
    c["c_ec"] = np.tile((np.arange(64, dtype=np.float32) * CAP)[None, :], (128, 1)).astype(np.float32)
    c["c_mhalf"] = np.full((128, 1), -0.5, np.float32)
    c["c_onesrow"] = np.ones((1, 128), np.float32)
    return c


def _bias_tiles(rpb, hf):
    out = np.full((5, 8, 128, 768), MASKV, np.float32)
    specs = [(10, [-2, -1, 0, 1, 2]), (0, [-2, -1, 0, 1, 2, 3]), (1, [-2, -1, 0, 1, 2]),
             (30, [-2, -1, 0, 1, 2]), (31, [-3, -2, -1, 0, 1, 2])]
    qc = np.arange(64); kc = np.arange(64)
    cs = np.clip(qc - 8, 0, 48)
    colvalid = (kc[:, None] >= cs[None, :]) & (kc[:, None] < cs[None, :] + 16)
    dc = kc[:, None] - qc[None, :] + 15
    dcc = np.clip(dc, 0, 30)
    for ty, (qp, offs) in enumerate(specs):
        for j, off in enumerate(offs):
            kp = qp + off
            for aq in range(2):
                r = 64 * hf + 2 * qp + aq
                rs = min(max(r - 4, 0), 120)
                for ak in range(2):
                    kr = 64 * hf + 2 * kp + ak
                    if kr < 0 or kr > 127 or kr < rs or kr > rs + 7:
                        continue
                    drr = kr - r + 7
                    vals = rpb[:, drr, :][:, dcc]
                    blk = np.where(colvalid[None], vals, np.float32(MASKV))
                    out[ty, :, ak * 64:(ak + 1) * 64, j * 128 + aq * 64:j * 128 + (aq + 1) * 64] = blk
    return out


def kernel(x, mem, w_in, w_gate, b_gate, w_mem_kv, rpb, w_fourier_o, w_na_o, w_mem_o, w_out, ln1_g, ln1_b,
           w_router_group, b_router_group, w_router_expert, b_router_expert, w_exp_gate, w_exp_up, w_exp_down,
           ln2_g, ln2_b, _debug=False):
    f = lambda a: np.ascontiguousarray(np.asarray(a, dtype=np.float32))
    x = f(x); mem = f(mem)
    key = bool(_debug)
    if key not in _NC_CACHE:
        _NC_CACHE[key] = build_program(debug=_debug)
    nc = _NC_CACHE[key]
    shared = {
        "w_in": f(w_in[0]), "w_gate": f(w_gate[0]),
        "bg": f(np.asarray(b_gate[0]).reshape(24, 128).T),
        "w_kv": f(w_mem_kv[0]), "w_fo": f(w_fourier_o[0]), "w_nao": f(w_na_o[0]), "w_memo": f(w_mem_o[0]),
        "w_out": f(w_out[0]), "ln1_g": f(ln1_g), "ln1_b": f(ln1_b), "ln2_g": f(ln2_g), "ln2_b": f(ln2_b),
        "wr": f(np.concatenate([np.asarray(w_router_group[0]), np.asarray(w_router_expert[0])], axis=1)),
        "br": f(np.concatenate([np.asarray(b_router_group[0]), np.asarray(b_router_expert[0])])[None, :]),
        "w_eg": f(w_exp_gate[0]), "w_eu": f(w_exp_up[0]), "w_ed": f(w_exp_down[0]),
    }
    rp = f(rpb[0])
    in_maps = []
    for c in range(NCORES):
        b, hf = c // 2, c % 2
        m = dict(shared)
        m.update(_consts(hf))
        m["xf"] = x[b]
        xo = np.zeros((4608, D), np.float32)
        xo[0:4096] = x[b, 4096 * hf:4096 * hf + 4096]
        if hf == 1:
            xo[4096:4352] = x[b, 4096 - 256:4096]
        else:
            xo[4352:4608] = x[b, 4096:4096 + 256]
        m["xo"] = xo
        m["mem"] = mem[b]
        m["biasT"] = _bias_tiles(rp, hf)
        in_maps.append(m)
    res = run_bass_kernel_spmd(nc, in_maps, core_ids=list(range(NCORES)))
    out = np.zeros((4, 8192, D), np.float32)
    for c in range(NCORES):
        b, hf = c // 2, c % 2
        out[b, 4096 * hf:4096 * hf + 4096] = res.results[c]["out"]
    if _debug:
        return out, res
    return out
```

```python
import contextlib
import math
import numpy as np
import ml_dtypes
import concourse.bass as bass
import concourse.mybir as mybir
from concourse.bass_utils import run_bass_kernel_spmd

F32 = mybir.dt.float32
BF16 = mybir.dt.bfloat16
I32 = mybir.dt.int32
U8 = mybir.dt.uint8
ALU = mybir.AluOpType
AF = mybir.ActivationFunctionType
AX = mybir.AxisListType

ENGS = ("tensor", "vector", "scalar", "gpsimd", "sync")
NCORES = 8
D = 1024
CAP = 256
NEXP = 64
NSLOT = NEXP * CAP
ALPHA = 2.0 ** 0.25
LN_EPS = 1e-5
MASKV = -30000.0
ARENA = 206 * 1024


class Op:
    __slots__ = ("eng", "fn", "dma", "deps", "sig", "sigval", "key", "dval", "slot")

    def __init__(self, eng, fn, dma):
        self.eng = eng
        self.fn = fn
        self.dma = dma
        self.deps = []
        self.sig = False
        self.sigval = 0
        self.key = None
        self.dval = 0
        self.slot = None


class Prog:
    def __init__(self, nc):
        self.nc = nc
        self.ops = {e: [] for e in ENGS}
        self.writers = {}
        self.readers = {}
        self.key_slot = {}
        self.slot_count = []
        self.nops = 0

    def op(self, eng, fn, reads=(), writes=(), dma=False, key=None, partial=False):
        o = Op(eng, fn, dma)
        self.nops += 1
        deps = {}
        banks = set()
        for b in list(reads) + list(writes):
            if isinstance(b, tuple) and b and b[0] == "ps":
                banks.add(b[1])
        for bk in banks:
            nm = ("psbank", bk)
            for w in self.writers.get(nm, ()):
                deps[id(w)] = w
            self.writers[nm] = [o]
        for b in reads:
            for w in self.writers.get(b, ()):
                deps[id(w)] = w
        for b in writes:
            if not partial:
                for w in self.writers.get(b, ()):
                    deps[id(w)] = w
            for r in self.readers.get(b, ()):
                deps[id(r)] = r
        for b in reads:
            self.readers.setdefault(b, []).append(o)
        for b in writes:
            if partial:
                self.writers.setdefault(b, []).append(o)
            else:
                self.writers[b] = [o]
                self.readers[b] = []
        deps.pop(id(o), None)
        o.deps = list(deps.values())
        if dma:
            if key is None:
                key = writes[0] if writes else reads[0]
            if key not in self.key_slot:
                self.key_slot[key] = len(self.key_slot)
                if len(self.key_slot) > len(self.slot_count):
                    self.slot_count.append(0)
            s = self.key_slot[key]
            self.slot_count[s] += 1
            o.slot = s
            o.dval = 16 * self.slot_count[s]
        self.ops[eng].append(o)
        return o

    def barrier(self):
        lasts = []
        for e in ENGS:
            nd = [o for o in self.ops[e] if not o.dma and o.fn is not None]
            if nd:
                lasts.append(nd[-1])
        dmas = {}
        for e in ENGS:
            for o in self.ops[e]:
                if o.dma and (o.slot not in dmas or dmas[o.slot].dval < o.dval):
                    dmas[o.slot] = o
        for e in ENGS:
            o = Op(e, None, False)
            o.deps = [l for l in lasts if l.eng != e] + list(dmas.values())
            self.ops[e].append(o)
        self.writers = {}
        self.readers = {}
        self.key_slot = {}

    def emit(self):
        nc = self.nc
        for e in ENGS:
            for o in self.ops[e]:
                for d in o.deps:
                    if not d.dma and not (d.eng == "tensor" and o.eng == "tensor"):
                        d.sig = True
        for e in ENGS:
            c = 0
            for o in self.ops[e]:
                if o.sig:
                    c += 1
                    o.sigval = c
        nslots = len(self.slot_count)
        with contextlib.ExitStack() as st:
            esem = {e: st.enter_context(nc.semaphore("e_" + e)) for e in ENGS}
            ksem = [st.enter_context(nc.semaphore("d%d" % i)) for i in range(nslots)]
            block = st.enter_context(nc.Block())

            def body(e):
                def _f(eng):
                    waited = {}
                    for o in self.ops[e]:
                        for d in o.deps:
                            if d.dma:
                                s, v = ksem[d.slot], d.dval
                            elif d.sig:
                                s, v = esem[d.eng], d.sigval
                            else:
                                continue
                            if waited.get(id(s), 0) >= v:
                                continue
                            waited[id(s)] = v
                            eng.wait_ge(s, v)
                        if o.fn is None:
                            continue
                        ins = o.fn(eng)
                        if o.dma:
                            ins.then_inc(ksem[o.slot], 16)
                        elif o.sig:
                            ins.then_inc(esem[e], 1)
                    if e == "sync":
                        for i in range(nslots):
                            v = 16 * self.slot_count[i]
                            if waited.get(id(ksem[i]), 0) < v:
                                eng.wait_ge(ksem[i], v)
                return _f

            block.sync(body("sync"))
            block.tensor(body("tensor"))
            block.vector(body("vector"))
            block.scalar(body("scalar"))
            block.gpsimd(body("gpsimd"))


def _isz(dt):
    return {F32: 4, BF16: 2, I32: 4, U8: 1}[dt]


class Arena:
    def __init__(self, ap, size):
        self.ap = ap
        self.size = size
        self.off = 0

    def alloc(self, shape, dt):
        n = int(np.prod(shape))
        nb = n * _isz(dt)
        off = self.off
        self.off += (nb + 63) // 64 * 64
        assert self.off <= self.size, ("arena overflow", self.off, self.size)
        v = self.ap[:, off:off + nb].bitcast(dt)
        if len(shape) == 2:
            v = v.rearrange("p (a b) -> p a b", a=shape[0], b=shape[1])
        elif len(shape) == 3:
            v = v.rearrange("p (a b c) -> p a b c", a=shape[0], b=shape[1], c=shape[2])
        elif len(shape) == 4:
            v = v.rearrange("p (a b c d) -> p a b c d", a=shape[0], b=shape[1], c=shape[2], d=shape[3])
        return v

    def mark(self):
        return self.off

    def reset(self, m):
        self.off = m


class K:
    def __init__(self, nc, P):
        self.nc = nc
        self.P = P
        self.ev = 0

    def mm(self, out, lhsT, rhs, start, stop, reads, writes):
        self.P.op("tensor", lambda e: e.matmul(out, lhsT, rhs, start=start, stop=stop),
                  reads=reads, writes=writes, partial=not start)

    def mmi(self, groups):
        n = max(len(g) for g in groups)
        for i in range(n):
            for g in groups:
                if i < len(g):
                    out, lhsT, rhs, reads, writes = g[i]
                    self.mm(out, lhsT, rhs, i == 0, i == len(g) - 1, reads, writes)

    def tri(self, groups):
        n = max(len(g) for g in groups)
        for i in range(n):
            for g in groups:
                if i < len(g):
                    out, in_, ident, reads, writes = g[i]
                    self.tr(out, in_, ident, reads, writes, partial=(i > 0))

    def tr(self, out, in_, ident, reads, writes, partial=True):
        self.P.op("tensor", lambda e: e.transpose(out, in_, ident), reads=reads, writes=writes,
                  partial=partial)

    def act(self, out, in_, func, reads, writes, bias=None, scale=None, partial=False):
        def f(e):
            kw = {}
            if bias is not None:
                kw["bias"] = bias
            if scale is not None:
                kw["scale"] = scale
            return e.activation(out, in_, func, **kw)
        self.P.op("scalar", f, reads=reads, writes=writes, partial=partial)

    def evac(self, out, in_, reads, writes, eng=None, partial=False):
        if eng is None:
            self.ev += 1
            eng = "vector" if self.ev % 2 else "scalar"
        if eng == "vector":
            self.P.op("vector", lambda e: e.tensor_copy(out, in_), reads=reads, writes=writes, partial=partial)
        else:
            self.P.op("scalar", lambda e: e.activation(out, in_, AF.Copy), reads=reads, writes=writes,
                      partial=partial)

    def v(self, eng, name, args, reads, writes, kw=None, partial=False):
        kw = kw or {}
        self.P.op(eng, lambda e: getattr(e, name)(*args, **kw), reads=reads, writes=writes, partial=partial)

    def dma(self, q, out, in_, reads, writes, key=None, partial=False):
        self.P.op(q, lambda e: e.dma_start(out=out, in_=in_), reads=reads, writes=writes, dma=True,
                  key=key, partial=partial)


def build_program(debug=False):
    nc = bass.Bass("TRN2", target_bir_lowering=False)
    _bc = {}

    def _bc_reg(e):
        if "r" not in _bc:
            _bc["r"] = e.to_reg(NSLOT - 1)
        return _bc["r"]
    dr = {}

    def din(name, shape, dt=F32):
        dr[name] = nc.dram_tensor(name, list(shape), dt, kind="ExternalInput").ap()

    def dscr(name, shape, dt):
        dr[name] = nc.dram_tensor(name, list(shape), dt, kind="Internal").ap()

    din("xf", [8192, D]); din("xo", [4608, D]); din("mem", [256, D])
    din("w_in", [D, 2560]); din("w_gate", [D, 3072]); din("bg", [128, 24])
    din("w_kv", [D, 1024]); din("w_fo", [512, D]); din("w_nao", [512, D]); din("w_memo", [512, D])
    din("w_out", [D, D]); din("ln1_g", [1, D]); din("ln1_b", [1, D]); din("ln2_g", [1, D]); din("ln2_b", [1, D])
    din("wr", [D, 72]); din("br", [1, 72])
    din("w_eg", [NEXP, D, 512]); din("w_eu", [NEXP, D, 512]); din("w_ed", [NEXP, 512, D])
    din("biasT", [5, 8, 128, 768])
    din("c_identf", [128, 128]); din("c_identb", [128, 128], BF16); din("c_ones", [128, 128], BF16)
    din("c_ustrict", [128, 128], BF16); din("c_edft", [64, 128, 256], BF16)
    din("c_fca", [128, 256], BF16); din("c_fcb", [128, 256], BF16)
    din("c_bdc", [128, 64], BF16); din("c_bds", [128, 64], BF16)
    din("c_ec", [128, 64]); din("c_mhalf", [128, 1]); din("c_onesrow", [1, 128])
    dr["out"] = nc.dram_tensor("out", [4096, D], F32, kind="ExternalOutput").ap()
    dscr("yfT_d", [128, 4, 4096], BF16); dscr("ynaT_d", [128, 4, 4096], BF16); dscr("ymemT_d", [128, 4, 4096], BF16)
    dscr("xT_d", [8, 128, 8, 512], BF16); dscr("qmT_d", [8, 128, 4, 512], BF16)
    dscr("h_d", [4096, D], F32); dscr("xg_d", [NSLOT, D], BF16); dscr("yb_d", [NSLOT, D], BF16)
    if debug:
        dr["dbg_h"] = nc.dram_tensor("dbg_h", [4096, D], F32, kind="ExternalOutput").ap()
        dr["dbg_y3"] = nc.dram_tensor("dbg_y3", [3, 128, 4, 4096], BF16, kind="ExternalOutput").ap()
        dr["dbg_rt"] = nc.dram_tensor("dbg_rt", [128, 32, 4], F32, kind="ExternalOutput").ap()

    P = Prog(nc)
    k = K(nc, P)
    with contextlib.ExitStack() as st:
        arena_t = st.enter_context(nc.sbuf_tensor("arena", [128, ARENA], U8))
        A = Arena(arena_t, ARENA)
        ps = [st.enter_context(nc.psum_tensor("ps%d" % i, [128, 512], F32)) for i in range(8)]
        psb = [p[:].bitcast(BF16) for p in ps]

        identf = A.alloc([128], F32); identb = A.alloc([128], BF16); ones_b = A.alloc([128], BF16)
        ustrict = A.alloc([128], BF16)
        slots_i = A.alloc([32, 2], I32); rw = A.alloc([32, 2], F32)
        k.dma("sync", identf, dr["c_identf"], [], ["identf"])
        k.dma("sync", identb, dr["c_identb"], [], ["identb"])
        k.dma("sync", ones_b, dr["c_ones"], [], ["ones_b"])
        k.dma("sync", ustrict, dr["c_ustrict"], [], ["ustrict"])
        base_mark = A.mark()

        wf = A.alloc([8, 512], BF16)
        fca = A.alloc([256], BF16); fcb = A.alloc([256], BF16); bdc = A.alloc([64], BF16); bds = A.alloc([64], BF16)
        AT = [A.alloc([2, 64, 64, 2], BF16) for _ in range(4)]
        xt = [A.alloc([1024], F32) for _ in range(4)]
        xT = [A.alloc([8, 128], BF16) for _ in range(2)]
        ub = [A.alloc([512], BF16) for _ in range(2)]
        Ej = [A.alloc([256], BF16) for _ in range(4)]
        Bt = [A.alloc([256], BF16) for _ in range(4)]
        yfs = [A.alloc([4096], BF16) for _ in range(2)]
        k.dma("gpsimd", wf, dr["w_in"][:, 0:512].rearrange("(c p) n -> p c n", p=128), [], ["wf"])
        k.dma("sync", fca, dr["c_fca"], [], ["fca"]); k.dma("sync", fcb, dr["c_fcb"], [], ["fcb"])
        k.dma("sync", bdc, dr["c_bdc"], [], ["bdc"]); k.dma("sync", bds, dr["c_bds"], [], ["bds"])
        xf_v = dr["xf"].rearrange("(r c) f -> c r f", c=64)

        def a_load(j):
            k.dma("sync", xt[j % 4], xf_v[j], [], [("xt", j % 4)])
            k.dma("sync", Ej[j % 4], dr["c_edft"][j], [], [("Ej", j % 4)])
        a_load(0); a_load(1)
        for j0 in range(0, 64, 2):
            for j in (j0, j0 + 1):
                if j + 2 < 64:
                    a_load(j + 2)
            tg = []
            for j in (j0, j0 + 1):
                for hb in range(2):
                    bank = 2 * (j % 2) + hb
                    tg.append([(ps[bank][:, q * 128:(q + 1) * 128], xt[j % 4][:, (hb * 4 + q) * 128:(hb * 4 + q + 1) * 128],
                                identf, [("xt", j % 4), "identf"], [("ps", bank)]) for q in range(4)])
            k.tri(tg)
            for j in (j0, j0 + 1):
                for hb in range(2):
                    bank = 2 * (j % 2) + hb
                    k.evac(xT[j % 2][:, hb * 4:(hb + 1) * 4, :], ps[bank][:].rearrange("p (a b) -> p a b", a=4),
                           [("ps", bank)], [("xT", j % 2, hb)])
            k.mmi([[(ps[4 + (j % 2)][:], xT[j % 2][:, fc, :], wf[:, fc, :],
                     [("xT", j % 2, 0), ("xT", j % 2, 1), "wf"], [("ps", 4 + (j % 2))]) for fc in range(8)]
                   for j in (j0, j0 + 1)])
            for j in (j0, j0 + 1):
                k.evac(ub[j % 2], ps[4 + (j % 2)][:], [("ps", 4 + (j % 2))], [("ub", j % 2)])
            for j in (j0, j0 + 1):
                for g in range(4):
                    pa = 6 + (g % 2)
                    k.mm(ps[pa][:, 0:256], ub[j % 2][:, g * 128:(g + 1) * 128], Ej[j % 4], True, True,
                         [("ub", j % 2), ("Ej", j % 4)], [("ps", pa)])
                    k.evac(AT[g][:, :, :, j, :], ps[pa][:, 0:256].rearrange("p (r k a) -> p r k a", r=2, k=64, a=2),
                           [("ps", pa)], [("AT", g)], partial=True)
        it = 0
        for g in range(4):
            ys = yfs[g % 2]
            ysv = ys.rearrange("p (k q) -> p k q", k=32)
            for kk0 in range(0, 64, 2):
                kks = (kk0, kk0 + 1)
                k.mmi([[(ps[kk % 2][:, 0:256], AT[g][:, 0, kk, :, :].rearrange("p s a -> p (s a)"), fca,
                         [("AT", g), "fca"], [("ps", kk % 2)]),
                        (ps[kk % 2][:, 0:256], AT[g][:, 1, kk, :, :].rearrange("p s a -> p (s a)"), fcb,
                         [("AT", g), "fcb"], [("ps", kk % 2)])] for kk in kks])
                bts = {}
                for kk in kks:
                    bts[kk] = it % 4
                    k.evac(Bt[it % 4], ps[kk % 2][:, 0:256], [("ps", kk % 2)], [("Bt", it % 4)])
                    it += 1
                k.mmi([[(ps[2 + kk % 2][:, 0:64], Bt[bts[kk]][:, 0:128], bdc, [("Bt", bts[kk]), "bdc"], [("ps", 2 + kk % 2)]),
                        (ps[2 + kk % 2][:, 0:64], Bt[bts[kk]][:, 128:256], bds, [("Bt", bts[kk]), "bds"], [("ps", 2 + kk % 2)])]
                       for kk in kks])
                for kk in kks:
                    k.evac(ysv[:, :, 2 * kk:2 * kk + 2].rearrange("p k a -> p a k"),
                           ps[2 + kk % 2][:, 0:64].rearrange("p (a k) -> p a k", a=2),
                           [("ps", 2 + kk % 2)], [("yfs", g % 2)], partial=True)
            k.dma("sync", dr["yfT_d"][:, g, :], ys, [("yfs", g % 2)], [("yfT_d", g)])
        P.barrier()
        A.reset(base_mark)

        kT = A.alloc([4, 4608], BF16)
        Vext = A.alloc([36, 8, 65], BF16)
        qT = A.alloc([4, 4096], BF16)
        b1_mark = A.mark()
        W = A.alloc([8, 2048], BF16)
        xt = [A.alloc([1024], F32) for _ in range(4)]
        xTq = [A.alloc([8, 512], BF16) for _ in range(2)]
        qmq = [A.alloc([4, 512], BF16) for _ in range(2)]
        for cb in range(4):
            k.dma("gpsimd", W[:, :, cb * 512:(cb + 1) * 512],
                  dr["w_in"][:, 512 + cb * 512:512 + (cb + 1) * 512].rearrange("(c p) n -> p c n", p=128),
                  [], [("W", cb)])
        Wr = [("W", cb) for cb in range(4)]
        k.v("vector", "memset", (Vext[:, :, :, 64:65], 1.0), [], ["Vext1"])
        xo_t = dr["xo"].rearrange("(t p) f -> t p f", p=128)

        def b_load(t):
            k.dma("sync", xt[t % 4], xo_t[t], [], [("xt", t % 4)])
        b_load(0); b_load(1)
        for Q in range(9):
            xq = xTq[Q % 2]
            for tt0 in range(0, 4, 2):
                tts = (tt0, tt0 + 1)
                for tt in tts:
                    t = 4 * Q + tt
                    if t + 2 < 36:
                        b_load(t + 2)
                tg = []
                for tt in tts:
                    t = 4 * Q + tt
                    for hb in range(2):
                        bank = 2 * (t % 2) + hb
                        tg.append([(ps[bank][:, q * 128:(q + 1) * 128],
                                    xt[t % 4][:, (hb * 4 + q) * 128:(hb * 4 + q + 1) * 128], identf,
                                    [("xt", t % 4), "identf"], [("ps", bank)]) for q in range(4)])
                k.tri(tg)
                for tt in tts:
                    t = 4 * Q + tt
                    for hb in range(2):
                        bank = 2 * (t % 2) + hb
                        k.evac(xq[:, hb * 4:(hb + 1) * 4, tt * 128:(tt + 1) * 128],
                               ps[bank][:].rearrange("p (a b) -> p a b", a=4),
                               [("ps", bank)], [("xTq", Q % 2)], partial=True)
            xr = [("xTq", Q % 2)] + Wr

            def proj_pair(col0, cs, banks):
                k.mmi([[(ps[banks[i]][:], W[:, fc, col0 + c * 128:col0 + (c + 1) * 128], xq[:, fc, :], xr,
                         [("ps", banks[i])]) for fc in range(8)] for i, c in enumerate(cs)])
            for cs, banks in (((0, 1), (4, 5)), ((2, 3), (6, 7))):
                proj_pair(512, cs, banks)
                for i, c in enumerate(cs):
                    k.evac(kT[:, c, Q * 512:(Q + 1) * 512], ps[banks[i]][:], [("ps", banks[i])], [("kT", Q)],
                           partial=True)
            if Q < 8:
                for cs, banks in (((0, 1), (4, 5)), ((2, 3), (6, 7))):
                    proj_pair(0, cs, banks)
                    for i, c in enumerate(cs):
                        k.act(qT[:, c, Q * 512:(Q + 1) * 512], ps[banks[i]][:], AF.Copy, [("ps", banks[i])],
                              [("qT", Q)], scale=0.125, partial=True)
                for cs, banks in (((0, 1), (4, 5)), ((2, 3), (6, 7))):
                    proj_pair(1536, cs, banks)
                    for i, c in enumerate(cs):
                        k.evac(qmq[Q % 2][:, c, :], ps[banks[i]][:], [("ps", banks[i])], [("qmq", Q % 2)],
                               partial=True)
                k.dma("sync", dr["qmT_d"][Q], qmq[Q % 2], [("qmq", Q % 2)], [("qmT_d", Q)])
                k.dma("sync", dr["xT_d"][Q], xq, [("xTq", Q % 2)], [("xT_d", Q)])
            for tts, banks in (((0, 1), (4, 5)), ((2, 3), (6, 7))):
                k.mmi([[(ps[banks[i]][:], xq[:, fc, tt * 128:(tt + 1) * 128], W[:, fc, 1024:1536], xr,
                         [("ps", banks[i])]) for fc in range(8)] for i, tt in enumerate(tts)])
                for i, tt in enumerate(tts):
                    t = 4 * Q + tt
                    k.evac(Vext[:, t, :, 0:64], ps[banks[i]][:].rearrange("p (h d) -> p h d", h=8),
                           [("ps", banks[i])], [("Vext", t)])
        P.barrier()
        A.reset(b1_mark)

        bias_i = A.alloc([8, 768], BF16)
        bias_s = [A.alloc([8, 768], BF16) for _ in range(2)]
        eS = [A.alloc([768], BF16) for _ in range(3)]
        yna = [A.alloc([512], BF16) for _ in range(2)]
        rc = [A.alloc([8], F32) for _ in range(2)]
        ynaTq = [A.alloc([4, 512], BF16) for _ in range(2)]
        for hh in range(8):
            k.dma("gpsimd", bias_i[:, hh, :], dr["biasT"][0, hh], [], [("bias_i", hh)])
        special = {0: 1, 1: 2, 30: 3, 31: 4}

        def tile_of_pair(lp):
            if 0 <= lp < 32:
                return lp
            return {-2: 32, -1: 33, 32: 34, 33: 35}[lp]
        nsp = 0

        def offs_of(qp):
            if qp == 0:
                return [-2, -1, 0, 1, 2, 3]
            if qp == 31:
                return [-3, -2, -1, 0, 1, 2]
            return [-2, -1, 0, 1, 2]
        bias_of = {}

        def na_finalize(qp):
            yq = yna[qp % 2]
            rcq = rc[qp % 2]
            for h2 in range(8):
                po2 = 4 + (qp % 2) * 2 + (h2 // 4)
                oc2 = (h2 % 4) * 65
                k.v("vector", "reciprocal", (rcq[:, h2:h2 + 1], ps[po2][:, oc2 + 64:oc2 + 65]),
                    [("ps", po2, h2 % 4)], [("rc", qp % 2, h2)])
                k.v("vector", "tensor_scalar", (yq[:, h2 * 64:(h2 + 1) * 64], ps[po2][:, oc2:oc2 + 64],
                                                 rcq[:, h2:h2 + 1], None, ALU.mult),
                    [("ps", po2, h2 % 4), ("rc", qp % 2, h2)], [("yna", qp % 2)], partial=True)

        def na_transposes(qp):
            yq = yna[qp % 2]
            Q = qp // 4
            pT = 4 + (qp % 2) * 2 + 1
            tv = psb[pT][:, 640:1024]
            pT2 = 4 + (qp % 2) * 2
            tv2 = psb[pT2][:, 640:768]
            k.tr(tv[:, 0:128], yq[:, 0:128], identb, [("yna", qp % 2), "identb"], [("ps", pT, "T")], partial=False)
            k.tr(tv2, yq[:, 384:512], identb, [("yna", qp % 2), "identb"], [("ps", pT2, "T")], partial=False)
            for cc in range(1, 3):
                k.tr(tv[:, cc * 128:(cc + 1) * 128], yq[:, cc * 128:(cc + 1) * 128], identb,
                     [("yna", qp % 2), "identb"], [("ps", pT, "T")], partial=True)
            yt = ynaTq[Q % 2]
            k.evac(yt[:, 0:3, (qp % 4) * 128:(qp % 4 + 1) * 128], tv.rearrange("p (a b) -> p a b", a=3),
                   [("ps", pT, "T")], [("ynaTq", Q % 2)], partial=True)
            k.evac(yt[:, 3, (qp % 4) * 128:(qp % 4 + 1) * 128], tv2, [("ps", pT2, "T")], [("ynaTq", Q % 2)],
                   partial=True)
            if qp % 4 == 3:
                k.dma("sync", dr["ynaT_d"][:, :, Q * 512:(Q + 1) * 512], yt, [("ynaTq", Q % 2)], [("ynaT_d", Q)])

        def pv_list(n):
            qp, h = n // 8, n % 8
            offs = offs_of(qp); ns = len(offs)
            es = eS[n % 3]; en = ("eS", n % 3)
            po = 4 + (qp % 2) * 2 + (h // 4)
            oc = (h % 4) * 65
            out = []
            for j, off in enumerate(offs):
                kt = tile_of_pair(qp + off)
                out.append((ps[po][:, oc:oc + 65], es[:, j * 128:(j + 1) * 128], Vext[:, kt, h, :], j == 0, j == ns - 1,
                            [en, ("Vext", kt), "Vext1"], [("ps", po, h % 4)]))
            return out
        NH = 256
        for n in range(NH + 1):
            qk = []
            if n < NH:
                qp, h = n // 8, n % 8
                offs = offs_of(qp); ns = len(offs)
                if h == 0:
                    if qp in special:
                        bt_ = bias_s[nsp % 2]; bname = ("bias_s", nsp % 2)
                        for hh in range(8):
                            k.dma("gpsimd", bt_[:, hh, :], dr["biasT"][special[qp], hh], [], [bname], key=bname,
                                  partial=(hh > 0))
                        bias_of[qp] = (bt_, [bname])
                        nsp += 1
                    else:
                        bias_of[qp] = (bias_i, None)
                bt_, breads = bias_of[qp]
                c = h // 2; pb0 = 64 * (h % 2)
                sX = (n % 2) * 2; sY = sX + 1
                br_ = breads if breads is not None else [("bias_i", h)]
                k.mm(ps[sX][:], identb, bt_[:, h, 0:512], True, False, br_ + ["identb"], [("ps", sX)])
                k.mm(ps[sY][:, 0:(ns - 4) * 128], identb, bt_[:, h, 512:512 + (ns - 4) * 128], True, False,
                     br_ + ["identb"], [("ps", sY)])
                for j, off in enumerate(offs):
                    kt = tile_of_pair(qp + off)
                    bank = sX if j < 4 else sY
                    last = (j == 3) or (j == ns - 1)
                    qk.append((ps[bank][:, (j % 4) * 128:(j % 4 + 1) * 128],
                               kT[pb0:pb0 + 64, c, kt * 128:(kt + 1) * 128],
                               qT[pb0:pb0 + 64, c, qp * 128:(qp + 1) * 128], False, last,
                               [("kT", kt // 4), ("qT", qp // 4)], [("ps", bank)]))
            pv = pv_list(n - 1) if n >= 1 else []
            for i in range(max(len(qk), len(pv))):
                if i < len(qk):
                    k.mm(*qk[i])
                if i < len(pv):
                    k.mm(*pv[i])
            if n < NH:
                es = eS[n % 3]; en = ("eS", n % 3)
                k.act(es[:, 0:512], ps[sX][:], AF.Exp, [("ps", sX)], [en])
                k.act(es[:, 512:ns * 128], ps[sY][:, 0:(ns - 4) * 128], AF.Exp, [("ps", sY)], [en], partial=True)
            if n >= 1 and (n - 1) % 8 == 7:
                na_finalize((n - 1) // 8)
            if n >= 1 and (n - 1) % 8 == 3 and (n - 1) // 8 >= 1:
                na_transposes((n - 1) // 8 - 1)
        na_transposes(31)
        P.barrier()
        A.reset(base_mark)

        wkv = A.alloc([8, 1024], BF16)
        memt = [A.alloc([1024], F32) for _ in range(2)]
        memT = A.alloc([8, 256], BF16)
        kmT = A.alloc([4, 256], BF16)
        Vm = A.alloc([2, 4, 129], BF16)
        qmq = [A.alloc([4, 512], BF16) for _ in range(2)]
        eSm = [A.alloc([512], BF16) for _ in range(4)]
        ym = [A.alloc([512], BF16) for _ in range(8)]
        rcm = [A.alloc([4], F32) for _ in range(8)]
        ymTq = [A.alloc([4, 512], BF16) for _ in range(2)]
        for cb in range(2):
            k.dma("gpsimd", wkv[:, :, cb * 512:(cb + 1) * 512],
                  dr["w_kv"][:, cb * 512:(cb + 1) * 512].rearrange("(c p) n -> p c n", p=128), [], [("wkv", cb)])
        k.v("vector", "memset", (Vm[:, :, :, 128:129], 1.0), [], ["Vm1"])
        for mc in range(2):
            k.dma("sync", memt[mc], dr["mem"][mc * 128:(mc + 1) * 128, :], [], [("memt", mc)])
            for hb in range(2):
                bank = 2 * mc + hb
                for q in range(4):
                    fc = hb * 4 + q
                    k.tr(ps[bank][:, q * 128:(q + 1) * 128], memt[mc][:, fc * 128:(fc + 1) * 128], identf,
                         [("memt", mc), "identf"], [("ps", bank)], partial=(q > 0))
                k.evac(memT[:, hb * 4:(hb + 1) * 4, mc * 128:(mc + 1) * 128],
                       ps[bank][:].rearrange("p (a b) -> p a b", a=4), [("ps", bank)], ["memT"], partial=True)
        for h in range(4):
            pb_ = 4 + h % 2
            for fc in range(8):
                k.mm(ps[pb_][:, 0:256], wkv[:, fc, h * 128:(h + 1) * 128], memT[:, fc, :], fc == 0, fc == 7,
                     ["memT", ("wkv", 0)], [("ps", pb_)])
            k.evac(kmT[:, h, :], ps[pb_][:, 0:256], [("ps", pb_)], ["kmT"], partial=True)
        for mc in range(2):
            pb_ = 6 + mc
            for fc in range(8):
                k.mm(ps[pb_][:], memT[:, fc, mc * 128:(mc + 1) * 128], wkv[:, fc, 512:1024], fc == 0, fc == 7,
                     ["memT", ("wkv", 1)], [("ps", pb_)])
            k.evac(Vm[:, mc, :, 0:128], ps[pb_][:].rearrange("p (h d) -> p h d", h=4), [("ps", pb_)], ["Vm"],
                   partial=True)
        msc = 128.0 ** -0.5
        k.dma("sync", qmq[0], dr["qmT_d"][0], [], [("qmq", 0)])
        ecnt = 0
        for Q in range(8):
            if Q + 1 < 8:
                k.dma("sync", qmq[(Q + 1) % 2], dr["qmT_d"][Q + 1], [], [("qmq", (Q + 1) % 2)])
            qq = qmq[Q % 2]
            for h in range(4):
                ebufs = []
                for mc in range(2):
                    bank = (ecnt % 2) * 2 + mc
                    k.mm(ps[bank][:], kmT[:, h, mc * 128:(mc + 1) * 128], qq[:, h, :], True, True,
                         ["kmT", ("qmq", Q % 2)], [("ps", bank)])
                    eb = (ecnt % 2) * 2 + mc
                    k.act(eSm[eb], ps[bank][:], AF.Exp, [("ps", bank)], [("eSm", eb)], scale=msc)
                    ebufs.append(eb)
                for tts in ((0, 2), (1, 3)):
                    k.mmi([[(ps[4 + (tt // 2) + 2 * (ecnt % 2)][:, (tt % 2) * 129:(tt % 2) * 129 + 129],
                             eSm[ebufs[mc]][:, tt * 128:(tt + 1) * 128], Vm[:, mc, h, :],
                             [("eSm", ebufs[mc]), "Vm", "Vm1"], [("ps", 4 + (tt // 2) + 2 * (ecnt % 2), tt % 2)])
                            for mc in range(2)] for tt in tts])
                for tt in range(4):
                    po = 4 + (tt // 2) + 2 * (ecnt % 2)
                    oc = (tt % 2) * 129
                    yb_ = (Q % 2) * 4 + tt
                    k.v("vector", "reciprocal", (rcm[yb_][:, h:h + 1], ps[po][:, oc + 128:oc + 129]),
                        [("ps", po, tt % 2)], [("rcm", yb_, h)])
                    k.v("vector", "tensor_scalar", (ym[yb_][:, h * 128:(h + 1) * 128], ps[po][:, oc:oc + 128],
                                                     rcm[yb_][:, h:h + 1], None, ALU.mult),
                        [("ps", po, tt % 2), ("rcm", yb_, h)], [("ym", yb_)], partial=True)
                ecnt += 1
            yt = ymTq[Q % 2]
            for tt in range(4):
                yb_ = (Q % 2) * 4 + tt
                bank = (tt % 2) * 2 + (ecnt % 2)
                tvm = psb[bank][:, 0:512]
                for cc in range(4):
                    k.tr(tvm[:, cc * 128:(cc + 1) * 128], ym[yb_][:, cc * 128:(cc + 1) * 128], identb,
                         [("ym", yb_), "identb"], [("ps", bank)], partial=(cc > 0))
                k.evac(yt[:, :, tt * 128:(tt + 1) * 128], tvm.rearrange("p (a b) -> p a b", a=4), [("ps", bank)],
                       [("ymTq", Q % 2)], partial=True)
            k.dma("sync", dr["ymemT_d"][:, :, Q * 512:(Q + 1) * 512], yt, [("ymTq", Q % 2)], [("ymemT_d", Q)])
        P.barrier()
        A.reset(base_mark)

        wg = A.alloc([8, 3072], BF16)
        wo3 = [A.alloc([4, 1024], BF16) for _ in range(3)]
        wout = A.alloc([8, 1024], BF16)
        wrt = A.alloc([8, 72], F32)
        bgt = A.alloc([24], F32)
        brt = A.alloc([72], F32); onesrow = A.alloc([128], F32)
        lng = A.alloc([1024], F32); lnb = A.alloc([1024], F32)
        ect = A.alloc([64], F32); mhalf = A.alloc([1], F32)
        atot = A.alloc([64], BF16)
        xTq = [A.alloc([8, 512], BF16) for _ in range(2)]
        ybq = [[A.alloc([4, 512], BF16) for _ in range(2)] for _ in range(3)]
        G = [[A.alloc([512], BF16) for _ in range(2)] for _ in range(3)]
        tm = [A.alloc([512], F32) for _ in range(4)]
        mT = A.alloc([8, 512], BF16)
        xt = [A.alloc([1024], F32) for _ in range(2)]
        rt = [A.alloc([1024], F32) for _ in range(2)]
        ht = [A.alloc([1024], F32) for _ in range(2)]
        hl = [A.alloc([1024], F32) for _ in range(2)]
        hb = [A.alloc([1024], BF16) for _ in range(2)]
        hT = A.alloc([8, 128], F32)
        sm = A.alloc([256], F32)
        sml = [A.alloc([16], F32) for _ in range(2)]
        k.A1 = A.alloc([64], F32); k.A2 = A.alloc([64], F32); k.Ab = A.alloc([64], BF16)
        for cb in range(6):
            k.dma("gpsimd", wg[:, :, cb * 512:(cb + 1) * 512],
                  dr["w_gate"][:, cb * 512:(cb + 1) * 512].rearrange("(c p) n -> p c n", p=128), [], [("wg", cb)])
        for i, nm in enumerate(("w_fo", "w_nao", "w_memo")):
            k.dma("gpsimd", wo3[i], dr[nm].rearrange("(c p) n -> p c n", p=128), [], [("wo3", i)])
        for cb in range(2):
            k.dma("gpsimd", wout[:, :, cb * 512:(cb + 1) * 512],
                  dr["w_out"][:, cb * 512:(cb + 1) * 512].rearrange("(c p) n -> p c n", p=128), [], [("wout", cb)])
        k.dma("sync", wrt, dr["wr"].rearrange("(c p) n -> p c n", p=128), [], ["wrt"])
        k.dma("sync", bgt, dr["bg"], [], ["bgt"])
        k.dma("sync", brt[0:1, :], dr["br"], [], ["brt"])
        k.dma("sync", onesrow[0:1, :], dr["c_onesrow"], [], ["onesrow"])
        k.dma("sync", lng, dr["ln1_g"].partition_broadcast(128)[:, 0, :], [], ["lng"])
        k.dma("sync", lnb, dr["ln1_b"].partition_broadcast(128)[:, 0, :], [], ["lnb"])
        k.dma("sync", ect, dr["c_ec"], [], ["ect"])
        k.dma("sync", mhalf, dr["c_mhalf"], [], ["mhalf"])
        k.v("vector", "memset", (atot, 0.0), [], ["atot"])
        ysrc = ("yfT_d", "ynaT_d", "ymemT_d")

        def c_load(Q):
            k.dma("sync", xTq[Q % 2], dr["xT_d"][Q], [], [("xTq", Q % 2)])
            for i in range(3):
                k.dma("sync", ybq[i][Q % 2], dr[ysrc[i]][:, :, Q * 512:(Q + 1) * 512], [], [("ybq", i, Q % 2)])
        c_load(0)
        wgr = [("wg", cb) for cb in range(6)]

        def r1(ti):
            b2 = ti % 2
            k.dma("sync", hl[b2], dr["h_d"][ti * 128:(ti + 1) * 128, :], [("h_d", ti)], [("hl", b2)])
            k.act(hb[b2], hl[b2], AF.Copy, [("hl", b2)], [("hb", b2)])
            for hh in range(2):
                for q in range(4):
                    k.tr(ps[6][:, q * 128:(q + 1) * 128], hl[b2][:, (hh * 4 + q) * 128:(hh * 4 + q + 1) * 128], identf,
                         [("hl", b2), "identf"], [("ps", 6)], partial=(q > 0))
                k.evac(hT[:, hh * 4:(hh + 1) * 4, :], ps[6][:].rearrange("p (a b) -> p a b", a=4),
                       [("ps", 6)], [("hT", hh)])

        def r2(ti):
            for fc in range(8):
                k.mm(ps[7][:, 0:72], hT[:, fc, :], wrt[:, fc, :], fc == 0, False,
                     [("hT", 0), ("hT", 1), "wrt"], [("ps", 7)])
            k.mm(ps[7][:, 0:72], onesrow[0:1, :], brt[0:1, :], False, True, ["onesrow", "brt"], [("ps", 7)])

        def r3(ti):
            emit_route(k, ps, sm, ti, slots_i, rw, atot, ustrict, ones_b, ect, 7, 1)

        def r4(ti):
            b2 = ti % 2
            emit_route(k, ps, sm, ti, slots_i, rw, atot, ustrict, ones_b, ect, 7, 2)
            for kk in range(2):
                P.op("gpsimd", (lambda e, ti=ti, kk=kk, b2=b2: e.indirect_dma_start(
                    out=dr["xg_d"][:, :],
                    out_offset=bass.IndirectOffsetOnAxis(ap=slots_i[:, ti, kk:kk + 1], axis=0),
                    in_=hb[b2][:, :], in_offset=None, bounds_check=_bc_reg(e), oob_is_err=False)),
                    reads=[("hb", b2), ("slots", ti)], writes=["xg_d"], dma=True, key=("xg_sc", b2), partial=True)

        def route_sched(Qp):
            t = [Qp * 4 + i for i in range(4)]
            return [[lambda: r1(t[0])], [lambda: r2(t[0]), lambda: r3(t[0])],
                    [lambda: r4(t[0]), lambda: r1(t[1])], [lambda: r2(t[1]), lambda: r3(t[1])],
                    [lambda: r4(t[1]), lambda: r1(t[2])], [lambda: r2(t[2]), lambda: r3(t[2])],
                    [lambda: r4(t[2]), lambda: r1(t[3])], [lambda: r2(t[3]), lambda: r3(t[3])],
                    [lambda: r4(t[3])]]
        for Q in range(9):
            sched = route_sched(Q - 1) if Q >= 1 else [[] for _ in range(9)]
            if Q < 8:
                if Q + 1 < 8:
                    c_load(Q + 1)
                xq = xTq[Q % 2]
            for dc in range(8):
                if Q < 8:
                    k.mmi([[(ps[i][:], wg[:, fc, i * 1024 + dc * 128:i * 1024 + (dc + 1) * 128], xq[:, fc, :],
                             wgr + [("xTq", Q % 2)], [("ps", i)]) for fc in range(8)] for i in range(3)])
                    for i in range(3):
                        k.act(G[i][dc % 2], ps[i][:], AF.Sigmoid, [("ps", i), "bgt"], [("G", i, dc % 2)],
                              bias=bgt[:, i * 8 + dc:i * 8 + dc + 1])
                    k.mmi([[(ps[3 + i][:], wo3[i][:, cc, dc * 128:(dc + 1) * 128], ybq[i][Q % 2][:, cc, :],
                             [("wo3", i), ("ybq", i, Q % 2)], [("ps", 3 + i)]) for cc in range(4)] for i in range(3)])
                for f_ in sched[dc]:
                    f_()
                if Q < 8:
                    for i in range(3):
                        k.v("vector", "tensor_tensor", (tm[i], G[i][dc % 2], ps[3 + i][:], ALU.mult),
                            [("G", i, dc % 2), ("ps", 3 + i)], [("tm", i)])
                    k.v("gpsimd", "tensor_tensor", (tm[3], tm[0], tm[1], ALU.add), [("tm", 0), ("tm", 1)], [("tm", 3)])
                    k.v("gpsimd", "tensor_tensor", (mT[:, dc, :], tm[3], tm[2], ALU.add), [("tm", 3), ("tm", 2)],
                        [("mT", dc)])
            if Q == 8:
                for f_ in sched[8]:
                    f_()
                break
            mTr = [("mT", dc) for dc in range(8)]
            k.dma("sync", xt[0], xo_t[Q * 4], [], [("xt", 0)])

            def s1a(tt):
                ti = Q * 4 + tt
                b2 = ti % 2
                if tt + 1 < 4:
                    k.dma("sync", xt[(tt + 1) % 2], xo_t[ti + 1], [], [("xt", (tt + 1) % 2)])
                pbk = (6, 7) if tt % 2 == 0 else (4, 5)
                k.mmi([[(ps[pbk[half]][:], mT[:, dc, tt * 128:(tt + 1) * 128], wout[:, dc, half * 512:(half + 1) * 512],
                         mTr + [("wout", half)], [("ps", pbk[half])]) for dc in range(8)] for half in range(2)])
                for half in range(2):
                    pm = pbk[half]
                    k.v("vector", "scalar_tensor_tensor",
                        (rt[b2][:, half * 512:(half + 1) * 512], xt[tt % 2][:, half * 512:(half + 1) * 512], ALPHA,
                         ps[pm][:], ALU.mult, ALU.add),
                        [("xt", tt % 2), ("ps", pm)], [("rt", b2, half)])
                emit_ln_a(k, rt[b2], [("rt", b2, 0), ("rt", b2, 1)], sml[b2], mhalf, tag=b2)

            def s1b(tt):
                ti = Q * 4 + tt
                b2 = ti % 2
                emit_ln_b(k, rt[b2], [("rt", b2, 0), ("rt", b2, 1)], ht[b2], ("ht", b2), lng, lnb, ["lng", "lnb"],
                          sml[b2], tag=b2)
                k.dma("sync", dr["h_d"][ti * 128:(ti + 1) * 128, :], ht[b2], [("ht", b2)], [("h_d", ti)])
            s1a(0)
            s1a(1)
            s1b(0)
            for f_ in sched[8]:
                f_()
            s1a(2)
            s1b(1)
            s1a(3)
            s1b(2)
            s1b(3)
        P.barrier()
        A.reset(base_mark)
        if debug:
            dbgt = A.alloc([32, 4], F32)
            k.v("vector", "tensor_copy", (dbgt[:, :, 0:2], slots_i), [], ["dbgt0"])
            k.v("vector", "tensor_copy", (dbgt[:, :, 2:4], rw), [], ["dbgt1"])
            k.dma("sync", dr["dbg_rt"], dbgt, ["dbgt0", "dbgt1"], ["dbg_rt"])
            k.dma("sync", dr["dbg_h"], dr["h_d"], [], ["dbg_h"])
            for i_, nm_ in enumerate(("yfT_d", "ynaT_d", "ymemT_d")):
                k.dma("sync", dr["dbg_y3"][i_], dr[nm_], [], [("dbg_y3", i_)])

        weg = [A.alloc([8, 512], BF16) for _ in range(3)]
        weu = [A.alloc([8, 512], BF16) for _ in range(3)]
        wed = [A.alloc([4, 1024], BF16) for _ in range(3)]
        xg = [A.alloc([2, 1024], BF16) for _ in range(3)]
        xgT = [A.alloc([8, 256], BF16) for _ in range(2)]
        sg = [A.alloc([256], F32) for _ in range(2)]
        aT = [A.alloc([4, 256], BF16) for _ in range(2)]
        yo = [A.alloc([1024], BF16) for _ in range(2)]

        def e_load(e_):
            s = e_ % 3
            k.dma("gpsimd", weg[s], dr["w_eg"][e_].rearrange("(c p) n -> p c n", p=128), [], [("weg", s)])
            k.dma("gpsimd", weu[s], dr["w_eu"][e_].rearrange("(c p) n -> p c n", p=128), [], [("weu", s)])
            k.dma("gpsimd", wed[s], dr["w_ed"][e_].rearrange("(c p) n -> p c n", p=128), [], [("wed", s)])
            k.dma("sync", xg[e_ % 3], dr["xg_d"][e_ * CAP:(e_ + 1) * CAP, :].rearrange("(t p) f -> p t f", p=128),
                  ["xg_d"], [("xg", e_ % 3)])
        e_load(0); e_load(1)

        def e_transposes(ee):
            xgt_ = xgT[ee % 2]
            k.tri([[(psb[tt][:, fc * 128:(fc + 1) * 128], xg[ee % 3][:, tt, fc * 128:(fc + 1) * 128], identb,
                     [("xg", ee % 3), "identb"], [("ps", tt)]) for fc in range(8)] for tt in range(2)])
            for tt in range(2):
                k.evac(xgt_[:, :, tt * 128:(tt + 1) * 128], psb[tt][:].rearrange("p (a b) -> p a b", a=8),
                       [("ps", tt)], [("xgT", ee % 2)], partial=True)
        yc = 0
        for e_ in range(NEXP):
            if e_ + 2 < NEXP:
                e_load(e_ + 2)
            s = e_ % 3
            xgt = xgT[e_ % 2]
            if e_ == 0:
                e_transposes(0)
            at = aT[e_ % 2]
            for dcn in range(4):
                pg = 2 + (dcn % 2) * 2; pu = pg + 1
                k.mmi([[(ps[pg][:, 0:256], weg[s][:, fc, dcn * 128:(dcn + 1) * 128], xgt[:, fc, :],
                         [("weg", s), ("xgT", e_ % 2)], [("ps", pg)]) for fc in range(8)],
                       [(ps[pu][:, 0:256], weu[s][:, fc, dcn * 128:(dcn + 1) * 128], xgt[:, fc, :],
                         [("weu", s), ("xgT", e_ % 2)], [("ps", pu)]) for fc in range(8)]])
                k.act(sg[dcn % 2], ps[pg][:, 0:256], AF.Silu, [("ps", pg)], [("sg", dcn % 2)])
                k.v("vector", "tensor_tensor", (at[:, dcn, :], sg[dcn % 2], ps[pu][:, 0:256], ALU.mult),
                    [("sg", dcn % 2), ("ps", pu)], [("aT", e_ % 2, dcn)])
            atr = [("aT", e_ % 2, dcn) for dcn in range(4)]
            if e_ + 1 < NEXP:
                e_transposes(e_ + 1)
            for tt in range(2):
                yt = yo[yc % 2]
                k.mmi([[(ps[6 + half][:], at[:, dcn, tt * 128:(tt + 1) * 128], wed[s][:, dcn, half * 512:(half + 1) * 512],
                         atr + [("wed", s)], [("ps", 6 + half)]) for dcn in range(4)] for half in range(2)])
                for half in range(2):
                    pd = 6 + half
                    k.evac(yt[:, half * 512:(half + 1) * 512], ps[pd][:], [("ps", pd)], [("yo", yc % 2, half)])
                k.dma("sync", dr["yb_d"][e_ * CAP + tt * 128:e_ * CAP + (tt + 1) * 128, :], yt,
                      [("yo", yc % 2, 0), ("yo", yc % 2, 1)], ["yb_d"], key=("yo_st", yc % 2), partial=True)
                yc += 1
        P.barrier()
        A.reset(base_mark)

        lng = A.alloc([1024], F32); lnb = A.alloc([1024], F32); mhalf = A.alloc([1], F32)
        sm = A.alloc([256], F32)
        NB = 4
        hd = [A.alloc([1024], F32) for _ in range(NB)]
        g1 = [A.alloc([1024], BF16) for _ in range(NB)]
        g2 = [A.alloc([1024], BF16) for _ in range(NB)]
        acc = [A.alloc([1024], F32) for _ in range(NB)]
        ot = [A.alloc([1024], F32) for _ in range(NB)]
        smd = [A.alloc([32], F32) for _ in range(NB)]
        k.dma("sync", lng, dr["ln2_g"].partition_broadcast(128)[:, 0, :], [], ["lng"])
        k.dma("sync", lnb, dr["ln2_b"].partition_broadcast(128)[:, 0, :], [], ["lnb"])
        k.dma("sync", mhalf, dr["c_mhalf"], [], ["mhalf"])

        def d_load(ti):
            b2 = ti % NB
            k.dma("sync", hd[b2], dr["h_d"][ti * 128:(ti + 1) * 128, :], [], [("hd", b2)])
            for kk, gt in enumerate((g1, g2)):
                P.op("gpsimd", (lambda e, ti=ti, kk=kk, gt=gt, b2=b2: e.indirect_dma_start(
                    out=gt[b2][:, :], out_offset=None, in_=dr["yb_d"][:, :],
                    in_offset=bass.IndirectOffsetOnAxis(ap=slots_i[:, ti, kk:kk + 1], axis=0),
                    bounds_check=_bc_reg(e), oob_is_err=False)),
                    reads=[], writes=[("g", kk, b2)], dma=True)
        d_load(0); d_load(1)

        def da(ti):
            if ti + 2 < 32:
                d_load(ti + 2)
            b2 = ti % NB
            k.act(acc[b2], hd[b2], AF.Copy, [("hd", b2)], [("acc", b2)], scale=ALPHA)
            k.v("vector", "scalar_tensor_tensor", (acc[b2], g1[b2], rw[:, ti, 0:1], acc[b2], ALU.mult, ALU.add),
                [("g", 0, b2), ("acc", b2)], [("acc", b2)])
            k.v("vector", "scalar_tensor_tensor", (acc[b2], g2[b2], rw[:, ti, 1:2], acc[b2], ALU.mult, ALU.add),
                [("g", 1, b2), ("acc", b2)], [("acc", b2)])
            emit_ln_a(k, acc[b2], [("acc", b2)], smd[b2], mhalf, tag=b2)

        def db(ti):
            b2 = ti % NB
            emit_ln_b(k, acc[b2], [("acc", b2)], ot[b2], ("ot", b2), lng, lnb, ["lng", "lnb"], smd[b2], tag=b2)
            k.dma("sync", dr["out"][ti * 128:(ti + 1) * 128, :], ot[b2], [("ot", b2)], [("out", ti)])
        da(0)
        for ti in range(32):
            if ti + 1 < 32:
                da(ti + 1)
            db(ti)
        P.emit()
    return nc


def emit_ln_a(k, src, src_names, sm, mhalf, tag=0):
    st = sm[:, 0:12].rearrange("p (a b) -> p a b", a=2)
    mv = sm[:, 12:14]
    rs = sm[:, 14:15]
    nb = sm[:, 15:16]
    T = lambda x: (x, tag)
    for half in range(2):
        k.v("vector", "bn_stats", (st[:, half, :], src[:, half * 512:(half + 1) * 512]), src_names,
            [T("lnst%d" % half)])
    k.v("vector", "bn_aggr", (mv, st), [T("lnst0"), T("lnst1")], [T("lnmv")])
    k.v("gpsimd", "tensor_scalar", (rs, mv[:, 1:2], LN_EPS, 1.0, ALU.add, ALU.mult), [T("lnmv")], [T("lnrs0")])
    k.v("gpsimd", "tensor_tensor", (rs, rs, mhalf, ALU.pow), [T("lnrs0"), "mhalf"], [T("lnrs")])
    k.v("vector", "scalar_tensor_tensor", (nb, mv[:, 0:1], -1.0, rs, ALU.mult, ALU.mult), [T("lnmv"), T("lnrs")],
        [T("lnnb")])


def emit_ln_b(k, src, src_names, dst, dst_name, g, b, gb_names, sm, tag=0):
    rs = sm[:, 14:15]
    nb = sm[:, 15:16]
    T = lambda x: (x, tag)
    k.act(dst, src, AF.Identity, src_names + [T("lnrs"), T("lnnb")], [dst_name + ("n",), dst_name], bias=nb, scale=rs)
    k.v("gpsimd", "tensor_tensor", (dst[:, 0:512], dst[:, 0:512], g[:, 0:512], ALU.mult),
        [dst_name + ("n",), gb_names[0]], [dst_name + ("g0",)])
    k.v("vector", "tensor_tensor", (dst[:, 512:1024], dst[:, 512:1024], g[:, 512:1024], ALU.mult),
        [dst_name + ("n",), gb_names[0]], [dst_name + ("g1",)])
    k.v("vector", "tensor_tensor", (dst, dst, b, ALU.add), [dst_name + ("g0",), dst_name + ("g1",), gb_names[1]],
        [dst_name])


def emit_route(k, ps, sm, ti, slots_i, rw, atot, ustrict, ones_b, ect, pl=2, stage=0):
    L = sm[:, 16:88]
    gm8 = sm[:, 88:96]
    ohg = sm[:, 96:104]
    dd = sm[:, 104:112]
    pg = sm[:, 112:113]
    el = sm[:, 113:121]
    tm8 = sm[:, 121:129]
    oh1 = sm[:, 129:137]
    oh2 = sm[:, 137:145]
    dv = sm[:, 145:146]
    t64 = sm[:, 146:210]
    sl = sm[:, 210:212]
    n = lambda s: ("rt_" + s,)
    V = lambda name, args, r, w: k.v("vector", name, args, r, w)
    if stage == 2:
        return _route_p2(k, ps, sm, ti, slots_i, atot, ustrict, ones_b, ect, pl)
    V("tensor_copy", (L, ps[pl][:, 0:72]), [("ps", pl)], [n("L")])
    V("max", (gm8, L[:, 0:8]), [n("L")], [n("gm8")])
    V("tensor_scalar", (ohg, L[:, 0:8], gm8[:, 0:1], None, ALU.is_equal), [n("L"), n("gm8")], [n("ohg")])
    V("tensor_scalar", (dd, L[:, 0:8], gm8[:, 0:1], None, ALU.subtract), [n("L"), n("gm8")], [n("dd")])
    k.act(dd, dd, AF.Sigmoid, [n("dd")], [n("dd")])
    V("tensor_scalar", (tm8, dd, -1.0, 1.0, ALU.mult, ALU.add), [n("dd")], [n("tm8")])
    V("reciprocal", (tm8, tm8), [n("tm8")], [n("tm8")])
    V("tensor_tensor", (dd, dd, tm8, ALU.mult), [n("dd"), n("tm8")], [n("dd")])
    V("tensor_reduce", (pg, dd, AX.X, ALU.add), [n("dd")], [n("pg")])
    V("reciprocal", (pg, pg), [n("pg")], [n("pg")])
    t3 = t64.rearrange("p (g e) -> p g e", g=8)
    L3 = L[:, 8:72].rearrange("p (g e) -> p g e", g=8)
    V("tensor_tensor", (t3, L3, ohg.unsqueeze(2).to_broadcast([128, 8, 8]), ALU.mult), [n("L"), n("ohg")], [n("t64")])
    V("tensor_reduce", (el, t64.rearrange("p (g e) -> p e g", g=8), AX.X, ALU.add), [n("t64")], [n("el")])
    V("max", (tm8, el), [n("el"), n("tm8")], [n("tm8")])
    V("tensor_scalar", (oh1, el, tm8[:, 0:1], None, ALU.is_equal), [n("el"), n("tm8")], [n("oh1")])
    V("tensor_scalar", (oh2, el, tm8[:, 1:2], None, ALU.is_equal), [n("el"), n("tm8")], [n("oh2")])
    V("tensor_tensor", (dv, tm8[:, 0:1], tm8[:, 1:2], ALU.subtract), [n("tm8")], [n("dv")])
    k.act(dv, dv, AF.Sigmoid, [n("dv")], [n("dv")])
    V("tensor_tensor", (rw[:, ti, 0:1], dv, pg, ALU.mult), [n("dv"), n("pg")], [("rw", ti, 0)])
    V("tensor_tensor", (rw[:, ti, 1:2], pg, rw[:, ti, 0:1], ALU.subtract), [n("pg"), ("rw", ti, 0)], [("rw", ti, 1)])
    A1 = k.A1
    A2 = k.A2
    Ab = k.Ab
    A13 = A1.rearrange("p (g e) -> p g e", g=8)
    A23 = A2.rearrange("p (g e) -> p g e", g=8)
    gb = ohg.unsqueeze(2).to_broadcast([128, 8, 8])
    V("tensor_tensor", (A13, gb, oh1.unsqueeze(1).to_broadcast([128, 8, 8]), ALU.mult), [n("ohg"), n("oh1")], [n("A1")])
    V("tensor_tensor", (A23, gb, oh2.unsqueeze(1).to_broadcast([128, 8, 8]), ALU.mult), [n("ohg"), n("oh2")], [n("A2")])
    V("tensor_tensor", (Ab, A1, A2, ALU.add), [n("A1"), n("A2")], [n("Ab")])
    if stage == 1:
        return
    _route_p2(k, ps, sm, ti, slots_i, atot, ustrict, ones_b, ect, pl)


def _route_p2(k, ps, sm, ti, slots_i, atot, ustrict, ones_b, ect, pl):
    t64 = sm[:, 146:210]
    sl = sm[:, 210:212]
    n = lambda s: ("rt_" + s,)
    V = lambda name, args, r, w: k.v("vector", name, args, r, w)
    A1 = k.A1
    A2 = k.A2
    Ab = k.Ab
    k.mm(ps[pl][:, 128:192], ustrict, Ab, True, False, ["ustrict", n("Ab")], [("ps", pl)])
    k.mm(ps[pl][:, 128:192], ones_b, atot, False, True, ["ones_b", "atot"], [("ps", pl)])
    V("tensor_tensor", (t64, ps[pl][:, 128:192], ect, ALU.add), [("ps", pl), "ect"], [n("t64")])
    V("tensor_tensor", (atot, atot, Ab, ALU.add), [n("Ab"), "atot"], ["atot"])
    V("tensor_tensor", (A1, A1, t64, ALU.mult), [n("A1"), n("t64")], [n("A1")])
    V("tensor_tensor", (A2, A2, t64, ALU.mult), [n("A2"), n("t64")], [n("A2")])
    V("tensor_reduce", (sl[:, 0:1], A1, AX.X, ALU.add), [n("A1")], [n("sl0")])
    V("tensor_reduce", (sl[:, 1:2], A2, AX.X, ALU.add), [n("A2")], [n("sl1")])
    V("tensor_copy", (slots_i[:, ti, :], sl), [n("sl0"), n("sl1")], [("slots", ti)])


_NC_CACHE = {}


def _consts(hf):
    bf = ml_dtypes.bfloat16
    c = {}
    c["c_identf"] = np.eye(128, dtype=np.float32)
    c["c_identb"] = np.eye(128, dtype=np.float32).astype(bf)
    c["c_ones"] = np.ones((128, 128), np.float32).astype(bf)
    c["c_ustrict"] = np.triu(np.ones((128, 128), np.float32), 1).astype(bf)
    row = np.arange(128)
    k2 = np.arange(128)
    ed = np.zeros((64, 128, 256), np.float32)
    for j in range(64):
        s = j + 64 * row
        th = 2.0 * np.pi * ((s[:, None] * k2[None, :]) % 8192) / 8192.0
        ed[j, :, 0:128] = np.cos(th) / 32.0
        ed[j, :, 128:256] = np.sin(th) / 32.0
    c["c_edft"] = ed.astype(bf)
    ph = 2.0 * np.pi * ((row[:, None] * row[None, :]) % 128) / 128.0
    C = np.cos(ph); S = np.sin(ph)
    c["c_fca"] = np.concatenate([C, -S], axis=1).astype(np.float32).astype(bf)
    c["c_fcb"] = np.concatenate([-S, -C], axis=1).astype(np.float32).astype(bf)
    s1 = np.arange(64)
    k1 = 32 * hf + np.arange(32)
    psi = 2.0 * np.pi * ((s1[:, None] * k1[None, :]) % 64) / 64.0
    bdc = np.zeros((128, 64), np.float32); bds = np.zeros((128, 64), np.float32)
    for a in range(2):
        bdc[a::2, a * 32:(a + 1) * 32] = np.cos(psi) / 32.0
        bds[a::2, a * 32:(a + 1) * 32] = np.sin(psi) / 32.0
    c["c_bdc"] = bdc.astype(bf); c["c_bds"] = bds.astype(bf)
    c["c_ec"] = np.tile((np.arange(64, dtype=np.float32) * CAP)[None, :], (128, 1)).astype(np.float32)
    c["c_mhalf"] = np.full((128, 1), -0.5, np.float32)
    c["c_onesrow"] = np.ones((1, 128), np.float32)
    return c


def _bias_tiles(rpb, hf):
    out = np.full((5, 8, 128, 768), MASKV, np.float32)
    specs = [(10, [-2, -1, 0, 1, 2]), (0, [-2, -1, 0, 1, 2, 3]), (1, [-2, -1, 0, 1, 2]),
             (30, [-2, -1, 0, 1, 2]), (31, [-3, -2, -1, 0, 1, 2])]
    qc = np.arange(64); kc = np.arange(64)
    cs = np.clip(qc - 8, 0, 48)
    colvalid = (kc[:, None] >= cs[None, :]) & (kc[:, None] < cs[None, :] + 16)
    dc = kc[:, None] - qc[None, :] + 15
    dcc = np.clip(dc, 0, 30)
    for ty, (qp, offs) in enumerate(specs):
        for j, off in enumerate(offs):
            kp = qp + off
            for aq in range(2):
                r = 64 * hf + 2 * qp + aq
                rs = min(max(r - 4, 0), 120)
                for ak in range(2):
                    kr = 64 * hf + 2 * kp + ak
                    if kr < 0 or kr > 127 or kr < rs or kr > rs + 7:
                        continue
                    drr = kr - r + 7
                    vals = rpb[:, drr, :][:, dcc]
                    blk = np.where(colvalid[None], vals, np.float32(MASKV))
                    out[ty, :, ak * 64:(ak + 1) * 64, j * 128 + aq * 64:j * 128 + (aq + 1) * 64] = blk
    return out


def kernel(x, mem, w_in, w_gate, b_gate, w_mem_kv, rpb, w_fourier_o, w_na_o, w_mem_o, w_out, ln1_g, ln1_b,
           w_router_group, b_router_group, w_router_expert, b_router_expert, w_exp_gate, w_exp_up, w_exp_down,
           ln2_g, ln2_b, _debug=False):
    f = lambda a: np.ascontiguousarray(np.asarray(a, dtype=np.float32))
    x = f(x); mem = f(mem)
    key = bool(_debug)
    if key not in _NC_CACHE:
        _NC_CACHE[key] = build_program(debug=_debug)
    nc = _NC_CACHE[key]
    shared = {
        "w_in": f(w_in[0]), "w_gate": f(w_gate[0]),
        "bg": f(np.asarray(b_gate[0]).reshape(24, 128).T),
        "w_kv": f(w_mem_kv[0]), "w_fo": f(w_fourier_o[0]), "w_nao": f(w_na_o[0]), "w_memo": f(w_mem_o[0]),
        "w_out": f(w_out[0]), "ln1_g": f(ln1_g), "ln1_b": f(ln1_b), "ln2_g": f(ln2_g), "ln2_b": f(ln2_b),
        "wr": f(np.concatenate([np.asarray(w_router_group[0]), np.asarray(w_router_expert[0])], axis=1)),
        "br": f(np.concatenate([np.asarray(b_router_group[0]), np.asarray(b_router_expert[0])])[None, :]),
        "w_eg": f(w_exp_gate[0]), "w_eu": f(w_exp_up[0]), "w_ed": f(w_exp_down[0]),
    }
    rp = f(rpb[0])
    in_maps = []
    for c in range(NCORES):
        b, hf = c // 2, c % 2
        m = dict(shared)
        m.update(_consts(hf))
        m["xf"] = x[b]
        xo = np.zeros((4608, D), np.float32)
        xo[0:4096] = x[b, 4096 * hf:4096 * hf + 4096]
        if hf == 1:
            xo[4096:4352] = x[b, 4096 - 256:4096]
        else:
            xo[4352:4608] = x[b, 4096:4096 + 256]
        m["xo"] = xo
        m["mem"] = mem[b]
        m["biasT"] = _bias_tiles(rp, hf)
        in_maps.append(m)
    res = run_bass_kernel_spmd(nc, in_maps, core_ids=list(range(NCORES)))
    out = np.zeros((4, 8192, D), np.float32)
    for c in range(NCORES):
        b, hf = c // 2, c % 2
        out[b, 4096 * hf:4096 * hf + 4096] = res.results[c]["out"]
    if _debug:
        return out, res
    return out
```

```python
import contextlib
import math
import numpy as np
import ml_dtypes
import concourse.bass as bass
import concourse.mybir as mybir
from concourse.bass_utils import run_bass_kernel_spmd

F32 = mybir.dt.float32
BF16 = mybir.dt.bfloat16
I32 = mybir.dt.int32
U8 = mybir.dt.uint8
ALU = mybir.AluOpType
AF = mybir.ActivationFunctionType
AX = mybir.AxisListType

ENGS = ("tensor", "vector", "scalar", "gpsimd", "sync")
NCORES = 8
D = 1024
CAP = 256
NEXP = 64
NSLOT = NEXP * CAP
ALPHA = 2.0 ** 0.25
LN_EPS = 1e-5
MASKV = -30000.0
ARENA = 206 * 1024


class Op:
    __slots__ = ("eng", "fn", "dma", "deps", "sig", "sigval", "key", "dval", "slot")

    def __init__(self, eng, fn, dma):
        self.eng = eng
        self.fn = fn
        self.dma = dma
        self.deps = []
        self.sig = False
        self.sigval = 0
        self.key = None
        self.dval = 0
        self.slot = None


class Prog:
    def __init__(self, nc):
        self.nc = nc
        self.ops = {e: [] for e in ENGS}
        self.writers = {}
        self.readers = {}
        self.key_slot = {}
        self.slot_count = []
        self.nops = 0

    def op(self, eng, fn, reads=(), writes=(), dma=False, key=None, partial=False):
        o = Op(eng, fn, dma)
        self.nops += 1
        deps = {}
        banks = set()
        for b in list(reads) + list(writes):
            if isinstance(b, tuple) and b and b[0] == "ps":
                banks.add(b[1])
        for bk in banks:
            nm = ("psbank", bk)
            for w in self.writers.get(nm, ()):
                deps[id(w)] = w
            self.writers[nm] = [o]
        for b in reads:
            for w in self.writers.get(b, ()):
                deps[id(w)] = w
        for b in writes:
            if not partial:
                for w in self.writers.get(b, ()):
                    deps[id(w)] = w
            for r in self.readers.get(b, ()):
                deps[id(r)] = r
        for b in reads:
            self.readers.setdefault(b, []).append(o)
        for b in writes:
            if partial:
                self.writers.setdefault(b, []).append(o)
            else:
                self.writers[b] = [o]
                self.readers[b] = []
        deps.pop(id(o), None)
        o.deps = list(deps.values())
        if dma:
            if key is None:
                key = writes[0] if writes else reads[0]
            if key not in self.key_slot:
                self.key_slot[key] = len(self.key_slot)
                if len(self.key_slot) > len(self.slot_count):
                    self.slot_count.append(0)
            s = self.key_slot[key]
            self.slot_count[s] += 1
            o.slot = s
            o.dval = 16 * self.slot_count[s]
        self.ops[eng].append(o)
        return o

    def barrier(self):
        lasts = []
        for e in ENGS:
            nd = [o for o in self.ops[e] if not o.dma and o.fn is not None]
            if nd:
                lasts.append(nd[-1])
        dmas = {}
        for e in ENGS:
            for o in self.ops[e]:
                if o.dma and (o.slot not in dmas or dmas[o.slot].dval < o.dval):
                    dmas[o.slot] = o
        for e in ENGS:
            o = Op(e, None, False)
            o.deps = [l for l in lasts if l.eng != e] + list(dmas.values())
            self.ops[e].append(o)
        self.writers = {}
        self.readers = {}
        self.key_slot = {}

    def emit(self):
        nc = self.nc
        for e in ENGS:
            for o in self.ops[e]:
                for d in o.deps:
                    if not d.dma and not (d.eng == "tensor" and o.eng == "tensor"):
                        d.sig = True
        for e in ENGS:
            c = 0
            for o in self.ops[e]:
                if o.sig:
                    c += 1
                    o.sigval = c
        nslots = len(self.slot_count)
        with contextlib.ExitStack() as st:
            esem = {e: st.enter_context(nc.semaphore("e_" + e)) for e in ENGS}
            ksem = [st.enter_context(nc.semaphore("d%d" % i)) for i in range(nslots)]
            block = st.enter_context(nc.Block())

            def body(e):
                def _f(eng):
                    waited = {}
                    for o in self.ops[e]:
                        for d in o.deps:
                            if d.dma:
                                s, v = ksem[d.slot], d.dval
                            elif d.sig:
                                s, v = esem[d.eng], d.sigval
                            else:
                                continue
                            if waited.get(id(s), 0) >= v:
                                continue
                            waited[id(s)] = v
                            eng.wait_ge(s, v)
                        if o.fn is None:
                            continue
                        ins = o.fn(eng)
                        if o.dma:
                            ins.then_inc(ksem[o.slot], 16)
                        elif o.sig:
                            ins.then_inc(esem[e], 1)
                    if e == "sync":
                        for i in range(nslots):
                            v = 16 * self.slot_count[i]
                            if waited.get(id(ksem[i]), 0) < v:
                                eng.wait_ge(ksem[i], v)
                return _f

            block.sync(body("sync"))
            block.tensor(body("tensor"))
            block.vector(body("vector"))
            block.scalar(body("scalar"))
            block.gpsimd(body("gpsimd"))


def _isz(dt):
    return {F32: 4, BF16: 2, I32: 4, U8: 1}[dt]


class Arena:
    def __init__(self, ap, size):
        self.ap = ap
        self.size = size
        self.off = 0

    def alloc(self, shape, dt):
        n = int(np.prod(shape))
        nb = n * _isz(dt)
        off = self.off
        self.off += (nb + 63) // 64 * 64
        assert self.off <= self.size, ("arena overflow", self.off, self.size)
        v = self.ap[:, off:off + nb].bitcast(dt)
        if len(shape) == 2:
            v = v.rearrange("p (a b) -> p a b", a=shape[0], b=shape[1])
        elif len(shape) == 3:
            v = v.rearrange("p (a b c) -> p a b c", a=shape[0], b=shape[1], c=shape[2])
        elif len(shape) == 4:
            v = v.rearrange("p (a b c d) -> p a b c d", a=shape[0], b=shape[1], c=shape[2], d=shape[3])
        return v

    def mark(self):
        return self.off

    def reset(self, m):
        self.off = m


class K:
    def __init__(self, nc, P):
        self.nc = nc
        self.P = P
        self.ev = 0

    def mm(self, out, lhsT, rhs, start, stop, reads, writes):
        self.P.op("tensor", lambda e: e.matmul(out, lhsT, rhs, start=start, stop=stop),
                  reads=reads, writes=writes, partial=not start)

    def mmi(self, groups):
        n = max(len(g) for g in groups)
        for i in range(n):
            for g in groups:
                if i < len(g):
                    out, lhsT, rhs, reads, writes = g[i]
                    self.mm(out, lhsT, rhs, i == 0, i == len(g) - 1, reads, writes)

    def tri(self, groups):
        n = max(len(g) for g in groups)
        for i in range(n):
            for g in groups:
                if i < len(g):
                    out, in_, ident, reads, writes = g[i]
                    self.tr(out, in_, ident, reads, writes, partial=(i > 0))

    def tr(self, out, in_, ident, reads, writes, partial=True):
        self.P.op("tensor", lambda e: e.transpose(out, in_, ident), reads=reads, writes=writes,
                  partial=partial)

    def act(self, out, in_, func, reads, writes, bias=None, scale=None, partial=False):
        def f(e):
            kw = {}
            if bias is not None:
                kw["bias"] = bias
            if scale is not None:
                kw["scale"] = scale
            return e.activation(out, in_, func, **kw)
        self.P.op("scalar", f, reads=reads, writes=writes, partial=partial)

    def evac(self, out, in_, reads, writes, eng=None, partial=False):
        if eng is None:
            self.ev += 1
            eng = "vector" if self.ev % 2 else "scalar"
        if eng == "vector":
            self.P.op("vector", lambda e: e.tensor_copy(out, in_), reads=reads, writes=writes, partial=partial)
        else:
            self.P.op("scalar", lambda e: e.activation(out, in_, AF.Copy), reads=reads, writes=writes,
                      partial=partial)

    def v(self, eng, name, args, reads, writes, kw=None, partial=False):
        kw = kw or {}
        self.P.op(eng, lambda e: getattr(e, name)(*args, **kw), reads=reads, writes=writes, partial=partial)

    def dma(self, q, out, in_, reads, writes, key=None, partial=False):
        self.P.op(q, lambda e: e.dma_start(out=out, in_=in_), reads=reads, writes=writes, dma=True,
                  key=key, partial=partial)


def build_program(debug=False):
    nc = bass.Bass("TRN2", target_bir_lowering=False)
    _bc = {}

    def _bc_reg(e):
        if "r" not in _bc:
            _bc["r"] = e.to_reg(NSLOT - 1)
        return _bc["r"]
    dr = {}

    def din(name, shape, dt=F32):
        dr[name] = nc.dram_tensor(name, list(shape), dt, kind="ExternalInput").ap()

    def dscr(name, shape, dt):
        dr[name] = nc.dram_tensor(name, list(shape), dt, kind="Internal").ap()

    din("xf", [8192, D]); din("xo", [4608, D]); din("mem", [256, D])
    din("w_in", [D, 2560]); din("w_gate", [D, 3072]); din("bg", [128, 24])
    din("w_kv", [D, 1024]); din("w_fo", [512, D]); din("w_nao", [512, D]); din("w_memo", [512, D])
    din("w_out", [D, D]); din("ln1_g", [1, D]); din("ln1_b", [1, D]); din("ln2_g", [1, D]); din("ln2_b", [1, D])
    din("wr", [D, 72]); din("br", [1, 72])
    din("w_eg", [NEXP, D, 512]); din("w_eu", [NEXP, D, 512]); din("w_ed", [NEXP, 512, D])
    din("biasT", [5, 8, 128, 768])
    din("c_identf", [128, 128]); din("c_identb", [128, 128], BF16); din("c_ones", [128, 128], BF16)
    din("c_ustrict", [128, 128], BF16); din("c_edft", [64, 128, 256], BF16)
    din("c_fca", [128, 256], BF16); din("c_fcb", [128, 256], BF16)
    din("c_bdc", [128, 64], BF16); din("c_bds", [128, 64], BF16)
    din("c_ec", [128, 64]); din("c_mhalf", [128, 1]); din("c_onesrow", [1, 128])
    dr["out"] = nc.dram_tensor("out", [4096, D], F32, kind="ExternalOutput").ap()
    dscr("yfT_d", [128, 4, 4096], BF16); dscr("ynaT_d", [128, 4, 4096], BF16); dscr("ymemT_d", [128, 4, 4096], BF16)
    dscr("xT_d", [8, 128, 8, 512], BF16); dscr("qmT_d", [8, 128, 4, 512], BF16)
    dscr("h_d", [4096, D], F32); dscr("xg_d", [NSLOT, D], BF16); dscr("yb_d", [NSLOT, D], BF16)
    if debug:
        dr["dbg_h"] = nc.dram_tensor("dbg_h", [4096, D], F32, kind="ExternalOutput").ap()
        dr["dbg_y3"] = nc.dram_tensor("dbg_y3", [3, 128, 4, 4096], BF16, kind="ExternalOutput").ap()
        dr["dbg_rt"] = nc.dram_tensor("dbg_rt", [128, 32, 4], F32, kind="ExternalOutput").ap()

    P = Prog(nc)
    k = K(nc, P)
    with contextlib.ExitStack() as st:
        arena_t = st.enter_context(nc.sbuf_tensor("arena", [128, ARENA], U8))
        A = Arena(arena_t, ARENA)
        ps = [st.enter_context(nc.psum_tensor("ps%d" % i, [128, 512], F32)) for i in range(8)]
        psb = [p[:].bitcast(BF16) for p in ps]

        identf = A.alloc([128], F32); identb = A.alloc([128], BF16); ones_b = A.alloc([128], BF16)
        ustrict = A.alloc([128], BF16)
        slots_i = A.alloc([32, 2], I32); rw = A.alloc([32, 2], F32)
        k.dma("sync", identf, dr["c_identf"], [], ["identf"])
        k.dma("sync", identb, dr["c_identb"], [], ["identb"])
        k.dma("sync", ones_b, dr["c_ones"], [], ["ones_b"])
        k.dma("sync", ustrict, dr["c_ustrict"], [], ["ustrict"])
        base_mark = A.mark()

        wf = A.alloc([8, 512], BF16)
        fca = A.alloc([256], BF16); fcb = A.alloc([256], BF16); bdc = A.alloc([64], BF16); bds = A.alloc([64], BF16)
        AT = [A.alloc([2, 64, 64, 2], BF16) for _ in range(4)]
        xt = [A.alloc([1024], F32) for _ in range(4)]
        xT = [A.alloc([8, 128], BF16) for _ in range(2)]
        ub = [A.alloc([512], BF16) for _ in range(2)]
        Ej = [A.alloc([256], BF16) for _ in range(4)]
        Bt = [A.alloc([256], BF16) for _ in range(4)]
        yfs = [A.alloc([4096], BF16) for _ in range(2)]
        k.dma("gpsimd", wf, dr["w_in"][:, 0:512].rearrange("(c p) n -> p c n", p=128), [], ["wf"])
        k.dma("sync", fca, dr["c_fca"], [], ["fca"]); k.dma("sync", fcb, dr["c_fcb"], [], ["fcb"])
        k.dma("sync", bdc, dr["c_bdc"], [], ["bdc"]); k.dma("sync", bds, dr["c_bds"], [], ["bds"])
        xf_v = dr["xf"].rearrange("(r c) f -> c r f", c=64)

        def a_load(j):
            k.dma("sync", xt[j % 4], xf_v[j], [], [("xt", j % 4)])
            k.dma("sync", Ej[j % 4], dr["c_edft"][j], [], [("Ej", j % 4)])
        a_load(0); a_load(1)
        for j0 in range(0, 64, 2):
            for j in (j0, j0 + 1):
                if j + 2 < 64:
                    a_load(j + 2)
            tg = []
            for j in (j0, j0 + 1):
                for hb in range(2):
                    bank = 2 * (j % 2) + hb
                    tg.append([(ps[bank][:, q * 128:(q + 1) * 128], xt[j % 4][:, (hb * 4 + q) * 128:(hb * 4 + q + 1) * 128],
                                identf, [("xt", j % 4), "identf"], [("ps", bank)]) for q in range(4)])
            k.tri(tg)
            for j in (j0, j0 + 1):
                for hb in range(2):
                    bank = 2 * (j % 2) + hb
                    k.evac(xT[j % 2][:, hb * 4:(hb + 1) * 4, :], ps[bank][:].rearrange("p (a b) -> p a b", a=4),
                           [("ps", bank)], [("xT", j % 2, hb)])
            k.mmi([[(ps[4 + (j % 2)][:], xT[j % 2][:, fc, :], wf[:, fc, :],
                     [("xT", j % 2, 0), ("xT", j % 2, 1), "wf"], [("ps", 4 + (j % 2))]) for fc in range(8)]
                   for j in (j0, j0 + 1)])
            for j in (j0, j0 + 1):
                k.evac(ub[j % 2], ps[4 + (j % 2)][:], [("ps", 4 + (j % 2))], [("ub", j % 2)])
            for j in (j0, j0 + 1):
                for g in range(4):
                    pa = 6 + (g % 2)
                    k.mm(ps[pa][:, 0:256], ub[j % 2][:, g * 128:(g + 1) * 128], Ej[j % 4], True, True,
                         [("ub", j % 2), ("Ej", j % 4)], [("ps", pa)])
                    k.evac(AT[g][:, :, :, j, :], ps[pa][:, 0:256].rearrange("p (r k a) -> p r k a", r=2, k=64, a=2),
                           [("ps", pa)], [("AT", g)], partial=True)
        it = 0
        for g in range(4):
            ys = yfs[g % 2]
            ysv = ys.rearrange("p (k q) -> p k q", k=32)
            for kk0 in range(0, 64, 2):
                kks = (kk0, kk0 + 1)
                k.mmi([[(ps[kk % 2][:, 0:256], AT[g][:, 0, kk, :, :].rearrange("p s a -> p (s a)"), fca,
                         [("AT", g), "fca"], [("ps", kk % 2)]),
                        (ps[kk % 2][:, 0:256], AT[g][:, 1, kk, :, :].rearrange("p s a -> p (s a)"), fcb,
                         [("AT", g), "fcb"], [("ps", kk % 2)])] for kk in kks])
                bts = {}
                for kk in kks:
                    bts[kk] = it % 4
                    k.evac(Bt[it % 4], ps[kk % 2][:, 0:256], [("ps", kk % 2)], [("Bt", it % 4)])
                    it += 1
                k.mmi([[(ps[2 + kk % 2][:, 0:64], Bt[bts[kk]][:, 0:128], bdc, [("Bt", bts[kk]), "bdc"], [("ps", 2 + kk % 2)]),
                        (ps[2 + kk % 2][:, 0:64], Bt[bts[kk]][:, 128:256], bds, [("Bt", bts[kk]), "bds"], [("ps", 2 + kk % 2)])]
                       for kk in kks])
                for kk in kks:
                    k.evac(ysv[:, :, 2 * kk:2 * kk + 2].rearrange("p k a -> p a k"),
                           ps[2 + kk % 2][:, 0:64].rearrange("p (a k) -> p a k", a=2),
                           [("ps", 2 + kk % 2)], [("yfs", g % 2)], partial=True)
            k.dma("sync", dr["yfT_d"][:, g, :], ys, [("yfs", g % 2)], [("yfT_d", g)])
        P.barrier()
        A.reset(base_mark)

        kT = A.alloc([4, 4608], BF16)
        Vext = A.alloc([36, 8, 65], BF16)
        qT = A.alloc([4, 4096], BF16)
        b1_mark = A.mark()
        W = A.alloc([8, 2048], BF16)
        xt = [A.alloc([1024], F32) for _ in range(4)]
        xTq = [A.alloc([8, 512], BF16) for _ in range(2)]
        qmq = [A.alloc([4, 512], BF16) for _ in range(2)]
        for cb in range(4):
            k.dma("gpsimd", W[:, :, cb * 512:(cb + 1) * 512],
                  dr["w_in"][:, 512 + cb * 512:512 + (cb + 1) * 512].rearrange("(c p) n -> p c n", p=128),
                  [], [("W", cb)])
        Wr = [("W", cb) for cb in range(4)]
        k.v("vector", "memset", (Vext[:, :, :, 64:65], 1.0), [], ["Vext1"])
        xo_t = dr["xo"].rearrange("(t p) f -> t p f", p=128)

        def b_load(t):
            k.dma("sync", xt[t % 4], xo_t[t], [], [("xt", t % 4)])
        b_load(0); b_load(1)
        for Q in range(9):
            xq = xTq[Q % 2]
            for tt0 in range(0, 4, 2):
                tts = (tt0, tt0 + 1)
                for tt in tts:
                    t = 4 * Q + tt
                    if t + 2 < 36:
                        b_load(t + 2)
                tg = []
                for tt in tts:
                    t = 4 * Q + tt
                    for hb in range(2):
                        bank = 2 * (t % 2) + hb
                        tg.append([(ps[bank][:, q * 128:(q + 1) * 128],
                                    xt[t % 4][:, (hb * 4 + q) * 128:(hb * 4 + q + 1) * 128], identf,
                                    [("xt", t % 4), "identf"], [("ps", bank)]) for q in range(4)])
                k.tri(tg)
                for tt in tts:
                    t = 4 * Q + tt
                    for hb in range(2):
                        bank = 2 * (t % 2) + hb
                        k.evac(xq[:, hb * 4:(hb + 1) * 4, tt * 128:(tt + 1) * 128],
                               ps[bank][:].rearrange("p (a b) -> p a b", a=4),
                               [("ps", bank)], [("xTq", Q % 2)], partial=True)
            xr = [("xTq", Q % 2)] + Wr

            def proj_pair(col0, cs, banks):
                k.mmi([[(ps[banks[i]][:], W[:, fc, col0 + c * 128:col0 + (c + 1) * 128], xq[:, fc, :], xr,
                         [("ps", banks[i])]) for fc in range(8)] for i, c in enumerate(cs)])
            for cs, banks in (((0, 1), (4, 5)), ((2, 3), (6, 7))):
                proj_pair(512, cs, banks)
                for i, c in enumerate(cs):
                    k.evac(kT[:, c, Q * 512:(Q + 1) * 512], ps[banks[i]][:], [("ps", banks[i])], [("kT", Q)],
                           partial=True)
            if Q < 8:
                for cs, banks in (((0, 1), (4, 5)), ((2, 3), (6, 7))):
                    proj_pair(0, cs, banks)
                    for i, c in enumerate(cs):
                        k.act(qT[:, c, Q * 512:(Q + 1) * 512], ps[banks[i]][:], AF.Copy, [("ps", banks[i])],
                              [("qT", Q)], scale=0.125, partial=True)
                for cs, banks in (((0, 1), (4, 5)), ((2, 3), (6, 7))):
                    proj_pair(1536, cs, banks)
                    for i, c in enumerate(cs):
                        k.evac(qmq[Q % 2][:, c, :], ps[banks[i]][:], [("ps", banks[i])], [("qmq", Q % 2)],
                               partial=True)
                k.dma("sync", dr["qmT_d"][Q], qmq[Q % 2], [("qmq", Q % 2)], [("qmT_d", Q)])
                k.dma("sync", dr["xT_d"][Q], xq, [("xTq", Q % 2)], [("xT_d", Q)])
            for tts, banks in (((0, 1), (4, 5)), ((2, 3), (6, 7))):
                k.mmi([[(ps[banks[i]][:], xq[:, fc, tt * 128:(tt + 1) * 128], W[:, fc, 1024:1536], xr,
                         [("ps", banks[i])]) for fc in range(8)] for i, tt in enumerate(tts)])
                for i, tt in enumerate(tts):
                    t = 4 * Q + tt
                    k.evac(Vext[:, t, :, 0:64], ps[banks[i]][:].rearrange("p (h d) -> p h d", h=8),
                           [("ps", banks[i])], [("Vext", t)])
        P.barrier()
        A.reset(b1_mark)

        bias_i = A.alloc([8, 768], BF16)
        bias_s = [A.alloc([8, 768], BF16) for _ in range(2)]
        eS = [A.alloc([768], BF16) for _ in range(3)]
        yna = [A.alloc([512], BF16) for _ in range(2)]
        rc = [A.alloc([8], F32) for _ in range(2)]
        ynaTq = [A.alloc([4, 512], BF16) for _ in range(2)]
        for hh in range(8):
            k.dma("gpsimd", bias_i[:, hh, :], dr["biasT"][0, hh], [], [("bias_i", hh)])
        special = {0: 1, 1: 2, 30: 3, 31: 4}

        def tile_of_pair(lp):
            if 0 <= lp < 32:
                return lp
            return {-2: 32, -1: 33, 32: 34, 33: 35}[lp]
        nsp = 0

        def offs_of(qp):
            if qp == 0:
                return [-2, -1, 0, 1, 2, 3]
            if qp == 31:
                return [-3, -2, -1, 0, 1, 2]
            return [-2, -1, 0, 1, 2]
        bias_of = {}

        def na_finalize(qp):
            yq = yna[qp % 2]
            rcq = rc[qp % 2]
            for h2 in range(8):
                po2 = 4 + (qp % 2) * 2 + (h2 // 4)
                oc2 = (h2 % 4) * 65
                k.v("vector", "reciprocal", (rcq[:, h2:h2 + 1], ps[po2][:, oc2 + 64:oc2 + 65]),
                    [("ps", po2, h2 % 4)], [("rc", qp % 2, h2)])
                k.v("vector", "tensor_scalar", (yq[:, h2 * 64:(h2 + 1) * 64], ps[po2][:, oc2:oc2 + 64],
                                                 rcq[:, h2:h2 + 1], None, ALU.mult),
                    [("ps", po2, h2 % 4), ("rc", qp % 2, h2)], [("yna", qp % 2)], partial=True)

        def na_transposes(qp):
            yq = yna[qp % 2]
            Q = qp // 4
            pT = 4 + (qp % 2) * 2 + 1
            tv = psb[pT][:, 640:1024]
            pT2 = 4 + (qp % 2) * 2
            tv2 = psb[pT2][:, 640:768]
            k.tr(tv[:, 0:128], yq[:, 0:128], identb, [("yna", qp % 2), "identb"], [("ps", pT, "T")], partial=False)
            k.tr(tv2, yq[:, 384:512], identb, [("yna", qp % 2), "identb"], [("ps", pT2, "T")], partial=False)
            for cc in range(1, 3):
                k.tr(tv[:, cc * 128:(cc + 1) * 128], yq[:, cc * 128:(cc + 1) * 128], identb,
                     [("yna", qp % 2), "identb"], [("ps", pT, "T")], partial=True)
            yt = ynaTq[Q % 2]
            k.evac(yt[:, 0:3, (qp % 4) * 128:(qp % 4 + 1) * 128], tv.rearrange("p (a b) -> p a b", a=3),
                   [("ps", pT, "T")], [("ynaTq", Q % 2)], partial=True)
            k.evac(yt[:, 3, (qp % 4) * 128:(qp % 4 + 1) * 128], tv2, [("ps", pT2, "T")], [("ynaTq", Q % 2)],
                   partial=True)
            if qp % 4 == 3:
                k.dma("sync", dr["ynaT_d"][:, :, Q * 512:(Q + 1) * 512], yt, [("ynaTq", Q % 2)], [("ynaT_d", Q)])

        def pv_list(n):
            qp, h = n // 8, n % 8
            offs = offs_of(qp); ns = len(offs)
            es = eS[n % 3]; en = ("eS", n % 3)
            po = 4 + (qp % 2) * 2 + (h // 4)
            oc = (h % 4) * 65
            out = []
            for j, off in enumerate(offs):
                kt = tile_of_pair(qp + off)
                out.append((ps[po][:, oc:oc + 65], es[:, j * 128:(j + 1) * 128], Vext[:, kt, h, :], j == 0, j == ns - 1,
                            [en, ("Vext", kt), "Vext1"], [("ps", po, h % 4)]))
            return out
        NH = 256
        for n in range(NH + 1):
            qk = []
            if n < NH:
                qp, h = n // 8, n % 8
                offs = offs_of(qp); ns = len(offs)
                if h == 0:
                    if qp in special:
                        bt_ = bias_s[nsp % 2]; bname = ("bias_s", nsp % 2)
                        for hh in range(8):
                            k.dma("gpsimd", bt_[:, hh, :], dr["biasT"][special[qp], hh], [], [bname], key=bname,
                                  partial=(hh > 0))
                        bias_of[qp] = (bt_, [bname])
                        nsp += 1
                    else:
                        bias_of[qp] = (bias_i, None)
                bt_, breads = bias_of[qp]
                c = h // 2; pb0 = 64 * (h % 2)
                sX = (n % 2) * 2; sY = sX + 1
                br_ = breads if breads is not None else [("bias_i", h)]
                k.mm(ps[sX][:], identb, bt_[:, h, 0:512], True, False, br_ + ["identb"], [("ps", sX)])
                k.mm(ps[sY][:, 0:(ns - 4) * 128], identb, bt_[:, h, 512:512 + (ns - 4) * 128], True, False,
                     br_ + ["identb"], [("ps", sY)])
                for j, off in enumerate(offs):
                    kt = tile_of_pair(qp + off)
                    bank = sX if j < 4 else sY
                    last = (j == 3) or (j == ns - 1)
                    qk.append((ps[bank][:, (j % 4) * 128:(j % 4 + 1) * 128],
                               kT[pb0:pb0 + 64, c, kt * 128:(kt + 1) * 128],
                               qT[pb0:pb0 + 64, c, qp * 128:(qp + 1) * 128], False, last,
                               [("kT", kt // 4), ("qT", qp // 4)], [("ps", bank)]))
            pv = pv_list(n - 1) if n >= 1 else []
            for i in range(max(len(qk), len(pv))):
                if i < len(qk):
                    k.mm(*qk[i])
                if i < len(pv):
                    k.mm(*pv[i])
            if n < NH:
                es = eS[n % 3]; en = ("eS", n % 3)
                k.act(es[:, 0:512], ps[sX][:], AF.Exp, [("ps", sX)], [en])
                k.act(es[:, 512:ns * 128], ps[sY][:, 0:(ns - 4) * 128], AF.Exp, [("ps", sY)], [en], partial=True)
            if n >= 1 and (n - 1) % 8 == 7:
                na_finalize((n - 1) // 8)
            if n >= 1 and (n - 1) % 8 == 3 and (n - 1) // 8 >= 1:
                na_transposes((n - 1) // 8 - 1)
        na_transposes(31)
        P.barrier()
        A.reset(base_mark)

        wkv = A.alloc([8, 1024], BF16)
        memt = [A.alloc([1024], F32) for _ in range(2)]
        memT = A.alloc([8, 256], BF16)
        kmT = A.alloc([4, 256], BF16)
        Vm = A.alloc([2, 4, 129], BF16)
        qmq = [A.alloc([4, 512], BF16) for _ in range(2)]
        eSm = [A.alloc([512], BF16) for _ in range(4)]
        ym = [A.alloc([512], BF16) for _ in range(8)]
        rcm = [A.alloc([4], F32) for _ in range(8)]
        ymTq = [A.alloc([4, 512], BF16) for _ in range(2)]
        for cb in range(2):
            k.dma("gpsimd", wkv[:, :, cb * 512:(cb + 1) * 512],
                  dr["w_kv"][:, cb * 512:(cb + 1) * 512].rearrange("(c p) n -> p c n", p=128), [], [("wkv", cb)])
        k.v("vector", "memset", (Vm[:, :, :, 128:129], 1.0), [], ["Vm1"])
        for mc in range(2):
            k.dma("sync", memt[mc], dr["mem"][mc * 128:(mc + 1) * 128, :], [], [("memt", mc)])
            for hb in range(2):
                bank = 2 * mc + hb
                for q in range(4):
                    fc = hb * 4 + q
                    k.tr(ps[bank][:, q * 128:(q + 1) * 128], memt[mc][:, fc * 128:(fc + 1) * 128], identf,
                         [("memt", mc), "identf"], [("ps", bank)], partial=(q > 0))
                k.evac(memT[:, hb * 4:(hb + 1) * 4, mc * 128:(mc + 1) * 128],
                       ps[bank][:].rearrange("p (a b) -> p a b", a=4), [("ps", bank)], ["memT"], partial=True)
        for h in range(4):
            pb_ = 4 + h % 2
            for fc in range(8):
                k.mm(ps[pb_][:, 0:256], wkv[:, fc, h * 128:(h + 1) * 128], memT[:, fc, :], fc == 0, fc == 7,
                     ["memT", ("wkv", 0)], [("ps", pb_)])
            k.evac(kmT[:, h, :], ps[pb_][:, 0:256], [("ps", pb_)], ["kmT"], partial=True)
        for mc in range(2):
            pb_ = 6 + mc
            for fc in range(8):
                k.mm(ps[pb_][:], memT[:, fc, mc * 128:(mc + 1) * 128], wkv[:, fc, 512:1024], fc == 0, fc == 7,
                     ["memT", ("wkv", 1)], [("ps", pb_)])
            k.evac(Vm[:, mc, :, 0:128], ps[pb_][:].rearrange("p (h d) -> p h d", h=4), [("ps", pb_)], ["Vm"],
                   partial=True)
        msc = 128.0 ** -0.5
        k.dma("sync", qmq[0], dr["qmT_d"][0], [], [("qmq", 0)])
        ecnt = 0
        for Q in range(8):
            if Q + 1 < 8:
                k.dma("sync", qmq[(Q + 1) % 2], dr["qmT_d"][Q + 1], [], [("qmq", (Q + 1) % 2)])
            qq = qmq[Q % 2]
            for h in range(4):
                ebufs = []
                for mc in range(2):
                    bank = (ecnt % 2) * 2 + mc
                    k.mm(ps[bank][:], kmT[:, h, mc * 128:(mc + 1) * 128], qq[:, h, :], True, True,
                         ["kmT", ("qmq", Q % 2)], [("ps", bank)])
                    eb = (ecnt % 2) * 2 + mc
                    k.act(eSm[eb], ps[bank][:], AF.Exp, [("ps", bank)], [("eSm", eb)], scale=msc)
                    ebufs.append(eb)
                for tts in ((0, 2), (1, 3)):
                    k.mmi([[(ps[4 + (tt // 2) + 2 * (ecnt % 2)][:, (tt % 2) * 129:(tt % 2) * 129 + 129],
                             eSm[ebufs[mc]][:, tt * 128:(tt + 1) * 128], Vm[:, mc, h, :],
                             [("eSm", ebufs[mc]), "Vm", "Vm1"], [("ps", 4 + (tt // 2) + 2 * (ecnt % 2), tt % 2)])
                            for mc in range(2)] for tt in tts])
                for tt in range(4):
                    po = 4 + (tt // 2) + 2 * (ecnt % 2)
                    oc = (tt % 2) * 129
                    yb_ = (Q % 2) * 4 + tt
                    k.v("vector", "reciprocal", (rcm[yb_][:, h:h + 1], ps[po][:, oc + 128:oc + 129]),
                        [("ps", po, tt % 2)], [("rcm", yb_, h)])
                    k.v("vector", "tensor_scalar", (ym[yb_][:, h * 128:(h + 1) * 128], ps[po][:, oc:oc + 128],
                                                     rcm[yb_][:, h:h + 1], None, ALU.mult),
                        [("ps", po, tt % 2), ("rcm", yb_, h)], [("ym", yb_)], partial=True)
                ecnt += 1
            yt = ymTq[Q % 2]
            for tt in range(4):
                yb_ = (Q % 2) * 4 + tt
                bank = (tt % 2) * 2 + (ecnt % 2)
                tvm = psb[bank][:, 0:512]
                for cc in range(4):
                    k.tr(tvm[:, cc * 128:(cc + 1) * 128], ym[yb_][:, cc * 128:(cc + 1) * 128], identb,
                         [("ym", yb_), "identb"], [("ps", bank)], partial=(cc > 0))
                k.evac(yt[:, :, tt * 128:(tt + 1) * 128], tvm.rearrange("p (a b) -> p a b", a=4), [("ps", bank)],
                       [("ymTq", Q % 2)], partial=True)
            k.dma("sync", dr["ymemT_d"][:, :, Q * 512:(Q + 1) * 512], yt, [("ymTq", Q % 2)], [("ymemT_d", Q)])
        P.barrier()
        A.reset(base_mark)

        wg = A.alloc([8, 3072], BF16)
        wo3 = [A.alloc([4, 1024], BF16) for _ in range(3)]
        wout = A.alloc([8, 1024], BF16)
        wrt = A.alloc([8, 72], F32)
        bgt = A.alloc([24], F32)
        brt = A.alloc([72], F32); onesrow = A.alloc([128], F32)
        lng = A.alloc([1024], F32); lnb = A.alloc([1024], F32)
        ect = A.alloc([64], F32); mhalf = A.alloc([1], F32)
        atot = A.alloc([64], BF16)
        xTq = [A.alloc([8, 512], BF16) for _ in range(2)]
        ybq = [[A.alloc([4, 512], BF16) for _ in range(2)] for _ in range(3)]
        G = [[A.alloc([512], BF16) for _ in range(2)] for _ in range(3)]
        tm = [A.alloc([512], F32) for _ in range(4)]
        mT = A.alloc([8, 512], BF16)
        xt = [A.alloc([1024], F32) for _ in range(2)]
        rt = [A.alloc([1024], F32) for _ in range(2)]
        ht = [A.alloc([1024], F32) for _ in range(2)]
        hl = [A.alloc([1024], F32) for _ in range(2)]
        hb = [A.alloc([1024], BF16) for _ in range(2)]
        hT = A.alloc([8, 128], F32)
        sm = A.alloc([256], F32)
        sml = [A.alloc([16], F32) for _ in range(2)]
        k.A1 = A.alloc([64], F32); k.A2 = A.alloc([64], F32); k.Ab = A.alloc([64], BF16)
        for cb in range(6):
            k.dma("gpsimd", wg[:, :, cb * 512:(cb + 1) * 512],
                  dr["w_gate"][:, cb * 512:(cb + 1) * 512].rearrange("(c p) n -> p c n", p=128), [], [("wg", cb)])
        for i, nm in enumerate(("w_fo", "w_nao", "w_memo")):
            k.dma("gpsimd", wo3[i], dr[nm].rearrange("(c p) n -> p c n", p=128), [], [("wo3", i)])
        for cb in range(2):
            k.dma("gpsimd", wout[:, :, cb * 512:(cb + 1) * 512],
                  dr["w_out"][:, cb * 512:(cb + 1) * 512].rearrange("(c p) n -> p c n", p=128), [], [("wout", cb)])
        k.dma("sync", wrt, dr["wr"].rearrange("(c p) n -> p c n", p=128), [], ["wrt"])
        k.dma("sync", bgt, dr["bg"], [], ["bgt"])
        k.dma("sync", brt[0:1, :], dr["br"], [], ["brt"])
        k.dma("sync", onesrow[0:1, :], dr["c_onesrow"], [], ["onesrow"])
        k.dma("sync", lng, dr["ln1_g"].partition_broadcast(128)[:, 0, :], [], ["lng"])
        k.dma("sync", lnb, dr["ln1_b"].partition_broadcast(128)[:, 0, :], [], ["lnb"])
        k.dma("sync", ect, dr["c_ec"], [], ["ect"])
        k.dma("sync", mhalf, dr["c_mhalf"], [], ["mhalf"])
        k.v("vector", "memset", (atot, 0.0), [], ["atot"])
        ysrc = ("yfT_d", "ynaT_d", "ymemT_d")

        def c_load(Q):
            k.dma("sync", xTq[Q % 2], dr["xT_d"][Q], [], [("xTq", Q % 2)])
            for i in range(3):
                k.dma("sync", ybq[i][Q % 2], dr[ysrc[i]][:, :, Q * 512:(Q + 1) * 512], [], [("ybq", i, Q % 2)])
        c_load(0)
        wgr = [("wg", cb) for cb in range(6)]

        def r1_load(ti):
            b2 = ti % 2
            k.dma("sync", hl[b2], dr["h_d"][ti * 128:(ti + 1) * 128, :], [("h_d", ti)], [("hl", b2)])

        def r1(ti):
            b2 = ti % 2
            if ti % 4 >= 2:
                r1_load(ti)
            k.act(hb[b2], hl[b2], AF.Copy, [("hl", b2)], [("hb", b2)])
            for hh in range(2):
                for q in range(4):
                    k.tr(ps[6][:, q * 128:(q + 1) * 128], hl[b2][:, (hh * 4 + q) * 128:(hh * 4 + q + 1) * 128], identf,
                         [("hl", b2), "identf"], [("ps", 6)], partial=(q > 0))
                k.evac(hT[:, hh * 4:(hh + 1) * 4, :], ps[6][:].rearrange("p (a b) -> p a b", a=4),
                       [("ps", 6)], [("hT", hh)])

        def r2(ti):
            for fc in range(8):
                k.mm(ps[7][:, 0:72], hT[:, fc, :], wrt[:, fc, :], fc == 0, False,
                     [("hT", 0), ("hT", 1), "wrt"], [("ps", 7)])
            k.mm(ps[7][:, 0:72], onesrow[0:1, :], brt[0:1, :], False, True, ["onesrow", "brt"], [("ps", 7)])

        def r3(ti):
            emit_route(k, ps, sm, ti, slots_i, rw, atot, ustrict, ones_b, ect, 7, 1)

        def r4(ti):
            b2 = ti % 2
            emit_route(k, ps, sm, ti, slots_i, rw, atot, ustrict, ones_b, ect, 7, 2)
            for kk in range(2):
                P.op("gpsimd", (lambda e, ti=ti, kk=kk, b2=b2: e.indirect_dma_start(
                    out=dr["xg_d"][:, :],
                    out_offset=bass.IndirectOffsetOnAxis(ap=slots_i[:, ti, kk:kk + 1], axis=0),
                    in_=hb[b2][:, :], in_offset=None, bounds_check=_bc_reg(e), oob_is_err=False)),
                    reads=[("hb", b2), ("slots", ti)], writes=["xg_d"], dma=True, key=("xg_sc", b2), partial=True)

        def route_sched(Qp):
            t = [Qp * 4 + i for i in range(4)]
            return [[lambda: r1(t[0])], [lambda: r2(t[0]), lambda: r3(t[0])],
                    [lambda: r4(t[0]), lambda: r1(t[1])], [lambda: r2(t[1]), lambda: r3(t[1])],
                    [lambda: r4(t[1]), lambda: r1(t[2])], [lambda: r2(t[2]), lambda: r3(t[2])],
                    [lambda: r4(t[2]), lambda: r1(t[3])], [lambda: r2(t[3]), lambda: r3(t[3])],
                    [lambda: r4(t[3])]]
        for Q in range(9):
            sched = route_sched(Q - 1) if Q >= 1 else [[] for _ in range(9)]
            if Q < 8:
                if Q + 1 < 8:
                    c_load(Q + 1)
                xq = xTq[Q % 2]
            for dc in range(8):
                if Q < 8:
                    k.mmi([[(ps[i][:], wg[:, fc, i * 1024 + dc * 128:i * 1024 + (dc + 1) * 128], xq[:, fc, :],
                             wgr + [("xTq", Q % 2)], [("ps", i)]) for fc in range(8)] for i in range(3)])
                    for i in range(3):
                        k.act(G[i][dc % 2], ps[i][:], AF.Sigmoid, [("ps", i), "bgt"], [("G", i, dc % 2)],
                              bias=bgt[:, i * 8 + dc:i * 8 + dc + 1])
                    k.mmi([[(ps[3 + i][:], wo3[i][:, cc, dc * 128:(dc + 1) * 128], ybq[i][Q % 2][:, cc, :],
                             [("wo3", i), ("ybq", i, Q % 2)], [("ps", 3 + i)]) for cc in range(4)] for i in range(3)])
                if Q < 8:
                    for i in range(3):
                        k.v("vector", "tensor_tensor", (tm[i], G[i][dc % 2], ps[3 + i][:], ALU.mult),
                            [("G", i, dc % 2), ("ps", 3 + i)], [("tm", i)])
                    k.v("gpsimd", "tensor_tensor", (tm[3], tm[0], tm[1], ALU.add), [("tm", 0), ("tm", 1)], [("tm", 3)])
                    k.v("gpsimd", "tensor_tensor", (mT[:, dc, :], tm[3], tm[2], ALU.add), [("tm", 3), ("tm", 2)],
                        [("mT", dc)])
                for f_ in sched[dc]:
                    f_()
            if Q == 8:
                for f_ in sched[8]:
                    f_()
                break
            mTr = [("mT", dc) for dc in range(8)]
            k.dma("sync", xt[0], xo_t[Q * 4], [], [("xt", 0)])

            def s1a(tt):
                ti = Q * 4 + tt
                b2 = ti % 2
                if tt + 1 < 4:
                    k.dma("sync", xt[(tt + 1) % 2], xo_t[ti + 1], [], [("xt", (tt + 1) % 2)])
                pbk = (6, 7) if tt % 2 == 0 else (4, 5)
                k.mmi([[(ps[pbk[half]][:], mT[:, dc, tt * 128:(tt + 1) * 128], wout[:, dc, half * 512:(half + 1) * 512],
                         mTr + [("wout", half)], [("ps", pbk[half])]) for dc in range(8)] for half in range(2)])
                for half in range(2):
                    pm = pbk[half]
                    k.v("vector", "scalar_tensor_tensor",
                        (rt[b2][:, half * 512:(half + 1) * 512], xt[tt % 2][:, half * 512:(half + 1) * 512], ALPHA,
                         ps[pm][:], ALU.mult, ALU.add),
                        [("xt", tt % 2), ("ps", pm)], [("rt", b2, half)])
                emit_ln_a(k, rt[b2], [("rt", b2, 0), ("rt", b2, 1)], sml[b2], mhalf, tag=b2)

            def s1b(tt):
                ti = Q * 4 + tt
                b2 = ti % 2
                emit_ln_b(k, rt[b2], [("rt", b2, 0), ("rt", b2, 1)], ht[b2], ("ht", b2), lng, lnb, ["lng", "lnb"],
                          sml[b2], tag=b2)
                k.dma("sync", dr["h_d"][ti * 128:(ti + 1) * 128, :], ht[b2], [("ht", b2)], [("h_d", ti)])
            s1a(0)
            s1a(1)
            s1b(0)
            r1_load(Q * 4)
            s1a(2)
            s1b(1)
            r1_load(Q * 4 + 1)
            s1a(3)
            s1b(2)
            s1b(3)
            for f_ in sched[8]:
                f_()
        P.barrier()
        A.reset(base_mark)
        if debug:
            dbgt = A.alloc([32, 4], F32)
            k.v("vector", "tensor_copy", (dbgt[:, :, 0:2], slots_i), [], ["dbgt0"])
            k.v("vector", "tensor_copy", (dbgt[:, :, 2:4], rw), [], ["dbgt1"])
            k.dma("sync", dr["dbg_rt"], dbgt, ["dbgt0", "dbgt1"], ["dbg_rt"])
            k.dma("sync", dr["dbg_h"], dr["h_d"], [], ["dbg_h"])
            for i_, nm_ in enumerate(("yfT_d", "ynaT_d", "ymemT_d")):
                k.dma("sync", dr["dbg_y3"][i_], dr[nm_], [], [("dbg_y3", i_)])

        weg = [A.alloc([8, 512], BF16) for _ in range(3)]
        weu = [A.alloc([8, 512], BF16) for _ in range(3)]
        wed = [A.alloc([4, 1024], BF16) for _ in range(3)]
        xg = [A.alloc([2, 1024], BF16) for _ in range(3)]
        xgT = [A.alloc([8, 256], BF16) for _ in range(2)]
        sg = [A.alloc([256], F32) for _ in range(2)]
        aT = [A.alloc([4, 256], BF16) for _ in range(2)]
        yo = [A.alloc([1024], BF16) for _ in range(2)]

        def e_load(e_):
            s = e_ % 3
            k.dma("gpsimd", weg[s], dr["w_eg"][e_].rearrange("(c p) n -> p c n", p=128), [], [("weg", s)])
            k.dma("gpsimd", weu[s], dr["w_eu"][e_].rearrange("(c p) n -> p c n", p=128), [], [("weu", s)])
            k.dma("gpsimd", wed[s], dr["w_ed"][e_].rearrange("(c p) n -> p c n", p=128), [], [("wed", s)])
            k.dma("sync", xg[e_ % 3], dr["xg_d"][e_ * CAP:(e_ + 1) * CAP, :].rearrange("(t p) f -> p t f", p=128),
                  ["xg_d"], [("xg", e_ % 3)])
        e_load(0); e_load(1)

        def e_transposes(ee):
            xgt_ = xgT[ee % 2]
            k.tri([[(psb[tt][:, fc * 128:(fc + 1) * 128], xg[ee % 3][:, tt, fc * 128:(fc + 1) * 128], identb,
                     [("xg", ee % 3), "identb"], [("ps", tt)]) for fc in range(8)] for tt in range(2)])
            for tt in range(2):
                k.evac(xgt_[:, :, tt * 128:(tt + 1) * 128], psb[tt][:].rearrange("p (a b) -> p a b", a=8),
                       [("ps", tt)], [("xgT", ee % 2)], partial=True)
        yc = 0
        for e_ in range(NEXP):
            if e_ + 2 < NEXP:
                e_load(e_ + 2)
            s = e_ % 3
            xgt = xgT[e_ % 2]
            if e_ == 0:
                e_transposes(0)
            at = aT[e_ % 2]
            for dcn in range(4):
                pg = 2 + (dcn % 2) * 2; pu = pg + 1
                k.mmi([[(ps[pg][:, 0:256], weg[s][:, fc, dcn * 128:(dcn + 1) * 128], xgt[:, fc, :],
                         [("weg", s), ("xgT", e_ % 2)], [("ps", pg)]) for fc in range(8)],
                       [(ps[pu][:, 0:256], weu[s][:, fc, dcn * 128:(dcn + 1) * 128], xgt[:, fc, :],
                         [("weu", s), ("xgT", e_ % 2)], [("ps", pu)]) for fc in range(8)]])
                k.act(sg[dcn % 2], ps[pg][:, 0:256], AF.Silu, [("ps", pg)], [("sg", dcn % 2)])
                k.v("vector", "tensor_tensor", (at[:, dcn, :], sg[dcn % 2], ps[pu][:, 0:256], ALU.mult),
                    [("sg", dcn % 2), ("ps", pu)], [("aT", e_ % 2, dcn)])
            atr = [("aT", e_ % 2, dcn) for dcn in range(4)]
            if e_ + 1 < NEXP:
                e_transposes(e_ + 1)
            for tt in range(2):
                yt = yo[yc % 2]
                k.mmi([[(ps[6 + half][:], at[:, dcn, tt * 128:(tt + 1) * 128], wed[s][:, dcn, half * 512:(half + 1) * 512],
                         atr + [("wed", s)], [("ps", 6 + half)]) for dcn in range(4)] for half in range(2)])
                for half in range(2):
                    pd = 6 + half
                    k.evac(yt[:, half * 512:(half + 1) * 512], ps[pd][:], [("ps", pd)], [("yo", yc % 2, half)])
                k.dma("sync", dr["yb_d"][e_ * CAP + tt * 128:e_ * CAP + (tt + 1) * 128, :], yt,
                      [("yo", yc % 2, 0), ("yo", yc % 2, 1)], ["yb_d"], key=("yo_st", yc % 2), partial=True)
                yc += 1
        P.barrier()
        A.reset(base_mark)

        lng = A.alloc([1024], F32); lnb = A.alloc([1024], F32); mhalf = A.alloc([1], F32)
        sm = A.alloc([256], F32)
        NB = 4
        hd = [A.alloc([1024], F32) for _ in range(NB)]
        g1 = [A.alloc([1024], BF16) for _ in range(NB)]
        g2 = [A.alloc([1024], BF16) for _ in range(NB)]
        acc = [A.alloc([1024], F32) for _ in range(NB)]
        ot = [A.alloc([1024], F32) for _ in range(NB)]
        smd = [A.alloc([32], F32) for _ in range(NB)]
        k.dma("sync", lng, dr["ln2_g"].partition_broadcast(128)[:, 0, :], [], ["lng"])
        k.dma("sync", lnb, dr["ln2_b"].partition_broadcast(128)[:, 0, :], [], ["lnb"])
        k.dma("sync", mhalf, dr["c_mhalf"], [], ["mhalf"])

        def d_load(ti):
            b2 = ti % NB
            k.dma("sync", hd[b2], dr["h_d"][ti * 128:(ti + 1) * 128, :], [], [("hd", b2)])
            for kk, gt in enumerate((g1, g2)):
                P.op("gpsimd", (lambda e, ti=ti, kk=kk, gt=gt, b2=b2: e.indirect_dma_start(
                    out=gt[b2][:, :], out_offset=None, in_=dr["yb_d"][:, :],
                    in_offset=bass.IndirectOffsetOnAxis(ap=slots_i[:, ti, kk:kk + 1], axis=0),
                    bounds_check=_bc_reg(e), oob_is_err=False)),
                    reads=[], writes=[("g", kk, b2)], dma=True)
        d_load(0); d_load(1)

        def da(ti):
            if ti + 2 < 32:
                d_load(ti + 2)
            b2 = ti % NB
            k.act(acc[b2], hd[b2], AF.Copy, [("hd", b2)], [("acc", b2)], scale=ALPHA)
            k.v("vector", "scalar_tensor_tensor", (acc[b2], g1[b2], rw[:, ti, 0:1], acc[b2], ALU.mult, ALU.add),
                [("g", 0, b2), ("acc", b2)], [("acc", b2)])
            k.v("vector", "scalar_tensor_tensor", (acc[b2], g2[b2], rw[:, ti, 1:2], acc[b2], ALU.mult, ALU.add),
                [("g", 1, b2), ("acc", b2)], [("acc", b2)])
            emit_ln_a(k, acc[b2], [("acc", b2)], smd[b2], mhalf, tag=b2)

        def db(ti):
            b2 = ti % NB
            emit_ln_b(k, acc[b2], [("acc", b2)], ot[b2], ("ot", b2), lng, lnb, ["lng", "lnb"], smd[b2], tag=b2)
            k.dma("sync", dr["out"][ti * 128:(ti + 1) * 128, :], ot[b2], [("ot", b2)], [("out", ti)])
        da(0)
        for ti in range(32):
            if ti + 1 < 32:
                da(ti + 1)
            db(ti)
        P.emit()
    return nc


def emit_ln_a(k, src, src_names, sm, mhalf, tag=0):
    st = sm[:, 0:12].rearrange("p (a b) -> p a b", a=2)
    mv = sm[:, 12:14]
    rs = sm[:, 14:15]
    nb = sm[:, 15:16]
    T = lambda x: (x, tag)
    for half in range(2):
        k.v("vector", "bn_stats", (st[:, half, :], src[:, half * 512:(half + 1) * 512]), src_names,
            [T("lnst%d" % half)])
    k.v("vector", "bn_aggr", (mv, st), [T("lnst0"), T("lnst1")], [T("lnmv")])
    k.v("gpsimd", "tensor_scalar", (rs, mv[:, 1:2], LN_EPS, 1.0, ALU.add, ALU.mult), [T("lnmv")], [T("lnrs0")])
    k.v("gpsimd", "tensor_tensor", (rs, rs, mhalf, ALU.pow), [T("lnrs0"), "mhalf"], [T("lnrs")])
    k.v("gpsimd", "tensor_scalar", (nb, mv[:, 0:1], rs, -1.0, ALU.mult, ALU.mult), [T("lnmv"), T("lnrs")],
        [T("lnnb")])


def emit_ln_b(k, src, src_names, dst, dst_name, g, b, gb_names, sm, tag=0):
    rs = sm[:, 14:15]
    nb = sm[:, 15:16]
    T = lambda x: (x, tag)
    k.act(dst, src, AF.Identity, src_names + [T("lnrs"), T("lnnb")], [dst_name + ("n",), dst_name], bias=nb, scale=rs)
    k.v("gpsimd", "tensor_tensor", (dst[:, 0:512], dst[:, 0:512], g[:, 0:512], ALU.mult),
        [dst_name + ("n",), gb_names[0]], [dst_name + ("g0",)])
    k.v("vector", "tensor_tensor", (dst[:, 512:1024], dst[:, 512:1024], g[:, 512:1024], ALU.mult),
        [dst_name + ("n",), gb_names[0]], [dst_name + ("g1",)])
    k.v("vector", "tensor_tensor", (dst, dst, b, ALU.add), [dst_name + ("g0",), dst_name + ("g1",), gb_names[1]],
        [dst_name])


def emit_route(k, ps, sm, ti, slots_i, rw, atot, ustrict, ones_b, ect, pl=2, stage=0):
    L = sm[:, 16:88]
    gm8 = sm[:, 88:96]
    ohg = sm[:, 96:104]
    dd = sm[:, 104:112]
    pg = sm[:, 112:113]
    el = sm[:, 113:121]
    tm8 = sm[:, 121:129]
    oh1 = sm[:, 129:137]
    oh2 = sm[:, 137:145]
    dv = sm[:, 145:146]
    t64 = sm[:, 146:210]
    sl = sm[:, 210:212]
    n = lambda s: ("rt_" + s,)
    V = lambda name, args, r, w: k.v("vector", name, args, r, w)
    if stage == 2:
        return _route_p2(k, ps, sm, ti, slots_i, atot, ustrict, ones_b, ect, pl)
    V("tensor_copy", (L, ps[pl][:, 0:72]), [("ps", pl)], [n("L")])
    V("max", (gm8, L[:, 0:8]), [n("L")], [n("gm8")])
    V("tensor_scalar", (ohg, L[:, 0:8], gm8[:, 0:1], None, ALU.is_equal), [n("L"), n("gm8")], [n("ohg")])
    V("tensor_scalar", (dd, L[:, 0:8], gm8[:, 0:1], None, ALU.subtract), [n("L"), n("gm8")], [n("dd")])
    k.act(dd, dd, AF.Sigmoid, [n("dd")], [n("dd")])
    V("tensor_scalar", (tm8, dd, -1.0, 1.0, ALU.mult, ALU.add), [n("dd")], [n("tm8")])
    V("reciprocal", (tm8, tm8), [n("tm8")], [n("tm8")])
    V("tensor_tensor", (dd, dd, tm8, ALU.mult), [n("dd"), n("tm8")], [n("dd")])
    V("tensor_reduce", (pg, dd, AX.X, ALU.add), [n("dd")], [n("pg")])
    V("reciprocal", (pg, pg), [n("pg")], [n("pg")])
    t3 = t64.rearrange("p (g e) -> p g e", g=8)
    L3 = L[:, 8:72].rearrange("p (g e) -> p g e", g=8)
    V("tensor_tensor", (t3, L3, ohg.unsqueeze(2).to_broadcast([128, 8, 8]), ALU.mult), [n("L"), n("ohg")], [n("t64")])
    V("tensor_reduce", (el, t64.rearrange("p (g e) -> p e g", g=8), AX.X, ALU.add), [n("t64")], [n("el")])
    V("max", (tm8, el), [n("el"), n("tm8")], [n("tm8")])
    V("tensor_scalar", (oh1, el, tm8[:, 0:1], None, ALU.is_equal), [n("el"), n("tm8")], [n("oh1")])
    V("tensor_scalar", (oh2, el, tm8[:, 1:2], None, ALU.is_equal), [n("el"), n("tm8")], [n("oh2")])
    V("tensor_tensor", (dv, tm8[:, 0:1], tm8[:, 1:2], ALU.subtract), [n("tm8")], [n("dv")])
    k.act(dv, dv, AF.Sigmoid, [n("dv")], [n("dv")])
    V("tensor_tensor", (rw[:, ti, 0:1], dv, pg, ALU.mult), [n("dv"), n("pg")], [("rw", ti, 0)])
    V("tensor_tensor", (rw[:, ti, 1:2], pg, rw[:, ti, 0:1], ALU.subtract), [n("pg"), ("rw", ti, 0)], [("rw", ti, 1)])
    A1 = k.A1
    A2 = k.A2
    Ab = k.Ab
    A13 = A1.rearrange("p (g e) -> p g e", g=8)
    A23 = A2.rearrange("p (g e) -> p g e", g=8)
    gb = ohg.unsqueeze(2).to_broadcast([128, 8, 8])
    V("tensor_tensor", (A13, gb, oh1.unsqueeze(1).to_broadcast([128, 8, 8]), ALU.mult), [n("ohg"), n("oh1")], [n("A1")])
    V("tensor_tensor", (A23, gb, oh2.unsqueeze(1).to_broadcast([128, 8, 8]), ALU.mult), [n("ohg"), n("oh2")], [n("A2")])
    V("tensor_tensor", (Ab, A1, A2, ALU.add), [n("A1"), n("A2")], [n("Ab")])
    if stage == 1:
        return
    _route_p2(k, ps, sm, ti, slots_i, atot, ustrict, ones_b, ect, pl)


def _route_p2(k, ps, sm, ti, slots_i, atot, ustrict, ones_b, ect, pl):
    t64 = sm[:, 146:210]
    sl = sm[:, 210:212]
    n = lambda s: ("rt_" + s,)
    V = lambda name, args, r, w: k.v("vector", name, args, r, w)
    A1 = k.A1
    A2 = k.A2
    Ab = k.Ab
    k.mm(ps[pl][:, 128:192], ustrict, Ab, True, False, ["ustrict", n("Ab")], [("ps", pl)])
    k.mm(ps[pl][:, 128:192], ones_b, atot, False, True, ["ones_b", "atot"], [("ps", pl)])
    V("tensor_tensor", (t64, ps[pl][:, 128:192], ect, ALU.add), [("ps", pl), "ect"], [n("t64")])
    V("tensor_tensor", (atot, atot, Ab, ALU.add), [n("Ab"), "atot"], ["atot"])
    V("tensor_tensor", (A1, A1, t64, ALU.mult), [n("A1"), n("t64")], [n("A1")])
    V("tensor_tensor", (A2, A2, t64, ALU.mult), [n("A2"), n("t64")], [n("A2")])
    V("tensor_reduce", (sl[:, 0:1], A1, AX.X, ALU.add), [n("A1")], [n("sl0")])
    V("tensor_reduce", (sl[:, 1:2], A2, AX.X, ALU.add), [n("A2")], [n("sl1")])
    V("tensor_copy", (slots_i[:, ti, :], sl), [n("sl0"), n("sl1")], [("slots", ti)])


_NC_CACHE = {}


def _consts(hf):
    bf = ml_dtypes.bfloat16
    c = {}
    c["c_identf"] = np.eye(128, dtype=np.float32)
    c["c_identb"] = np.eye(128, dtype=np.float32).astype(bf)
    c["c_ones"] = np.ones((128, 128), np.float32).astype(bf)
    c["c_ustrict"] = np.triu(np.ones((128, 128), np.float32), 1).astype(bf)
    row = np.arange(128)
    k2 = np.arange(128)
    ed = np.zeros((64, 128, 256), np.float32)
    for j in range(64):
        s = j + 64 * row
        th = 2.0 * np.pi * ((s[:, None] * k2[None, :]) % 8192) / 8192.0
        ed[j, :, 0:128] = np.cos(th) / 32.0
        ed[j, :, 128:256] = np.sin(th) / 32.0
    c["c_edft"] = ed.astype(bf)
    ph = 2.0 * np.pi * ((row[:, None] * row[None, :]) % 128) / 128.0
    C = np.cos(ph); S = np.sin(ph)
    c["c_fca"] = np.concatenate([C, -S], axis=1).astype(np.float32).astype(bf)
    c["c_fcb"] = np.concatenate([-S, -C], axis=1).astype(np.float32).astype(bf)
    s1 = np.arange(64)
    k1 = 32 * hf + np.arange(32)
    psi = 2.0 * np.pi * ((s1[:, None] * k1[None, :]) % 64) / 64.0
    bdc = np.zeros((128, 64), np.float32); bds = np.zeros((128, 64), np.float32)
    for a in range(2):
        bdc[a::2, a * 32:(a + 1) * 32] = np.cos(psi) / 32.0
        bds[a::2, a * 32:(a + 1) * 32] = np.sin(psi) / 32.0
    c["c_bdc"] = bdc.astype(bf); c["c_bds"] = bds.astype(bf)
    c["c_ec"] = np.tile((np.arange(64, dtype=np.float32) * CAP)[None, :], (128, 1)).astype(np.float32)
    c["c_mhalf"] = np.full((128, 1), -0.5, np.float32)
    c["c_onesrow"] = np.ones((1, 128), np.float32)
    return c


def _bias_tiles(rpb, hf):
    out = np.full((5, 8, 128, 768), MASKV, np.float32)
    specs = [(10, [-2, -1, 0, 1, 2]), (0, [-2, -1, 0, 1, 2, 3]), (1, [-2, -1, 0, 1, 2]),
             (30, [-2, -1, 0, 1, 2]), (31, [-3, -2, -1, 0, 1, 2])]
    qc = np.arange(64); kc = np.arange(64)
    cs = np.clip(qc - 8, 0, 48)
    colvalid = (kc[:, None] >= cs[None, :]) & (kc[:, None] < cs[None, :] + 16)
    dc = kc[:, None] - qc[None, :] + 15
    dcc = np.clip(dc, 0, 30)
    for ty, (qp, offs) in enumerate(specs):
        for j, off in enumerate(offs):
            kp = qp + off
            for aq in range(2):
                r = 64 * hf + 2 * qp + aq
                rs = min(max(r - 4, 0), 120)
                for ak in range(2):
                    kr = 64 * hf + 2 * kp + ak
                    if kr < 0 or kr > 127 or kr < rs or kr > rs + 7:
                        continue
                    drr = kr - r + 7
                    vals = rpb[:, drr, :][:, dcc]
                    blk = np.where(colvalid[None], vals, np.float32(MASKV))
                    out[ty, :, ak * 64:(ak + 1) * 64, j * 128 + aq * 64:j * 128 + (aq + 1) * 64] = blk
    return out


def kernel(x, mem, w_in, w_gate, b_gate, w_mem_kv, rpb, w_fourier_o, w_na_o, w_mem_o, w_out, ln1_g, ln1_b,
           w_router_group, b_router_group, w_router_expert, b_router_expert, w_exp_gate, w_exp_up, w_exp_down,
           ln2_g, ln2_b, _debug=False):
    f = lambda a: np.ascontiguousarray(np.asarray(a, dtype=np.float32))
    x = f(x); mem = f(mem)
    key = bool(_debug)
    if key not in _NC_CACHE:
        _NC_CACHE[key] = build_program(debug=_debug)
    nc = _NC_CACHE[key]
    shared = {
        "w_in": f(w_in[0]), "w_gate": f(w_gate[0]),
        "bg": f(np.asarray(b_gate[0]).reshape(24, 128).T),
        "w_kv": f(w_mem_kv[0]), "w_fo": f(w_fourier_o[0]), "w_nao": f(w_na_o[0]), "w_memo": f(w_mem_o[0]),
        "w_out": f(w_out[0]), "ln1_g": f(ln1_g), "ln1_b": f(ln1_b), "ln2_g": f(ln2_g), "ln2_b": f(ln2_b),
        "wr": f(np.concatenate([np.asarray(w_router_group[0]), np.asarray(w_router_expert[0])], axis=1)),
        "br": f(np.concatenate([np.asarray(b_router_group[0]), np.asarray(b_router_expert[0])])[None, :]),
        "w_eg": f(w_exp_gate[0]), "w_eu": f(w_exp_up[0]), "w_ed": f(w_exp_down[0]),
    }
    rp = f(rpb[0])
    in_maps = []
    for c in range(NCORES):
        b, hf = c // 2, c % 2
        m = dict(shared)
        m.update(_consts(hf))
        m["xf"] = x[b]
        xo = np.zeros((4608, D), np.float32)
        xo[0:4096] = x[b, 4096 * hf:4096 * hf + 4096]
        if hf == 1:
            xo[4096:4352] = x[b, 4096 - 256:4096]
        else:
            xo[4352:4608] = x[b, 4096:4096 + 256]
        m["xo"] = xo
        m["mem"] = mem[b]
        m["biasT"] = _bias_tiles(rp, hf)
        in_maps.append(m)
    res = run_bass_kernel_spmd(nc, in_maps, core_ids=list(range(NCORES)))
    out = np.zeros((4, 8192, D), np.float32)
    for c in range(NCORES):
        b, hf = c // 2, c % 2
        out[b, 4096 * hf:4096 * hf + 4096] = res.results[c]["out"]
    if _debug:
        return out, res
    return out
```

```python
import contextlib
import math
import numpy as np
import ml_dtypes
import concourse.bass as bass
import concourse.mybir as mybir
from concourse.bass_utils import run_bass_kernel_spmd

F32 = mybir.dt.float32
BF16 = mybir.dt.bfloat16
I32 = mybir.dt.int32
U8 = mybir.dt.uint8
ALU = mybir.AluOpType
AF = mybir.ActivationFunctionType
AX = mybir.AxisListType

ENGS = ("tensor", "vector", "scalar", "gpsimd", "sync")
NCORES = 8
D = 1024
CAP = 256
NEXP = 64
NSLOT = NEXP * CAP
ALPHA = 2.0 ** 0.25
LN_EPS = 1e-5
MASKV = -30000.0
ARENA = 206 * 1024


class Op:
    __slots__ = ("eng", "fn", "dma", "deps", "sig", "sigval", "key", "dval", "slot")

    def __init__(self, eng, fn, dma):
        self.eng = eng
        self.fn = fn
        self.dma = dma
        self.deps = []
        self.sig = False
        self.sigval = 0
        self.key = None
        self.dval = 0
        self.slot = None


class Prog:
    def __init__(self, nc):
        self.nc = nc
        self.ops = {e: [] for e in ENGS}
        self.writers = {}
        self.readers = {}
        self.key_slot = {}
        self.slot_count = []
        self.nops = 0

    def op(self, eng, fn, reads=(), writes=(), dma=False, key=None, partial=False):
        o = Op(eng, fn, dma)
        self.nops += 1
        deps = {}
        banks = set()
        for b in list(reads) + list(writes):
            if isinstance(b, tuple) and b and b[0] == "ps":
                banks.add(b[1])
        for bk in banks:
            nm = ("psbank", bk)
            for w in self.writers.get(nm, ()):
                deps[id(w)] = w
            self.writers[nm] = [o]
        for b in reads:
            for w in self.writers.get(b, ()):
                deps[id(w)] = w
        for b in writes:
            if not partial:
                for w in self.writers.get(b, ()):
                    deps[id(w)] = w
            for r in self.readers.get(b, ()):
                deps[id(r)] = r
        for b in reads:
            self.readers.setdefault(b, []).append(o)
        for b in writes:
            if partial:
                self.writers.setdefault(b, []).append(o)
            else:
                self.writers[b] = [o]
                self.readers[b] = []
        deps.pop(id(o), None)
        o.deps = list(deps.values())
        if dma:
            if key is None:
                key = writes[0] if writes else reads[0]
            if key not in self.key_slot:
                self.key_slot[key] = len(self.key_slot)
                if len(self.key_slot) > len(self.slot_count):
                    self.slot_count.append(0)
            s = self.key_slot[key]
            self.slot_count[s] += 1
            o.slot = s
            o.dval = 16 * self.slot_count[s]
        self.ops[eng].append(o)
        return o

    def barrier(self):
        lasts = []
        for e in ENGS:
            nd = [o for o in self.ops[e] if not o.dma and o.fn is not None]
            if nd:
                lasts.append(nd[-1])
        dmas = {}
        for e in ENGS:
            for o in self.ops[e]:
                if o.dma and (o.slot not in dmas or dmas[o.slot].dval < o.dval):
                    dmas[o.slot] = o
        for e in ENGS:
            o = Op(e, None, False)
            o.deps = [l for l in lasts if l.eng != e] + list(dmas.values())
            self.ops[e].append(o)
        self.writers = {}
        self.readers = {}
        self.key_slot = {}

    def emit(self):
        nc = self.nc
        for e in ENGS:
            for o in self.ops[e]:
                for d in o.deps:
                    if not d.dma and not (d.eng == "tensor" and o.eng == "tensor"):
                        d.sig = True
        for e in ENGS:
            c = 0
            for o in self.ops[e]:
                if o.sig:
                    c += 1
                    o.sigval = c
        nslots = len(self.slot_count)
        with contextlib.ExitStack() as st:
            esem = {e: st.enter_context(nc.semaphore("e_" + e)) for e in ENGS}
            ksem = [st.enter_context(nc.semaphore("d%d" % i)) for i in range(nslots)]
            block = st.enter_context(nc.Block())

            def body(e):
                def _f(eng):
                    waited = {}
                    for o in self.ops[e]:
                        for d in o.deps:
                            if d.dma:
                                s, v = ksem[d.slot], d.dval
                            elif d.sig:
                                s, v = esem[d.eng], d.sigval
                            else:
                                continue
                            if waited.get(id(s), 0) >= v:
                                continue
                            waited[id(s)] = v
                            eng.wait_ge(s, v)
                        if o.fn is None:
                            continue
                        ins = o.fn(eng)
                        if o.dma:
                            ins.then_inc(ksem[o.slot], 16)
                        elif o.sig:
                            ins.then_inc(esem[e], 1)
                    if e == "sync":
                        for i in range(nslots):
                            v = 16 * self.slot_count[i]
                            if waited.get(id(ksem[i]), 0) < v:
                                eng.wait_ge(ksem[i], v)
                return _f

            block.sync(body("sync"))
            block.tensor(body("tensor"))
            block.vector(body("vector"))
            block.scalar(body("scalar"))
            block.gpsimd(body("gpsimd"))


def _isz(dt):
    return {F32: 4, BF16: 2, I32: 4, U8: 1}[dt]


class Arena:
    def __init__(self, ap, size):
        self.ap = ap
        self.size = size
        self.off = 0

    def alloc(self, shape, dt):
        n = int(np.prod(shape))
        nb = n * _isz(dt)
        off = self.off
        self.off += (nb + 63) // 64 * 64
        assert self.off <= self.size, ("arena overflow", self.off, self.size)
        v = self.ap[:, off:off + nb].bitcast(dt)
        if len(shape) == 2:
            v = v.rearrange("p (a b) -> p a b", a=shape[0], b=shape[1])
        elif len(shape) == 3:
            v = v.rearrange("p (a b c) -> p a b c", a=shape[0], b=shape[1], c=shape[2])
        elif len(shape) == 4:
            v = v.rearrange("p (a b c d) -> p a b c d", a=shape[0], b=shape[1], c=shape[2], d=shape[3])
        return v

    def mark(self):
        return self.off

    def reset(self, m):
        self.off = m


class K:
    def __init__(self, nc, P):
        self.nc = nc
        self.P = P
        self.ev = 0

    def mm(self, out, lhsT, rhs, start, stop, reads, writes):
        self.P.op("tensor", lambda e: e.matmul(out, lhsT, rhs, start=start, stop=stop),
                  reads=reads, writes=writes, partial=not start)

    def mmi(self, groups):
        n = max(len(g) for g in groups)
        for i in range(n):
            for g in groups:
                if i < len(g):
                    out, lhsT, rhs, reads, writes = g[i]
                    self.mm(out, lhsT, rhs, i == 0, i == len(g) - 1, reads, writes)

    def tri(self, groups):
        n = max(len(g) for g in groups)
        for i in range(n):
            for g in groups:
                if i < len(g):
                    out, in_, ident, reads, writes = g[i]
                    self.tr(out, in_, ident, reads, writes, partial=(i > 0))

    def tr(self, out, in_, ident, reads, writes, partial=True):
        self.P.op("tensor", lambda e: e.transpose(out, in_, ident), reads=reads, writes=writes,
                  partial=partial)

    def act(self, out, in_, func, reads, writes, bias=None, scale=None, partial=False):
        def f(e):
            kw = {}
            if bias is not None:
                kw["bias"] = bias
            if scale is not None:
                kw["scale"] = scale
            return e.activation(out, in_, func, **kw)
        self.P.op("scalar", f, reads=reads, writes=writes, partial=partial)

    def evac(self, out, in_, reads, writes, eng=None, partial=False):
        if eng is None:
            self.ev += 1
            eng = "vector" if self.ev % 2 else "scalar"
        if eng == "vector":
            self.P.op("vector", lambda e: e.tensor_copy(out, in_), reads=reads, writes=writes, partial=partial)
        else:
            self.P.op("scalar", lambda e: e.activation(out, in_, AF.Copy), reads=reads, writes=writes,
                      partial=partial)

    def v(self, eng, name, args, reads, writes, kw=None, partial=False):
        kw = kw or {}
        self.P.op(eng, lambda e: getattr(e, name)(*args, **kw), reads=reads, writes=writes, partial=partial)

    def dma(self, q, out, in_, reads, writes, key=None, partial=False):
        self.P.op(q, lambda e: e.dma_start(out=out, in_=in_), reads=reads, writes=writes, dma=True,
                  key=key, partial=partial)


def build_program(debug=False):
    nc = bass.Bass("TRN2", target_bir_lowering=False)
    _bc = {}

    def _bc_reg(e):
        if "r" not in _bc:
            _bc["r"] = e.to_reg(NSLOT - 1)
        return _bc["r"]
    dr = {}

    def din(name, shape, dt=F32):
        dr[name] = nc.dram_tensor(name, list(shape), dt, kind="ExternalInput").ap()

    def dscr(name, shape, dt):
        dr[name] = nc.dram_tensor(name, list(shape), dt, kind="Internal").ap()

    din("xf", [8192, D]); din("xo", [4608, D]); din("mem", [256, D])
    din("w_in", [D, 2560]); din("w_gate", [D, 3072]); din("bg", [128, 24])
    din("w_kv", [D, 1024]); din("w_fo", [512, D]); din("w_nao", [512, D]); din("w_memo", [512, D])
    din("w_out", [D, D]); din("ln1_g", [1, D]); din("ln1_b", [1, D]); din("ln2_g", [1, D]); din("ln2_b", [1, D])
    din("wr", [D, 72]); din("br", [1, 72])
    din("w_eg", [NEXP, D, 512]); din("w_eu", [NEXP, D, 512]); din("w_ed", [NEXP, 512, D])
    din("biasT", [5, 8, 128, 768])
    din("c_identf", [128, 128]); din("c_identb", [128, 128], BF16); din("c_ones", [128, 128], BF16)
    din("c_ustrict", [128, 128], BF16); din("c_edft", [64, 128, 256], BF16)
    din("c_fca", [128, 256], BF16); din("c_fcb", [128, 256], BF16)
    din("c_bdc", [128, 64], BF16); din("c_bds", [128, 64], BF16)
    din("c_ec", [128, 64]); din("c_mhalf", [128, 1]); din("c_onesrow", [1, 128])
    dr["out"] = nc.dram_tensor("out", [4096, D], F32, kind="ExternalOutput").ap()
    dscr("yfT_d", [128, 4, 4096], BF16); dscr("ynaT_d", [128, 4, 4096], BF16); dscr("ymemT_d", [128, 4, 4096], BF16)
    dscr("xT_d", [8, 128, 8, 512], BF16); dscr("qmT_d", [8, 128, 4, 512], BF16)
    dscr("h_d", [4096, D], F32); dscr("xg_d", [NSLOT, D], BF16); dscr("yb_d", [NSLOT, D], BF16)
    if debug:
        dr["dbg_h"] = nc.dram_tensor("dbg_h", [4096, D], F32, kind="ExternalOutput").ap()
        dr["dbg_y3"] = nc.dram_tensor("dbg_y3", [3, 128, 4, 4096], BF16, kind="ExternalOutput").ap()
        dr["dbg_rt"] = nc.dram_tensor("dbg_rt", [128, 32, 4], F32, kind="ExternalOutput").ap()

    P = Prog(nc)
    k = K(nc, P)
    with contextlib.ExitStack() as st:
        arena_t = st.enter_context(nc.sbuf_tensor("arena", [128, ARENA], U8))
        A = Arena(arena_t, ARENA)
        ps = [st.enter_context(nc.psum_tensor("ps%d" % i, [128, 512], F32)) for i in range(8)]
        psb = [p[:].bitcast(BF16) for p in ps]

        identf = A.alloc([128], F32); identb = A.alloc([128], BF16); ones_b = A.alloc([128], BF16)
        ustrict = A.alloc([128], BF16)
        slots_i = A.alloc([32, 2], I32); rw = A.alloc([32, 2], F32)
        k.dma("sync", identf, dr["c_identf"], [], ["identf"])
        k.dma("sync", identb, dr["c_identb"], [], ["identb"])
        k.dma("sync", ones_b, dr["c_ones"], [], ["ones_b"])
        k.dma("sync", ustrict, dr["c_ustrict"], [], ["ustrict"])
        base_mark = A.mark()

        wf = A.alloc([8, 512], BF16)
        fca = A.alloc([256], BF16); fcb = A.alloc([256], BF16); bdc = A.alloc([64], BF16); bds = A.alloc([64], BF16)
        AT = [A.alloc([2, 64, 64, 2], BF16) for _ in range(4)]
        xt = [A.alloc([1024], F32) for _ in range(4)]
        xT = [A.alloc([8, 128], BF16) for _ in range(2)]
        ub = [A.alloc([512], BF16) for _ in range(2)]
        Ej = [A.alloc([256], BF16) for _ in range(4)]
        Bt = [A.alloc([256], BF16) for _ in range(4)]
        yfs = [A.alloc([4096], BF16) for _ in range(2)]
        k.dma("gpsimd", wf, dr["w_in"][:, 0:512].rearrange("(c p) n -> p c n", p=128), [], ["wf"])
        k.dma("sync", fca, dr["c_fca"], [], ["fca"]); k.dma("sync", fcb, dr["c_fcb"], [], ["fcb"])
        k.dma("sync", bdc, dr["c_bdc"], [], ["bdc"]); k.dma("sync", bds, dr["c_bds"], [], ["bds"])
        xf_v = dr["xf"].rearrange("(r c) f -> c r f", c=64)

        def a_load(j):
            k.dma("sync", xt[j % 4], xf_v[j], [], [("xt", j % 4)])
            k.dma("sync", Ej[j % 4], dr["c_edft"][j], [], [("Ej", j % 4)])
        a_load(0); a_load(1)
        for j0 in range(0, 64, 2):
            for j in (j0, j0 + 1):
                if j + 2 < 64:
                    a_load(j + 2)
            tg = []
            for j in (j0, j0 + 1):
                for hb in range(2):
                    bank = 2 * (j % 2) + hb
                    tg.append([(ps[bank][:, q * 128:(q + 1) * 128], xt[j % 4][:, (hb * 4 + q) * 128:(hb * 4 + q + 1) * 128],
                                identf, [("xt", j % 4), "identf"], [("ps", bank)]) for q in range(4)])
            k.tri(tg)
            for j in (j0, j0 + 1):
                for hb in range(2):
                    bank = 2 * (j % 2) + hb
                    k.evac(xT[j % 2][:, hb * 4:(hb + 1) * 4, :], ps[bank][:].rearrange("p (a b) -> p a b", a=4),
                           [("ps", bank)], [("xT", j % 2, hb)])
            k.mmi([[(ps[4 + (j % 2)][:], xT[j % 2][:, fc, :], wf[:, fc, :],
                     [("xT", j % 2, 0), ("xT", j % 2, 1), "wf"], [("ps", 4 + (j % 2))]) for fc in range(8)]
                   for j in (j0, j0 + 1)])
            for j in (j0, j0 + 1):
                k.evac(ub[j % 2], ps[4 + (j % 2)][:], [("ps", 4 + (j % 2))], [("ub", j % 2)])
            for j in (j0, j0 + 1):
                for g in range(4):
                    pa = 6 + (g % 2)
                    k.mm(ps[pa][:, 0:256], ub[j % 2][:, g * 128:(g + 1) * 128], Ej[j % 4], True, True,
                         [("ub", j % 2), ("Ej", j % 4)], [("ps", pa)])
                    k.evac(AT[g][:, :, :, j, :], ps[pa][:, 0:256].rearrange("p (r k a) -> p r k a", r=2, k=64, a=2),
                           [("ps", pa)], [("AT", g)], partial=True)
        it = 0
        for g in range(4):
            ys = yfs[g % 2]
            ysv = ys.rearrange("p (k q) -> p k q", k=32)
            for kk0 in range(0, 64, 2):
                kks = (kk0, kk0 + 1)
                k.mmi([[(ps[kk % 2][:, 0:256], AT[g][:, 0, kk, :, :].rearrange("p s a -> p (s a)"), fca,
                         [("AT", g), "fca"], [("ps", kk % 2)]),
                        (ps[kk % 2][:, 0:256], AT[g][:, 1, kk, :, :].rearrange("p s a -> p (s a)"), fcb,
                         [("AT", g), "fcb"], [("ps", kk % 2)])] for kk in kks])
                bts = {}
                for kk in kks:
                    bts[kk] = it % 4
                    k.evac(Bt[it % 4], ps[kk % 2][:, 0:256], [("ps", kk % 2)], [("Bt", it % 4)])
                    it += 1
                k.mmi([[(ps[2 + kk % 2][:, 0:64], Bt[bts[kk]][:, 0:128], bdc, [("Bt", bts[kk]), "bdc"], [("ps", 2 + kk % 2)]),
                        (ps[2 + kk % 2][:, 0:64], Bt[bts[kk]][:, 128:256], bds, [("Bt", bts[kk]), "bds"], [("ps", 2 + kk % 2)])]
                       for kk in kks])
                for kk in kks:
                    k.evac(ysv[:, :, 2 * kk:2 * kk + 2].rearrange("p k a -> p a k"),
                           ps[2 + kk % 2][:, 0:64].rearrange("p (a k) -> p a k", a=2),
                           [("ps", 2 + kk % 2)], [("yfs", g % 2)], partial=True)
            k.dma("sync", dr["yfT_d"][:, g, :], ys, [("yfs", g % 2)], [("yfT_d", g)])
        P.barrier()
        A.reset(base_mark)

        kT = A.alloc([4, 4608], BF16)
        Vext = A.alloc([36, 8, 65], BF16)
        qT = A.alloc([4, 4096], BF16)
        b1_mark = A.mark()
        W = A.alloc([8, 2048], BF16)
        xt = [A.alloc([1024], F32) for _ in range(4)]
        xTq = [A.alloc([8, 512], BF16) for _ in range(2)]
        qmq = [A.alloc([4, 512], BF16) for _ in range(2)]
        for cb in range(4):
            k.dma("gpsimd", W[:, :, cb * 512:(cb + 1) * 512],
                  dr["w_in"][:, 512 + cb * 512:512 + (cb + 1) * 512].rearrange("(c p) n -> p c n", p=128),
                  [], [("W", cb)])
        Wr = [("W", cb) for cb in range(4)]
        k.v("vector", "memset", (Vext[:, :, :, 64:65], 1.0), [], ["Vext1"])
        xo_t = dr["xo"].rearrange("(t p) f -> t p f", p=128)

        def b_load(t):
            k.dma("sync", xt[t % 4], xo_t[t], [], [("xt", t % 4)])
        b_load(0); b_load(1)
        for Q in range(9):
            xq = xTq[Q % 2]
            for tt0 in range(0, 4, 2):
                tts = (tt0, tt0 + 1)
                for tt in tts:
                    t = 4 * Q + tt
                    if t + 2 < 36:
                        b_load(t + 2)
                tg = []
                for tt in tts:
                    t = 4 * Q + tt
                    for hb in range(2):
                        bank = 2 * (t % 2) + hb
                        tg.append([(ps[bank][:, q * 128:(q + 1) * 128],
                                    xt[t % 4][:, (hb * 4 + q) * 128:(hb * 4 + q + 1) * 128], identf,
                                    [("xt", t % 4), "identf"], [("ps", bank)]) for q in range(4)])
                k.tri(tg)
                for tt in tts:
                    t = 4 * Q + tt
                    for hb in range(2):
                        bank = 2 * (t % 2) + hb
                        k.evac(xq[:, hb * 4:(hb + 1) * 4, tt * 128:(tt + 1) * 128],
                               ps[bank][:].rearrange("p (a b) -> p a b", a=4),
                               [("ps", bank)], [("xTq", Q % 2)], partial=True)
            xr = [("xTq", Q % 2)] + Wr

            def proj_pair(col0, cs, banks):
                k.mmi([[(ps[banks[i]][:], W[:, fc, col0 + c * 128:col0 + (c + 1) * 128], xq[:, fc, :], xr,
                         [("ps", banks[i])]) for fc in range(8)] for i, c in enumerate(cs)])
            for cs, banks in (((0, 1), (4, 5)), ((2, 3), (6, 7))):
                proj_pair(512, cs, banks)
                for i, c in enumerate(cs):
                    k.evac(kT[:, c, Q * 512:(Q + 1) * 512], ps[banks[i]][:], [("ps", banks[i])], [("kT", Q)],
                           partial=True)
            if Q < 8:
                for cs, banks in (((0, 1), (4, 5)), ((2, 3), (6, 7))):
                    proj_pair(0, cs, banks)
                    for i, c in enumerate(cs):
                        k.act(qT[:, c, Q * 512:(Q + 1) * 512], ps[banks[i]][:], AF.Copy, [("ps", banks[i])],
                              [("qT", Q)], scale=0.125, partial=True)
                for cs, banks in (((0, 1), (4, 5)), ((2, 3), (6, 7))):
                    proj_pair(1536, cs, banks)
                    for i, c in enumerate(cs):
                        k.evac(qmq[Q % 2][:, c, :], ps[banks[i]][:], [("ps", banks[i])], [("qmq", Q % 2)],
                               partial=True)
                k.dma("sync", dr["qmT_d"][Q], qmq[Q % 2], [("qmq", Q % 2)], [("qmT_d", Q)])
                k.dma("sync", dr["xT_d"][Q], xq, [("xTq", Q % 2)], [("xT_d", Q)])
            for tts, banks in (((0, 1), (4, 5)), ((2, 3), (6, 7))):
                k.mmi([[(ps[banks[i]][:], xq[:, fc, tt * 128:(tt + 1) * 128], W[:, fc, 1024:1536], xr,
                         [("ps", banks[i])]) for fc in range(8)] for i, tt in enumerate(tts)])
                for i, tt in enumerate(tts):
                    t = 4 * Q + tt
                    k.evac(Vext[:, t, :, 0:64], ps[banks[i]][:].rearrange("p (h d) -> p h d", h=8),
                           [("ps", banks[i])], [("Vext", t)])
        P.barrier()
        A.reset(b1_mark)

        bias_i = A.alloc([8, 768], BF16)
        bias_s = [A.alloc([8, 768], BF16) for _ in range(2)]
        eS = [A.alloc([768], BF16) for _ in range(3)]
        yna = [A.alloc([512], BF16) for _ in range(2)]
        rc = [A.alloc([8], F32) for _ in range(2)]
        ynaTq = [A.alloc([4, 512], BF16) for _ in range(2)]
        for hh in range(8):
            k.dma("gpsimd", bias_i[:, hh, :], dr["biasT"][0, hh], [], [("bias_i", hh)])
        special = {0: 1, 1: 2, 30: 3, 31: 4}

        def tile_of_pair(lp):
            if 0 <= lp < 32:
                return lp
            return {-2: 32, -1: 33, 32: 34, 33: 35}[lp]
        nsp = 0

        def offs_of(qp):
            if qp == 0:
                return [-2, -1, 0, 1, 2, 3]
            if qp == 31:
                return [-3, -2, -1, 0, 1, 2]
            return [-2, -1, 0, 1, 2]
        bias_of = {}

        def na_finalize(qp):
            yq = yna[qp % 2]
            rcq = rc[qp % 2]
            for h2 in range(8):
                po2 = 4 + (qp % 2) * 2 + (h2 // 4)
                oc2 = (h2 % 4) * 65
                k.v("vector", "reciprocal", (rcq[:, h2:h2 + 1], ps[po2][:, oc2 + 64:oc2 + 65]),
                    [("ps", po2, h2 % 4)], [("rc", qp % 2, h2)])
                k.v("vector", "tensor_scalar", (yq[:, h2 * 64:(h2 + 1) * 64], ps[po2][:, oc2:oc2 + 64],
                                                 rcq[:, h2:h2 + 1], None, ALU.mult),
                    [("ps", po2, h2 % 4), ("rc", qp % 2, h2)], [("yna", qp % 2)], partial=True)

        def na_transposes(qp):
            yq = yna[qp % 2]
            Q = qp // 4
            pT = 4 + (qp % 2) * 2 + 1
            tv = psb[pT][:, 640:1024]
            pT2 = 4 + (qp % 2) * 2
            tv2 = psb[pT2][:, 640:768]
            k.tr(tv[:, 0:128], yq[:, 0:128], identb, [("yna", qp % 2), "identb"], [("ps", pT, "T")], partial=False)
            k.tr(tv2, yq[:, 384:512], identb, [("yna", qp % 2), "identb"], [("ps", pT2, "T")], partial=False)
            for cc in range(1, 3):
                k.tr(tv[:, cc * 128:(cc + 1) * 128], yq[:, cc * 128:(cc + 1) * 128], identb,
                     [("yna", qp % 2), "identb"], [("ps", pT, "T")], partial=True)
            yt = ynaTq[Q % 2]
            k.evac(yt[:, 0:3, (qp % 4) * 128:(qp % 4 + 1) * 128], tv.rearrange("p (a b) -> p a b", a=3),
                   [("ps", pT, "T")], [("ynaTq", Q % 2)], partial=True)
            k.evac(yt[:, 3, (qp % 4) * 128:(qp % 4 + 1) * 128], tv2, [("ps", pT2, "T")], [("ynaTq", Q % 2)],
                   partial=True)
            if qp % 4 == 3:
                k.dma("sync", dr["ynaT_d"][:, :, Q * 512:(Q + 1) * 512], yt, [("ynaTq", Q % 2)], [("ynaT_d", Q)])

        def pv_list(n):
            qp, h = n // 8, n % 8
            offs = offs_of(qp); ns = len(offs)
            es = eS[n % 3]; en = ("eS", n % 3)
            po = 4 + (qp % 2) * 2 + (h // 4)
            oc = (h % 4) * 65
            out = []
            for j, off in enumerate(offs):
                kt = tile_of_pair(qp + off)
                out.append((ps[po][:, oc:oc + 65], es[:, j * 128:(j + 1) * 128], Vext[:, kt, h, :], j == 0, j == ns - 1,
                            [en, ("Vext", kt), "Vext1"], [("ps", po, h % 4)]))
            return out
        NH = 256
        for n in range(NH + 1):
            qk = []
            if n < NH:
                qp, h = n // 8, n % 8
                offs = offs_of(qp); ns = len(offs)
                if h == 0:
                    if qp in special:
                        bt_ = bias_s[nsp % 2]; bname = ("bias_s", nsp % 2)
                        for hh in range(8):
                            k.dma("gpsimd", bt_[:, hh, :], dr["biasT"][special[qp], hh], [], [bname], key=bname,
                                  partial=(hh > 0))
                        bias_of[qp] = (bt_, [bname])
                        nsp += 1
                    else:
                        bias_of[qp] = (bias_i, None)
                bt_, breads = bias_of[qp]
                c = h // 2; pb0 = 64 * (h % 2)
                sX = (n % 2) * 2; sY = sX + 1
                br_ = breads if breads is not None else [("bias_i", h)]
                k.mm(ps[sX][:], identb, bt_[:, h, 0:512], True, False, br_ + ["identb"], [("ps", sX)])
                k.mm(ps[sY][:, 0:(ns - 4) * 128], identb, bt_[:, h, 512:512 + (ns - 4) * 128], True, False,
                     br_ + ["identb"], [("ps", sY)])
                for j, off in enumerate(offs):
                    kt = tile_of_pair(qp + off)
                    bank = sX if j < 4 else sY
                    last = (j == 3) or (j == ns - 1)
                    qk.append((ps[bank][:, (j % 4) * 128:(j % 4 + 1) * 128],
                               kT[pb0:pb0 + 64, c, kt * 128:(kt + 1) * 128],
                               qT[pb0:pb0 + 64, c, qp * 128:(qp + 1) * 128], False, last,
                               [("kT", kt // 4), ("qT", qp // 4)], [("ps", bank)]))
            pv = pv_list(n - 1) if n >= 1 else []
            for i in range(max(len(qk), len(pv))):
                if i < len(qk):
                    k.mm(*qk[i])
                if i < len(pv):
                    k.mm(*pv[i])
            if n < NH:
                es = eS[n % 3]; en = ("eS", n % 3)
                k.act(es[:, 0:512], ps[sX][:], AF.Exp, [("ps", sX)], [en])
                k.act(es[:, 512:ns * 128], ps[sY][:, 0:(ns - 4) * 128], AF.Exp, [("ps", sY)], [en], partial=True)
            if n >= 1 and (n - 1) % 8 == 7:
                na_finalize((n - 1) // 8)
            if n >= 1 and (n - 1) % 8 == 3 and (n - 1) // 8 >= 1:
                na_transposes((n - 1) // 8 - 1)
        na_transposes(31)
        P.barrier()
        A.reset(base_mark)

        Atop = Arena(arena_t, ARENA)
        Atop.off = ARENA - (48 + 24 + 16) * 1024
        wg = Atop.alloc([8, 3072], BF16)
        wo3 = [Atop.alloc([4, 1024], BF16) for _ in range(3)]
        wout = Atop.alloc([8, 1024], BF16)
        top_limit = ARENA - (48 + 24 + 16) * 1024
        for cb in range(6):
            k.dma("gpsimd", wg[:, :, cb * 512:(cb + 1) * 512],
                  dr["w_gate"][:, cb * 512:(cb + 1) * 512].rearrange("(c p) n -> p c n", p=128), [], [("wg", cb)])
        for i, nm in enumerate(("w_fo", "w_nao", "w_memo")):
            k.dma("gpsimd", wo3[i], dr[nm].rearrange("(c p) n -> p c n", p=128), [], [("wo3", i)])
        for cb in range(2):
            k.dma("gpsimd", wout[:, :, cb * 512:(cb + 1) * 512],
                  dr["w_out"][:, cb * 512:(cb + 1) * 512].rearrange("(c p) n -> p c n", p=128), [], [("wout", cb)])
        wkv = A.alloc([8, 1024], BF16)
        memt = [A.alloc([1024], F32) for _ in range(2)]
        memT = A.alloc([8, 256], BF16)
        kmT = A.alloc([4, 256], BF16)
        Vm = A.alloc([2, 4, 129], BF16)
        qmq = [A.alloc([4, 512], BF16) for _ in range(2)]
        eSm = [A.alloc([512], BF16) for _ in range(4)]
        ym = [A.alloc([512], BF16) for _ in range(8)]
        rcm = [A.alloc([4], F32) for _ in range(8)]
        ymTq = [A.alloc([4, 512], BF16) for _ in range(2)]
        assert A.off <= top_limit, ("B1c arena overlaps prefetched weights", A.off, top_limit)
        for cb in range(2):
            k.dma("gpsimd", wkv[:, :, cb * 512:(cb + 1) * 512],
                  dr["w_kv"][:, cb * 512:(cb + 1) * 512].rearrange("(c p) n -> p c n", p=128), [], [("wkv", cb)])
        k.v("vector", "memset", (Vm[:, :, :, 128:129], 1.0), [], ["Vm1"])
        for mc in range(2):
            k.dma("sync", memt[mc], dr["mem"][mc * 128:(mc + 1) * 128, :], [], [("memt", mc)])
            for hb in range(2):
                bank = 2 * mc + hb
                for q in range(4):
                    fc = hb * 4 + q
                    k.tr(ps[bank][:, q * 128:(q + 1) * 128], memt[mc][:, fc * 128:(fc + 1) * 128], identf,
                         [("memt", mc), "identf"], [("ps", bank)], partial=(q > 0))
                k.evac(memT[:, hb * 4:(hb + 1) * 4, mc * 128:(mc + 1) * 128],
                       ps[bank][:].rearrange("p (a b) -> p a b", a=4), [("ps", bank)], ["memT"], partial=True)
        for h in range(4):
            pb_ = 4 + h % 2
            for fc in range(8):
                k.mm(ps[pb_][:, 0:256], wkv[:, fc, h * 128:(h + 1) * 128], memT[:, fc, :], fc == 0, fc == 7,
                     ["memT", ("wkv", 0)], [("ps", pb_)])
            k.evac(kmT[:, h, :], ps[pb_][:, 0:256], [("ps", pb_)], ["kmT"], partial=True)
        for mc in range(2):
            pb_ = 6 + mc
            for fc in range(8):
                k.mm(ps[pb_][:], memT[:, fc, mc * 128:(mc + 1) * 128], wkv[:, fc, 512:1024], fc == 0, fc == 7,
                     ["memT", ("wkv", 1)], [("ps", pb_)])
            k.evac(Vm[:, mc, :, 0:128], ps[pb_][:].rearrange("p (h d) -> p h d", h=4), [("ps", pb_)], ["Vm"],
                   partial=True)
        msc = 128.0 ** -0.5
        k.dma("sync", qmq[0], dr["qmT_d"][0], [], [("qmq", 0)])
        ecnt = 0
        for Q in range(8):
            if Q + 1 < 8:
                k.dma("sync", qmq[(Q + 1) % 2], dr["qmT_d"][Q + 1], [], [("qmq", (Q + 1) % 2)])
            qq = qmq[Q % 2]
            for h in range(4):
                ebufs = []
                for mc in range(2):
                    bank = (ecnt % 2) * 2 + mc
                    k.mm(ps[bank][:], kmT[:, h, mc * 128:(mc + 1) * 128], qq[:, h, :], True, True,
                         ["kmT", ("qmq", Q % 2)], [("ps", bank)])
                    eb = (ecnt % 2) * 2 + mc
                    k.act(eSm[eb], ps[bank][:], AF.Exp, [("ps", bank)], [("eSm", eb)], scale=msc)
                    ebufs.append(eb)
                for tts in ((0, 2), (1, 3)):
                    k.mmi([[(ps[4 + (tt // 2) + 2 * (ecnt % 2)][:, (tt % 2) * 129:(tt % 2) * 129 + 129],
                             eSm[ebufs[mc]][:, tt * 128:(tt + 1) * 128], Vm[:, mc, h, :],
                             [("eSm", ebufs[mc]), "Vm", "Vm1"], [("ps", 4 + (tt // 2) + 2 * (ecnt % 2), tt % 2)])
                            for mc in range(2)] for tt in tts])
                for tt in range(4):
                    po = 4 + (tt // 2) + 2 * (ecnt % 2)
                    oc = (tt % 2) * 129
                    yb_ = (Q % 2) * 4 + tt
                    k.v("vector", "reciprocal", (rcm[yb_][:, h:h + 1], ps[po][:, oc + 128:oc + 129]),
                        [("ps", po, tt % 2)], [("rcm", yb_, h)])
                    k.v("vector", "tensor_scalar", (ym[yb_][:, h * 128:(h + 1) * 128], ps[po][:, oc:oc + 128],
                                                     rcm[yb_][:, h:h + 1], None, ALU.mult),
                        [("ps", po, tt % 2), ("rcm", yb_, h)], [("ym", yb_)], partial=True)
                ecnt += 1
            yt = ymTq[Q % 2]
            for tt in range(4):
                yb_ = (Q % 2) * 4 + tt
                bank = (tt % 2) * 2 + (ecnt % 2)
                tvm = psb[bank][:, 0:512]
                for cc in range(4):
                    k.tr(tvm[:, cc * 128:(cc + 1) * 128], ym[yb_][:, cc * 128:(cc + 1) * 128], identb,
                         [("ym", yb_), "identb"], [("ps", bank)], partial=(cc > 0))
                k.evac(yt[:, :, tt * 128:(tt + 1) * 128], tvm.rearrange("p (a b) -> p a b", a=4), [("ps", bank)],
                       [("ymTq", Q % 2)], partial=True)
            k.dma("sync", dr["ymemT_d"][:, :, Q * 512:(Q + 1) * 512], yt, [("ymTq", Q % 2)], [("ymemT_d", Q)])
        P.barrier()
        A.reset(base_mark)

        wrt = A.alloc([8, 72], F32)
        bgt = A.alloc([24], F32)
        brt = A.alloc([72], F32); onesrow = A.alloc([128], F32)
        lng = A.alloc([1024], F32); lnb = A.alloc([1024], F32)
        ect = A.alloc([64], F32); mhalf = A.alloc([1], F32)
        atot = A.alloc([64], BF16)
        xTq = [A.alloc([8, 512], BF16) for _ in range(2)]
        ybq = [[A.alloc([4, 512], BF16) for _ in range(2)] for _ in range(3)]
        G = [[A.alloc([512], BF16) for _ in range(2)] for _ in range(3)]
        tm = [A.alloc([512], F32) for _ in range(4)]
        mT = A.alloc([8, 512], BF16)
        xt = [A.alloc([1024], F32) for _ in range(2)]
        rt = [A.alloc([1024], F32) for _ in range(2)]
        ht = [A.alloc([1024], F32) for _ in range(2)]
        hl = [A.alloc([1024], F32) for _ in range(2)]
        hb = [A.alloc([1024], BF16) for _ in range(2)]
        hT = A.alloc([8, 128], F32)
        sm = A.alloc([256], F32)
        sml = [A.alloc([16], F32) for _ in range(2)]
        k.A1 = A.alloc([64], F32); k.A2 = A.alloc([64], F32); k.Ab = A.alloc([64], BF16)
        assert A.off <= top_limit, ("B3 arena overlaps prefetched weights", A.off, top_limit)
        k.dma("sync", wrt, dr["wr"].rearrange("(c p) n -> p c n", p=128), [], ["wrt"])
        k.dma("sync", bgt, dr["bg"], [], ["bgt"])
        k.dma("sync", brt[0:1, :], dr["br"], [], ["brt"])
        k.dma("sync", onesrow[0:1, :], dr["c_onesrow"], [], ["onesrow"])
        k.dma("sync", lng, dr["ln1_g"].partition_broadcast(128)[:, 0, :], [], ["lng"])
        k.dma("sync", lnb, dr["ln1_b"].partition_broadcast(128)[:, 0, :], [], ["lnb"])
        k.dma("sync", ect, dr["c_ec"], [], ["ect"])
        k.dma("sync", mhalf, dr["c_mhalf"], [], ["mhalf"])
        k.v("vector", "memset", (atot, 0.0), [], ["atot"])
        ysrc = ("yfT_d", "ynaT_d", "ymemT_d")

        def c_load(Q):
            k.dma("sync", xTq[Q % 2], dr["xT_d"][Q], [], [("xTq", Q % 2)])
            for i in range(3):
                k.dma("sync", ybq[i][Q % 2], dr[ysrc[i]][:, :, Q * 512:(Q + 1) * 512], [], [("ybq", i, Q % 2)])
        c_load(0)
        wgr = [("wg", cb) for cb in range(6)]

        def r1_load(ti):
            b2 = ti % 2
            k.dma("sync", hl[b2], dr["h_d"][ti * 128:(ti + 1) * 128, :], [("h_d", ti)], [("hl", b2)])

        def r1(ti):
            b2 = ti % 2
            if ti % 4 >= 2:
                r1_load(ti)
            k.act(hb[b2], hl[b2], AF.Copy, [("hl", b2)], [("hb", b2)])
            k.tri([[(ps[6 + hh][:, q * 128:(q + 1) * 128], hl[b2][:, (hh * 4 + q) * 128:(hh * 4 + q + 1) * 128], identf,
                     [("hl", b2), "identf"], [("ps", 6 + hh)]) for q in range(4)] for hh in range(2)])
            for hh in range(2):
                k.evac(hT[:, hh * 4:(hh + 1) * 4, :], ps[6 + hh][:].rearrange("p (a b) -> p a b", a=4),
                       [("ps", 6 + hh)], [("hT", hh)])

        def r2(ti):
            for fc in range(8):
                k.mm(ps[7][:, 0:72], hT[:, fc, :], wrt[:, fc, :], fc == 0, False,
                     [("hT", 0), ("hT", 1), "wrt"], [("ps", 7)])
            k.mm(ps[7][:, 0:72], onesrow[0:1, :], brt[0:1, :], False, True, ["onesrow", "brt"], [("ps", 7)])

        def r3(ti):
            emit_route(k, ps, sm, ti, slots_i, rw, atot, ustrict, ones_b, ect, 7, 1)

        def r4(ti):
            b2 = ti % 2
            emit_route(k, ps, sm, ti, slots_i, rw, atot, ustrict, ones_b, ect, 7, 2)
            for kk in range(2):
                P.op("gpsimd", (lambda e, ti=ti, kk=kk, b2=b2: e.indirect_dma_start(
                    out=dr["xg_d"][:, :],
                    out_offset=bass.IndirectOffsetOnAxis(ap=slots_i[:, ti, kk:kk + 1], axis=0),
                    in_=hb[b2][:, :], in_offset=None, bounds_check=_bc_reg(e), oob_is_err=False)),
                    reads=[("hb", b2), ("slots", ti)], writes=["xg_d"], dma=True, key=("xg_sc", b2), partial=True)

        def route_sched(Qp):
            t = [Qp * 4 + i for i in range(4)]
            return [[lambda: r1(t[0])], [lambda: r2(t[0]), lambda: r3(t[0])],
                    [lambda: r4(t[0]), lambda: r1(t[1])], [lambda: r2(t[1]), lambda: r3(t[1])],
                    [lambda: r4(t[1]), lambda: r1(t[2])], [lambda: r2(t[2]), lambda: r3(t[2])],
                    [lambda: r4(t[2]), lambda: r1(t[3])], [lambda: r2(t[3]), lambda: r3(t[3])],
                    [lambda: r4(t[3])]]
        for Q in range(9):
            sched = route_sched(Q - 1) if Q >= 1 else [[] for _ in range(9)]
            if Q < 8:
                if Q + 1 < 8:
                    c_load(Q + 1)
                xq = xTq[Q % 2]
            for dc in range(8):
                if Q < 8:
                    k.mmi([[(ps[i][:], wg[:, fc, i * 1024 + dc * 128:i * 1024 + (dc + 1) * 128], xq[:, fc, :],
                             wgr + [("xTq", Q % 2)], [("ps", i)]) for fc in range(8)] for i in range(3)])
                    for i in range(3):
                        k.act(G[i][dc % 2], ps[i][:], AF.Sigmoid, [("ps", i), "bgt"], [("G", i, dc % 2)],
                              bias=bgt[:, i * 8 + dc:i * 8 + dc + 1])
                    k.mmi([[(ps[3 + i][:], wo3[i][:, cc, dc * 128:(dc + 1) * 128], ybq[i][Q % 2][:, cc, :],
                             [("wo3", i), ("ybq", i, Q % 2)], [("ps", 3 + i)]) for cc in range(4)] for i in range(3)])
                if Q < 8:
                    for i in range(3):
                        k.v("vector", "tensor_tensor", (tm[i], G[i][dc % 2], ps[3 + i][:], ALU.mult),
                            [("G", i, dc % 2), ("ps", 3 + i)], [("tm", i)])
                    k.v("gpsimd", "tensor_tensor", (tm[3], tm[0], tm[1], ALU.add), [("tm", 0), ("tm", 1)], [("tm", 3)])
                    k.v("gpsimd", "tensor_tensor", (mT[:, dc, :], tm[3], tm[2], ALU.add), [("tm", 3), ("tm", 2)],
                        [("mT", dc)])
                for f_ in sched[dc]:
                    f_()
            if Q == 8:
                for f_ in sched[8]:
                    f_()
                break
            mTr = [("mT", dc) for dc in range(8)]
            k.dma("sync", xt[0], xo_t[Q * 4], [], [("xt", 0)])

            def s1a(tt):
                ti = Q * 4 + tt
                b2 = ti % 2
                if tt + 1 < 4:
                    k.dma("sync", xt[(tt + 1) % 2], xo_t[ti + 1], [], [("xt", (tt + 1) % 2)])
                pbk = (6, 7) if tt % 2 == 0 else (4, 5)
                k.mmi([[(ps[pbk[half]][:], mT[:, dc, tt * 128:(tt + 1) * 128], wout[:, dc, half * 512:(half + 1) * 512],
                         mTr + [("wout", half)], [("ps", pbk[half])]) for dc in range(8)] for half in range(2)])
                for half in range(2):
                    pm = pbk[half]
                    k.v("vector", "scalar_tensor_tensor",
                        (rt[b2][:, half * 512:(half + 1) * 512], xt[tt % 2][:, half * 512:(half + 1) * 512], ALPHA,
                         ps[pm][:], ALU.mult, ALU.add),
                        [("xt", tt % 2), ("ps", pm)], [("rt", b2, half)])
                emit_ln_a(k, rt[b2], [("rt", b2, 0), ("rt", b2, 1)], sml[b2], mhalf, tag=b2)

            def s1b(tt):
                ti = Q * 4 + tt
                b2 = ti % 2
                emit_ln_b(k, rt[b2], [("rt", b2, 0), ("rt", b2, 1)], ht[b2], ("ht", b2), lng, lnb, ["lng", "lnb"],
                          sml[b2], tag=b2)
                k.dma("sync", dr["h_d"][ti * 128:(ti + 1) * 128, :], ht[b2], [("ht", b2)], [("h_d", ti)])
            s1a(0)
            s1a(1)
            s1b(0)
            r1_load(Q * 4)
            s1a(2)
            s1b(1)
            r1_load(Q * 4 + 1)
            s1a(3)
            s1b(2)
            s1b(3)
            for f_ in sched[8]:
                f_()
        P.barrier()
        A.reset(base_mark)
        if debug:
            dbgt = A.alloc([32, 4], F32)
            k.v("vector", "tensor_copy", (dbgt[:, :, 0:2], slots_i), [], ["dbgt0"])
            k.v("vector", "tensor_copy", (dbgt[:, :, 2:4], rw), [], ["dbgt1"])
            k.dma("sync", dr["dbg_rt"], dbgt, ["dbgt0", "dbgt1"], ["dbg_rt"])
            k.dma("sync", dr["dbg_h"], dr["h_d"], [], ["dbg_h"])
            for i_, nm_ in enumerate(("yfT_d", "ynaT_d", "ymemT_d")):
                k.dma("sync", dr["dbg_y3"][i_], dr[nm_], [], [("dbg_y3", i_)])

        weg = [A.alloc([8, 512], BF16) for _ in range(3)]
        weu = [A.alloc([8, 512], BF16) for _ in range(3)]
        wed = [A.alloc([4, 1024], BF16) for _ in range(3)]
        xg = [A.alloc([2, 1024], BF16) for _ in range(3)]
        xgT = [A.alloc([8, 256], BF16) for _ in range(2)]
        sg = [A.alloc([256], F32) for _ in range(2)]
        aT = [A.alloc([4, 256], BF16) for _ in range(2)]
        yo = [A.alloc([1024], BF16) for _ in range(2)]

        def e_load(e_):
            s = e_ % 3
            k.dma("gpsimd", weg[s], dr["w_eg"][e_].rearrange("(c p) n -> p c n", p=128), [], [("weg", s)])
            k.dma("gpsimd", weu[s], dr["w_eu"][e_].rearrange("(c p) n -> p c n", p=128), [], [("weu", s)])
            k.dma("gpsimd", wed[s], dr["w_ed"][e_].rearrange("(c p) n -> p c n", p=128), [], [("wed", s)])
            k.dma("sync", xg[e_ % 3], dr["xg_d"][e_ * CAP:(e_ + 1) * CAP, :].rearrange("(t p) f -> p t f", p=128),
                  ["xg_d"], [("xg", e_ % 3)])
        e_load(0); e_load(1)

        def e_transposes(ee):
            xgt_ = xgT[ee % 2]
            k.tri([[(psb[tt][:, fc * 128:(fc + 1) * 128], xg[ee % 3][:, tt, fc * 128:(fc + 1) * 128], identb,
                     [("xg", ee % 3), "identb"], [("ps", tt)]) for fc in range(8)] for tt in range(2)])
            for tt in range(2):
                k.evac(xgt_[:, :, tt * 128:(tt + 1) * 128], psb[tt][:].rearrange("p (a b) -> p a b", a=8),
                       [("ps", tt)], [("xgT", ee % 2)], partial=True)
        yc = 0
        for e_ in range(NEXP):
            if e_ + 2 < NEXP:
                e_load(e_ + 2)
            s = e_ % 3
            xgt = xgT[e_ % 2]
            if e_ == 0:
                e_transposes(0)
            at = aT[e_ % 2]
            for dcn in range(4):
                pg = 2 + (dcn % 2) * 2; pu = pg + 1
                k.mmi([[(ps[pg][:, 0:256], weg[s][:, fc, dcn * 128:(dcn + 1) * 128], xgt[:, fc, :],
                         [("weg", s), ("xgT", e_ % 2)], [("ps", pg)]) for fc in range(8)],
                       [(ps[pu][:, 0:256], weu[s][:, fc, dcn * 128:(dcn + 1) * 128], xgt[:, fc, :],
                         [("weu", s), ("xgT", e_ % 2)], [("ps", pu)]) for fc in range(8)]])
                k.act(sg[dcn % 2], ps[pg][:, 0:256], AF.Silu, [("ps", pg)], [("sg", dcn % 2)])
                k.v("vector", "tensor_tensor", (at[:, dcn, :], sg[dcn % 2], ps[pu][:, 0:256], ALU.mult),
                    [("sg", dcn % 2), ("ps", pu)], [("aT", e_ % 2, dcn)])
            atr = [("aT", e_ % 2, dcn) for dcn in range(4)]
            if e_ + 1 < NEXP:
                e_transposes(e_ + 1)
            for tt in range(2):
                yt = yo[yc % 2]
                k.mmi([[(ps[6 + half][:], at[:, dcn, tt * 128:(tt + 1) * 128], wed[s][:, dcn, half * 512:(half + 1) * 512],
                         atr + [("wed", s)], [("ps", 6 + half)]) for dcn in range(4)] for half in range(2)])
                for half in range(2):
                    pd = 6 + half
                    k.evac(yt[:, half * 512:(half + 1) * 512], ps[pd][:], [("ps", pd)], [("yo", yc % 2, half)])
                k.dma("sync", dr["yb_d"][e_ * CAP + tt * 128:e_ * CAP + (tt + 1) * 128, :], yt,
                      [("yo", yc % 2, 0), ("yo", yc % 2, 1)], ["yb_d"], key=("yo_st", yc % 2), partial=True)
                yc += 1
        P.barrier()
        A.reset(base_mark)

        lng = A.alloc([1024], F32); lnb = A.alloc([1024], F32); mhalf = A.alloc([1], F32)
        sm = A.alloc([256], F32)
        NB = 4
        hd = [A.alloc([1024], F32) for _ in range(NB)]
        g1 = [A.alloc([1024], BF16) for _ in range(NB)]
        g2 = [A.alloc([1024], BF16) for _ in range(NB)]
        acc = [A.alloc([1024], F32) for _ in range(NB)]
        ot = [A.alloc([1024], F32) for _ in range(NB)]
        smd = [A.alloc([32], F32) for _ in range(NB)]
        k.dma("sync", lng, dr["ln2_g"].partition_broadcast(128)[:, 0, :], [], ["lng"])
        k.dma("sync", lnb, dr["ln2_b"].partition_broadcast(128)[:, 0, :], [], ["lnb"])
        k.dma("sync", mhalf, dr["c_mhalf"], [], ["mhalf"])

        def d_load(ti):
            b2 = ti % NB
            k.dma("sync", hd[b2], dr["h_d"][ti * 128:(ti + 1) * 128, :], [], [("hd", b2)])
            for kk, gt in enumerate((g1, g2)):
                P.op("gpsimd", (lambda e, ti=ti, kk=kk, gt=gt, b2=b2: e.indirect_dma_start(
                    out=gt[b2][:, :], out_offset=None, in_=dr["yb_d"][:, :],
                    in_offset=bass.IndirectOffsetOnAxis(ap=slots_i[:, ti, kk:kk + 1], axis=0),
                    bounds_check=_bc_reg(e), oob_is_err=False)),
                    reads=[], writes=[("g", kk, b2)], dma=True)
        d_load(0); d_load(1)

        def da(ti):
            if ti + 2 < 32:
                d_load(ti + 2)
            b2 = ti % NB
            k.act(acc[b2], hd[b2], AF.Copy, [("hd", b2)], [("acc", b2)], scale=ALPHA)
            k.v("vector", "scalar_tensor_tensor", (acc[b2], g1[b2], rw[:, ti, 0:1], acc[b2], ALU.mult, ALU.add),
                [("g", 0, b2), ("acc", b2)], [("acc", b2)])
            k.v("vector", "scalar_tensor_tensor", (acc[b2], g2[b2], rw[:, ti, 1:2], acc[b2], ALU.mult, ALU.add),
                [("g", 1, b2), ("acc", b2)], [("acc", b2)])
            emit_ln_a(k, acc[b2], [("acc", b2)], smd[b2], mhalf, tag=b2)

        def db(ti):
            b2 = ti % NB
            emit_ln_b(k, acc[b2], [("acc", b2)], ot[b2], ("ot", b2), lng, lnb, ["lng", "lnb"], smd[b2], tag=b2)
            k.dma("sync", dr["out"][ti * 128:(ti + 1) * 128, :], ot[b2], [("ot", b2)], [("out", ti)])
        da(0)
        for ti in range(32):
            if ti + 1 < 32:
                da(ti + 1)
            db(ti)
        P.emit()
    return nc


def emit_ln_a(k, src, src_names, sm, mhalf, tag=0):
    st = sm[:, 0:12].rearrange("p (a b) -> p a b", a=2)
    mv = sm[:, 12:14]
    rs = sm[:, 14:15]
    nb = sm[:, 15:16]
    T = lambda x: (x, tag)
    for half in range(2):
        k.v("vector", "bn_stats", (st[:, half, :], src[:, half * 512:(half + 1) * 512]), src_names,
            [T("lnst%d" % half)])
    k.v("vector", "bn_aggr", (mv, st), [T("lnst0"), T("lnst1")], [T("lnmv")])
    k.v("gpsimd", "tensor_scalar", (rs, mv[:, 1:2], LN_EPS, 1.0, ALU.add, ALU.mult), [T("lnmv")], [T("lnrs0")])
    k.v("gpsimd", "tensor_tensor", (rs, rs, mhalf, ALU.pow), [T("lnrs0"), "mhalf"], [T("lnrs")])
    k.v("gpsimd", "tensor_scalar", (nb, mv[:, 0:1], rs, -1.0, ALU.mult, ALU.mult), [T("lnmv"), T("lnrs")],
        [T("lnnb")])


def emit_ln_b(k, src, src_names, dst, dst_name, g, b, gb_names, sm, tag=0):
    rs = sm[:, 14:15]
    nb = sm[:, 15:16]
    T = lambda x: (x, tag)
    k.act(dst, src, AF.Identity, src_names + [T("lnrs"), T("lnnb")], [dst_name + ("n",), dst_name], bias=nb, scale=rs)
    k.v("gpsimd", "tensor_tensor", (dst[:, 0:512], dst[:, 0:512], g[:, 0:512], ALU.mult),
        [dst_name + ("n",), gb_names[0]], [dst_name + ("g0",)])
    k.v("vector", "tensor_tensor", (dst[:, 512:1024], dst[:, 512:1024], g[:, 512:1024], ALU.mult),
        [dst_name + ("n",), gb_names[0]], [dst_name + ("g1",)])
    k.v("vector", "tensor_tensor", (dst, dst, b, ALU.add), [dst_name + ("g0",), dst_name + ("g1",), gb_names[1]],
        [dst_name])


def emit_route(k, ps, sm, ti, slots_i, rw, atot, ustrict, ones_b, ect, pl=2, stage=0):
    L = sm[:, 16:88]
    gm8 = sm[:, 88:96]
    ohg = sm[:, 96:104]
    dd = sm[:, 104:112]
    pg = sm[:, 112:113]
    el = sm[:, 113:121]
    tm8 = sm[:, 121:129]
    oh1 = sm[:, 129:137]
    oh2 = sm[:, 137:145]
    dv = sm[:, 145:146]
    t64 = sm[:, 146:210]
    sl = sm[:, 210:212]
    n = lambda s: ("rt_" + s,)
    V = lambda name, args, r, w: k.v("vector", name, args, r, w)
    if stage == 2:
        return _route_p2(k, ps, sm, ti, slots_i, atot, ustrict, ones_b, ect, pl)
    V("tensor_copy", (L, ps[pl][:, 0:72]), [("ps", pl)], [n("L")])
    V("max", (gm8, L[:, 0:8]), [n("L")], [n("gm8")])
    V("tensor_scalar", (ohg, L[:, 0:8], gm8[:, 0:1], None, ALU.is_equal), [n("L"), n("gm8")], [n("ohg")])
    V("tensor_scalar", (dd, L[:, 0:8], gm8[:, 0:1], None, ALU.subtract), [n("L"), n("gm8")], [n("dd")])
    k.act(dd, dd, AF.Sigmoid, [n("dd")], [n("dd")])
    V("tensor_scalar", (tm8, dd, -1.0, 1.0, ALU.mult, ALU.add), [n("dd")], [n("tm8")])
    V("reciprocal", (tm8, tm8), [n("tm8")], [n("tm8")])
    V("tensor_tensor", (dd, dd, tm8, ALU.mult), [n("dd"), n("tm8")], [n("dd")])
    V("tensor_reduce", (pg, dd, AX.X, ALU.add), [n("dd")], [n("pg")])
    V("reciprocal", (pg, pg), [n("pg")], [n("pg")])
    t3 = t64.rearrange("p (g e) -> p g e", g=8)
    L3 = L[:, 8:72].rearrange("p (g e) -> p g e", g=8)
    V("tensor_tensor", (t3, L3, ohg.unsqueeze(2).to_broadcast([128, 8, 8]), ALU.mult), [n("L"), n("ohg")], [n("t64")])
    V("tensor_reduce", (el, t64.rearrange("p (g e) -> p e g", g=8), AX.X, ALU.add), [n("t64")], [n("el")])
    V("max", (tm8, el), [n("el"), n("tm8")], [n("tm8")])
    V("tensor_scalar", (oh1, el, tm8[:, 0:1], None, ALU.is_equal), [n("el"), n("tm8")], [n("oh1")])
    V("tensor_scalar", (oh2, el, tm8[:, 1:2], None, ALU.is_equal), [n("el"), n("tm8")], [n("oh2")])
    V("tensor_tensor", (dv, tm8[:, 0:1], tm8[:, 1:2], ALU.subtract), [n("tm8")], [n("dv")])
    k.act(dv, dv, AF.Sigmoid, [n("dv")], [n("dv")])
    V("tensor_tensor", (rw[:, ti, 0:1], dv, pg, ALU.mult), [n("dv"), n("pg")], [("rw", ti, 0)])
    V("tensor_tensor", (rw[:, ti, 1:2], pg, rw[:, ti, 0:1], ALU.subtract), [n("pg"), ("rw", ti, 0)], [("rw", ti, 1)])
    A1 = k.A1
    A2 = k.A2
    Ab = k.Ab
    A13 = A1.rearrange("p (g e) -> p g e", g=8)
    A23 = A2.rearrange("p (g e) -> p g e", g=8)
    gb = ohg.unsqueeze(2).to_broadcast([128, 8, 8])
    V("tensor_tensor", (A13, gb, oh1.unsqueeze(1).to_broadcast([128, 8, 8]), ALU.mult), [n("ohg"), n("oh1")], [n("A1")])
    V("tensor_tensor", (A23, gb, oh2.unsqueeze(1).to_broadcast([128, 8, 8]), ALU.mult), [n("ohg"), n("oh2")], [n("A2")])
    V("tensor_tensor", (Ab, A1, A2, ALU.add), [n("A1"), n("A2")], [n("Ab")])
    if stage == 1:
        return
    _route_p2(k, ps, sm, ti, slots_i, atot, ustrict, ones_b, ect, pl)


def _route_p2(k, ps, sm, ti, slots_i, atot, ustrict, ones_b, ect, pl):
    t64 = sm[:, 146:210]
    sl = sm[:, 210:212]
    n = lambda s: ("rt_" + s,)
    V = lambda name, args, r, w: k.v("vector", name, args, r, w)
    A1 = k.A1
    A2 = k.A2
    Ab = k.Ab
    k.mm(ps[pl][:, 128:192], ustrict, Ab, True, False, ["ustrict", n("Ab")], [("ps", pl)])
    k.mm(ps[pl][:, 128:192], ones_b, atot, False, True, ["ones_b", "atot"], [("ps", pl)])
    V("tensor_tensor", (t64, ps[pl][:, 128:192], ect, ALU.add), [("ps", pl), "ect"], [n("t64")])
    V("tensor_tensor", (atot, atot, Ab, ALU.add), [n("Ab"), "atot"], ["atot"])
    V("tensor_tensor", (A1, A1, t64, ALU.mult), [n("A1"), n("t64")], [n("A1")])
    V("tensor_tensor", (A2, A2, t64, ALU.mult), [n("A2"), n("t64")], [n("A2")])
    V("tensor_reduce", (sl[:, 0:1], A1, AX.X, ALU.add), [n("A1")], [n("sl0")])
    V("tensor_reduce", (sl[:, 1:2], A2, AX.X, ALU.add), [n("A2")], [n("sl1")])
    V("tensor_copy", (slots_i[:, ti, :], sl), [n("sl0"), n("sl1")], [("slots", ti)])


_NC_CACHE = {}


def _consts(hf):
    bf = ml_dtypes.bfloat16
    c = {}
    c["c_identf"] = np.eye(128, dtype=np.float32)
    c["c_identb"] = np.eye(128, dtype=np.float32).astype(bf)
    c["c_ones"] = np.ones((128, 128), np.float32).astype(bf)
    c["c_ustrict"] = np.triu(np.ones((128, 128), np.float32), 1).astype(bf)
    row = np.arange(128)
    k2 = np.arange(128)
    ed = np.zeros((64, 128, 256), np.float32)
    for j in range(64):
        s = j + 64 * row
        th = 2.0 * np.pi * ((s[:, None] * k2[None, :]) % 8192) / 8192.0
        ed[j, :, 0:128] = np.cos(th) / 32.0
        ed[j, :, 128:256] = np.sin(th) / 32.0
    c["c_edft"] = ed.astype(bf)
    ph = 2.0 * np.pi * ((row[:, None] * row[None, :]) % 128) / 128.0
    C = np.cos(ph); S = np.sin(ph)
    c["c_fca"] = np.concatenate([C, -S], axis=1).astype(np.float32).astype(bf)
    c["c_fcb"] = np.concatenate([-S, -C], axis=1).astype(np.float32).astype(bf)
    s1 = np.arange(64)
    k1 = 32 * hf + np.arange(32)
    psi = 2.0 * np.pi * ((s1[:, None] * k1[None, :]) % 64) / 64.0
    bdc = np.zeros((128, 64), np.float32); bds = np.zeros((128, 64), np.float32)
    for a in range(2):
        bdc[a::2, a * 32:(a + 1) * 32] = np.cos(psi) / 32.0
        bds[a::2, a * 32:(a + 1) * 32] = np.sin(psi) / 32.0
    c["c_bdc"] = bdc.astype(bf); c["c_bds"] = bds.astype(bf)
    c["c_ec"] = np.tile((np.arange(64, dtype=np.float32) * CAP)[None, :], (128, 1)).astype(np.float32)
    c["c_mhalf"] = np.full((128, 1), -0.5, np.float32)
    c["c_onesrow"] = np.ones((1, 128), np.float32)
    return c


def _bias_tiles(rpb, hf):
    out = np.full((5, 8, 128, 768), MASKV, np.float32)
    specs = [(10, [-2, -1, 0, 1, 2]), (0, [-2, -1, 0, 1, 2, 3]), (1, [-2, -1, 0, 1, 2]),
             (30, [-2, -1, 0, 1, 2]), (31, [-3, -2, -1, 0, 1, 2])]
    qc = np.arange(64); kc = np.arange(64)
    cs = np.clip(qc - 8, 0, 48)
    colvalid = (kc[:, None] >= cs[None, :]) & (kc[:, None] < cs[None, :] + 16)
    dc = kc[:, None] - qc[None, :] + 15
    dcc = np.clip(dc, 0, 30)
    for ty, (qp, offs) in enumerate(specs):
        for j, off in enumerate(offs):
            kp = qp + off
            for aq in range(2):
                r = 64 * hf + 2 * qp + aq
                rs = min(max(r - 4, 0), 120)
                for ak in range(2):
                    kr = 64 * hf + 2 * kp + ak
                    if kr < 0 or kr > 127 or kr < rs or kr > rs + 7:
                        continue
                    drr = kr - r + 7
                    vals = rpb[:, drr, :][:, dcc]
                    blk = np.where(colvalid[None], vals, np.float32(MASKV))
                    out[ty, :, ak * 64:(ak + 1) * 64, j * 128 + aq * 64:j * 128 + (aq + 1) * 64] = blk
    return out


def kernel(x, mem, w_in, w_gate, b_gate, w_mem_kv, rpb, w_fourier_o, w_na_o, w_mem_o, w_out, ln1_g, ln1_b,
           w_router_group, b_router_group, w_router_expert, b_router_expert, w_exp_gate, w_exp_up, w_exp_down,
           ln2_g, ln2_b, _debug=False):
    f = lambda a: np.ascontiguousarray(np.asarray(a, dtype=np.float32))
    x = f(x); mem = f(mem)
    key = bool(_debug)
    if key not in _NC_CACHE:
        _NC_CACHE[key] = build_program(debug=_debug)
    nc = _NC_CACHE[key]
    shared = {
        "w_in": f(w_in[0]), "w_gate": f(w_gate[0]),
        "bg": f(np.asarray(b_gate[0]).reshape(24, 128).T),
        "w_kv": f(w_mem_kv[0]), "w_fo": f(w_fourier_o[0]), "w_nao": f(w_na_o[0]), "w_memo": f(w_mem_o[0]),
        "w_out": f(w_out[0]), "ln1_g": f(ln1_g), "ln1_b": f(ln1_b), "ln2_g": f(ln2_g), "ln2_b": f(ln2_b),
        "wr": f(np.concatenate([np.asarray(w_router_group[0]), np.asarray(w_router_expert[0])], axis=1)),
        "br": f(np.concatenate([np.asarray(b_router_group[0]), np.asarray(b_router_expert[0])])[None, :]),
        "w_eg": f(w_exp_gate[0]), "w_eu": f(w_exp_up[0]), "w_ed": f(w_exp_down[0]),
    }
    rp = f(rpb[0])
    in_maps = []
    for c in range(NCORES):
        b, hf = c // 2, c % 2
        m = dict(shared)
        m.update(_consts(hf))
        m["xf"] = x[b]
        xo = np.zeros((4608, D), np.float32)
        xo[0:4096] = x[b, 4096 * hf:4096 * hf + 4096]
        if hf == 1:
            xo[4096:4352] = x[b, 4096 - 256:4096]
        else:
            xo[4352:4608] = x[b, 4096:4096 + 256]
        m["xo"] = xo
        m["mem"] = mem[b]
        m["biasT"] = _bias_tiles(rp, hf)
        in_maps.append(m)
    res = run_bass_kernel_spmd(nc, in_maps, core_ids=list(range(NCORES)))
    out = np.zeros((4, 8192, D), np.float32)
    for c in range(NCORES):
        b, hf = c // 2, c % 2
        out[b, 4096 * hf:4096 * hf + 4096] = res.results[c]["out"]
    if _debug:
        return out, res
    return out
```

```python
import contextlib
import math
import numpy as np
import ml_dtypes
import concourse.bass as bass
import concourse.mybir as mybir
from concourse.bass_utils import run_bass_kernel_spmd

F32 = mybir.dt.float32
BF16 = mybir.dt.bfloat16
I32 = mybir.dt.int32
U8 = mybir.dt.uint8
ALU = mybir.AluOpType
AF = mybir.ActivationFunctionType
AX = mybir.AxisListType

ENGS = ("tensor", "vector", "scalar", "gpsimd", "sync")
NCORES = 8
D = 1024
CAP = 256
NEXP = 64
NSLOT = NEXP * CAP
ALPHA = 2.0 ** 0.25
LN_EPS = 1e-5
MASKV = -30000.0
ARENA = 206 * 1024


class Op:
    __slots__ = ("eng", "fn", "dma", "deps", "sig", "sigval", "key", "dval", "slot")

    def __init__(self, eng, fn, dma):
        self.eng = eng
        self.fn = fn
        self.dma = dma
        self.deps = []
        self.sig = False
        self.sigval = 0
        self.key = None
        self.dval = 0
        self.slot = None


class Prog:
    def __init__(self, nc):
        self.nc = nc
        self.ops = {e: [] for e in ENGS}
        self.writers = {}
        self.readers = {}
        self.key_slot = {}
        self.slot_count = []
        self.nops = 0

    def op(self, eng, fn, reads=(), writes=(), dma=False, key=None, partial=False):
        o = Op(eng, fn, dma)
        self.nops += 1
        deps = {}
        banks = set()
        for b in list(reads) + list(writes):
            if isinstance(b, tuple) and b and b[0] == "ps":
                banks.add(b[1])
        for bk in banks:
            nm = ("psbank", bk)
            for w in self.writers.get(nm, ()):
                deps[id(w)] = w
            self.writers[nm] = [o]
        for b in reads:
            for w in self.writers.get(b, ()):
                deps[id(w)] = w
        for b in writes:
            if not partial:
                for w in self.writers.get(b, ()):
                    deps[id(w)] = w
            for r in self.readers.get(b, ()):
                deps[id(r)] = r
        for b in reads:
            self.readers.setdefault(b, []).append(o)
        for b in writes:
            if partial:
                self.writers.setdefault(b, []).append(o)
            else:
                self.writers[b] = [o]
                self.readers[b] = []
        deps.pop(id(o), None)
        o.deps = list(deps.values())
        if dma:
            if key is None:
                key = writes[0] if writes else reads[0]
            if key not in self.key_slot:
                self.key_slot[key] = len(self.key_slot)
                if len(self.key_slot) > len(self.slot_count):
                    self.slot_count.append(0)
            s = self.key_slot[key]
            self.slot_count[s] += 1
            o.slot = s
            o.dval = 16 * self.slot_count[s]
        self.ops[eng].append(o)
        return o

    def barrier(self):
        lasts = []
        for e in ENGS:
            nd = [o for o in self.ops[e] if not o.dma and o.fn is not None]
            if nd:
                lasts.append(nd[-1])
        dmas = {}
        for e in ENGS:
            for o in self.ops[e]:
                if o.dma and (o.slot not in dmas or dmas[o.slot].dval < o.dval):
                    dmas[o.slot] = o
        for e in ENGS:
            o = Op(e, None, False)
            o.deps = [l for l in lasts if l.eng != e] + list(dmas.values())
            self.ops[e].append(o)
        self.writers = {}
        self.readers = {}
        self.key_slot = {}

    def emit(self):
        nc = self.nc
        for e in ENGS:
            for o in self.ops[e]:
                for d in o.deps:
                    if not d.dma and not (d.eng == "tensor" and o.eng == "tensor"):
                        d.sig = True
        for e in ENGS:
            c = 0
            for o in self.ops[e]:
                if o.sig:
                    c += 1
                    o.sigval = c
        nslots = len(self.slot_count)
        with contextlib.ExitStack() as st:
            esem = {e: st.enter_context(nc.semaphore("e_" + e)) for e in ENGS}
            ksem = [st.enter_context(nc.semaphore("d%d" % i)) for i in range(nslots)]
            block = st.enter_context(nc.Block())

            def body(e):
                def _f(eng):
                    waited = {}
                    for o in self.ops[e]:
                        for d in o.deps:
                            if d.dma:
                                s, v = ksem[d.slot], d.dval
                            elif d.sig:
                                s, v = esem[d.eng], d.sigval
                            else:
                                continue
                            if waited.get(id(s), 0) >= v:
                                continue
                            waited[id(s)] = v
                            eng.wait_ge(s, v)
                        if o.fn is None:
                            continue
                        ins = o.fn(eng)
                        if o.dma:
                            ins.then_inc(ksem[o.slot], 16)
                        elif o.sig:
                            ins.then_inc(esem[e], 1)
                    if e == "sync":
                        for i in range(nslots):
                            v = 16 * self.slot_count[i]
                            if waited.get(id(ksem[i]), 0) < v:
                                eng.wait_ge(ksem[i], v)
                return _f

            block.sync(body("sync"))
            block.tensor(body("tensor"))
            block.vector(body("vector"))
            block.scalar(body("scalar"))
            block.gpsimd(body("gpsimd"))


def _isz(dt):
    return {F32: 4, BF16: 2, I32: 4, U8: 1}[dt]


class Arena:
    def __init__(self, ap, size):
        self.ap = ap
        self.size = size
        self.off = 0

    def alloc(self, shape, dt):
        n = int(np.prod(shape))
        nb = n * _isz(dt)
        off = self.off
        self.off += (nb + 63) // 64 * 64
        assert self.off <= self.size, ("arena overflow", self.off, self.size)
        v = self.ap[:, off:off + nb].bitcast(dt)
        if len(shape) == 2:
            v = v.rearrange("p (a b) -> p a b", a=shape[0], b=shape[1])
        elif len(shape) == 3:
            v = v.rearrange("p (a b c) -> p a b c", a=shape[0], b=shape[1], c=shape[2])
        elif len(shape) == 4:
            v = v.rearrange("p (a b c d) -> p a b c d", a=shape[0], b=shape[1], c=shape[2], d=shape[3])
        return v

    def mark(self):
        return self.off

    def reset(self, m):
        self.off = m


class K:
    def __init__(self, nc, P):
        self.nc = nc
        self.P = P
        self.ev = 0

    def mm(self, out, lhsT, rhs, start, stop, reads, writes):
        self.P.op("tensor", lambda e: e.matmul(out, lhsT, rhs, start=start, stop=stop),
                  reads=reads, writes=writes, partial=not start)

    def mmi(self, groups):
        n = max(len(g) for g in groups)
        for i in range(n):
            for g in groups:
                if i < len(g):
                    out, lhsT, rhs, reads, writes = g[i]
                    self.mm(out, lhsT, rhs, i == 0, i == len(g) - 1, reads, writes)

    def tri(self, groups):
        n = max(len(g) for g in groups)
        for i in range(n):
            for g in groups:
                if i < len(g):
                    out, in_, ident, reads, writes = g[i]
                    self.tr(out, in_, ident, reads, writes, partial=(i > 0))

    def tr(self, out, in_, ident, reads, writes, partial=True):
        self.P.op("tensor", lambda e: e.transpose(out, in_, ident), reads=reads, writes=writes,
                  partial=partial)

    def act(self, out, in_, func, reads, writes, bias=None, scale=None, partial=False):
        def f(e):
            kw = {}
            if bias is not None:
                kw["bias"] = bias
            if scale is not None:
                kw["scale"] = scale
            return e.activation(out, in_, func, **kw)
        self.P.op("scalar", f, reads=reads, writes=writes, partial=partial)

    def evac(self, out, in_, reads, writes, eng=None, partial=False):
        if eng is None:
            self.ev += 1
            eng = "vector" if self.ev % 2 else "scalar"
        if eng == "vector":
            self.P.op("vector", lambda e: e.tensor_copy(out, in_), reads=reads, writes=writes, partial=partial)
        else:
            self.P.op("scalar", lambda e: e.activation(out, in_, AF.Copy), reads=reads, writes=writes,
                      partial=partial)

    def v(self, eng, name, args, reads, writes, kw=None, partial=False):
        kw = kw or {}
        self.P.op(eng, lambda e: getattr(e, name)(*args, **kw), reads=reads, writes=writes, partial=partial)

    def dma(self, q, out, in_, reads, writes, key=None, partial=False):
        self.P.op(q, lambda e: e.dma_start(out=out, in_=in_), reads=reads, writes=writes, dma=True,
                  key=key, partial=partial)


def build_program(debug=False):
    nc = bass.Bass("TRN2", target_bir_lowering=False)
    _bc = {}

    def _bc_reg(e):
        if "r" not in _bc:
            _bc["r"] = e.to_reg(NSLOT - 1)
        return _bc["r"]
    dr = {}

    def din(name, shape, dt=F32):
        dr[name] = nc.dram_tensor(name, list(shape), dt, kind="ExternalInput").ap()

    def dscr(name, shape, dt):
        dr[name] = nc.dram_tensor(name, list(shape), dt, kind="Internal").ap()

    din("xf", [8192, D]); din("xo", [4608, D]); din("mem", [256, D])
    din("w_in", [D, 2560]); din("w_gate", [D, 3072]); din("bg", [128, 24])
    din("w_kv", [D, 1024]); din("w_fo", [512, D]); din("w_nao", [512, D]); din("w_memo", [512, D])
    din("w_out", [D, D]); din("ln1_g", [1, D]); din("ln1_b", [1, D]); din("ln2_g", [1, D]); din("ln2_b", [1, D])
    din("wr", [D, 72]); din("br", [1, 72])
    din("w_eg", [NEXP, D, 512]); din("w_eu", [NEXP, D, 512]); din("w_ed", [NEXP, 512, D])
    din("biasT", [5, 8, 128, 768])
    din("c_identf", [128, 128]); din("c_identb", [128, 128], BF16); din("c_ones", [128, 128], BF16)
    din("c_ustrict", [128, 128], BF16); din("c_edft", [64, 128, 256], BF16)
    din("c_fca", [128, 256], BF16); din("c_fcb", [128, 256], BF16)
    din("c_bdc", [128, 64], BF16); din("c_bds", [128, 64], BF16)
    din("c_ec", [128, 64]); din("c_mhalf", [128, 1]); din("c_onesrow", [1, 128])
    dr["out"] = nc.dram_tensor("out", [4096, D], F32, kind="ExternalOutput").ap()
    dscr("yfT_d", [128, 4, 4096], BF16); dscr("ynaT_d", [128, 4, 4096], BF16); dscr("ymemT_d", [128, 4, 4096], BF16)
    dscr("xT_d", [8, 128, 8, 512], BF16); dscr("qmT_d", [8, 128, 4, 512], BF16)
    dscr("h_d", [4096, D], F32); dscr("xg_d", [NSLOT, D], BF16); dscr("yb_d", [NSLOT, D], BF16)
    if debug:
        dr["dbg_h"] = nc.dram_tensor("dbg_h", [4096, D], F32, kind="ExternalOutput").ap()
        dr["dbg_y3"] = nc.dram_tensor("dbg_y3", [3, 128, 4, 4096], BF16, kind="ExternalOutput").ap()
        dr["dbg_rt"] = nc.dram_tensor("dbg_rt", [128, 32, 4], F32, kind="ExternalOutput").ap()

    P = Prog(nc)
    k = K(nc, P)
    with contextlib.ExitStack() as st:
        arena_t = st.enter_context(nc.sbuf_tensor("arena", [128, ARENA], U8))
        A = Arena(arena_t, ARENA)
        ps = [st.enter_context(nc.psum_tensor("ps%d" % i, [128, 512], F32)) for i in range(8)]
        psb = [p[:].bitcast(BF16) for p in ps]

        identf = A.alloc([128], F32); identb = A.alloc([128], BF16); ones_b = A.alloc([128], BF16)
        ustrict = A.alloc([128], BF16)
        slots_i = A.alloc([32, 2], I32); rw = A.alloc([32, 2], F32)
        k.dma("sync", identf, dr["c_identf"], [], ["identf"])
        k.dma("sync", identb, dr["c_identb"], [], ["identb"])
        k.dma("sync", ones_b, dr["c_ones"], [], ["ones_b"])
        k.dma("sync", ustrict, dr["c_ustrict"], [], ["ustrict"])
        base_mark = A.mark()

        wf = A.alloc([8, 512], BF16)
        fca = A.alloc([256], BF16); fcb = A.alloc([256], BF16); bdc = A.alloc([64], BF16); bds = A.alloc([64], BF16)
        AT = [A.alloc([2, 64, 64, 2], BF16) for _ in range(4)]
        xt = [A.alloc([1024], F32) for _ in range(4)]
        xT = [A.alloc([8, 128], BF16) for _ in range(2)]
        ub = [A.alloc([512], BF16) for _ in range(2)]
        Ej = [A.alloc([256], BF16) for _ in range(4)]
        Bt = [A.alloc([256], BF16) for _ in range(4)]
        yfs = [A.alloc([4096], BF16) for _ in range(2)]
        k.dma("gpsimd", wf, dr["w_in"][:, 0:512].rearrange("(c p) n -> p c n", p=128), [], ["wf"])
        k.dma("sync", fca, dr["c_fca"], [], ["fca"]); k.dma("sync", fcb, dr["c_fcb"], [], ["fcb"])
        k.dma("sync", bdc, dr["c_bdc"], [], ["bdc"]); k.dma("sync", bds, dr["c_bds"], [], ["bds"])
        xf_v = dr["xf"].rearrange("(r c) f -> c r f", c=64)

        def a_load(j):
            k.dma("sync", xt[j % 4], xf_v[j], [], [("xt", j % 4)])
            k.dma("sync", Ej[j % 4], dr["c_edft"][j], [], [("Ej", j % 4)])
        a_load(0); a_load(1)
        for j0 in range(0, 64, 2):
            for j in (j0, j0 + 1):
                if j + 2 < 64:
                    a_load(j + 2)
            tg = []
            for j in (j0, j0 + 1):
                for hb in range(2):
                    bank = 2 * (j % 2) + hb
                    tg.append([(ps[bank][:, q * 128:(q + 1) * 128], xt[j % 4][:, (hb * 4 + q) * 128:(hb * 4 + q + 1) * 128],
                                identf, [("xt", j % 4), "identf"], [("ps", bank)]) for q in range(4)])
            k.tri(tg)
            for j in (j0, j0 + 1):
                for hb in range(2):
                    bank = 2 * (j % 2) + hb
                    k.evac(xT[j % 2][:, hb * 4:(hb + 1) * 4, :], ps[bank][:].rearrange("p (a b) -> p a b", a=4),
                           [("ps", bank)], [("xT", j % 2, hb)])
            k.mmi([[(ps[4 + (j % 2)][:], xT[j % 2][:, fc, :], wf[:, fc, :],
                     [("xT", j % 2, 0), ("xT", j % 2, 1), "wf"], [("ps", 4 + (j % 2))]) for fc in range(8)]
                   for j in (j0, j0 + 1)])
            for j in (j0, j0 + 1):
                k.evac(ub[j % 2], ps[4 + (j % 2)][:], [("ps", 4 + (j % 2))], [("ub", j % 2)])
            for j in (j0, j0 + 1):
                for g in range(4):
                    pa = 6 + (g % 2)
                    k.mm(ps[pa][:, 0:256], ub[j % 2][:, g * 128:(g + 1) * 128], Ej[j % 4], True, True,
                         [("ub", j % 2), ("Ej", j % 4)], [("ps", pa)])
                    k.evac(AT[g][:, :, :, j, :], ps[pa][:, 0:256].rearrange("p (r k a) -> p r k a", r=2, k=64, a=2),
                           [("ps", pa)], [("AT", g)], partial=True)
        it = 0
        for g in range(4):
            ys = yfs[g % 2]
            ysv = ys.rearrange("p (k q) -> p k q", k=32)
            for kk0 in range(0, 64, 2):
                kks = (kk0, kk0 + 1)
                k.mmi([[(ps[kk % 2][:, 0:256], AT[g][:, 0, kk, :, :].rearrange("p s a -> p (s a)"), fca,
                         [("AT", g), "fca"], [("ps", kk % 2)]),
                        (ps[kk % 2][:, 0:256], AT[g][:, 1, kk, :, :].rearrange("p s a -> p (s a)"), fcb,
                         [("AT", g), "fcb"], [("ps", kk % 2)])] for kk in kks])
                bts = {}
                for kk in kks:
                    bts[kk] = it % 4
                    k.evac(Bt[it % 4], ps[kk % 2][:, 0:256], [("ps", kk % 2)], [("Bt", it % 4)])
                    it += 1
                k.mmi([[(ps[2 + kk % 2][:, 0:64], Bt[bts[kk]][:, 0:128], bdc, [("Bt", bts[kk]), "bdc"], [("ps", 2 + kk % 2)]),
                        (ps[2 + kk % 2][:, 0:64], Bt[bts[kk]][:, 128:256], bds, [("Bt", bts[kk]), "bds"], [("ps", 2 + kk % 2)])]
                       for kk in kks])
                for kk in kks:
                    k.evac(ysv[:, :, 2 * kk:2 * kk + 2].rearrange("p k a -> p a k"),
                           ps[2 + kk % 2][:, 0:64].rearrange("p (a k) -> p a k", a=2),
                           [("ps", 2 + kk % 2)], [("yfs", g % 2)], partial=True)
            k.dma("sync", dr["yfT_d"][:, g, :], ys, [("yfs", g % 2)], [("yfT_d", g)])
        P.barrier()
        A.reset(base_mark)

        kT = A.alloc([4, 4608], BF16)
        Vext = A.alloc([36, 8, 65], BF16)
        qT = A.alloc([4, 4096], BF16)
        b1_mark = A.mark()
        W = A.alloc([8, 2048], BF16)
        xt = [A.alloc([1024], F32) for _ in range(4)]
        xTq = [A.alloc([8, 512], BF16) for _ in range(2)]
        qmq = [A.alloc([4, 512], BF16) for _ in range(2)]
        for cb in range(4):
            k.dma("gpsimd", W[:, :, cb * 512:(cb + 1) * 512],
                  dr["w_in"][:, 512 + cb * 512:512 + (cb + 1) * 512].rearrange("(c p) n -> p c n", p=128),
                  [], [("W", cb)])
        Wr = [("W", cb) for cb in range(4)]
        k.v("vector", "memset", (Vext[:, :, :, 64:65], 1.0), [], ["Vext1"])
        xo_t = dr["xo"].rearrange("(t p) f -> t p f", p=128)

        def b_load(t):
            k.dma("sync", xt[t % 4], xo_t[t], [], [("xt", t % 4)])
        b_load(0); b_load(1)
        for Q in range(9):
            xq = xTq[Q % 2]
            for tt0 in range(0, 4, 2):
                tts = (tt0, tt0 + 1)
                for tt in tts:
                    t = 4 * Q + tt
                    if t + 2 < 36:
                        b_load(t + 2)
                tg = []
                for tt in tts:
                    t = 4 * Q + tt
                    for hb in range(2):
                        bank = 2 * (t % 2) + hb
                        tg.append([(ps[bank][:, q * 128:(q + 1) * 128],
                                    xt[t % 4][:, (hb * 4 + q) * 128:(hb * 4 + q + 1) * 128], identf,
                                    [("xt", t % 4), "identf"], [("ps", bank)]) for q in range(4)])
                k.tri(tg)
                for tt in tts:
                    t = 4 * Q + tt
                    for hb in range(2):
                        bank = 2 * (t % 2) + hb
                        k.evac(xq[:, hb * 4:(hb + 1) * 4, tt * 128:(tt + 1) * 128],
                               ps[bank][:].rearrange("p (a b) -> p a b", a=4),
                               [("ps", bank)], [("xTq", Q % 2)], partial=True)
            xr = [("xTq", Q % 2)] + Wr

            def proj_pair(col0, cs, banks):
                k.mmi([[(ps[banks[i]][:], W[:, fc, col0 + c * 128:col0 + (c + 1) * 128], xq[:, fc, :], xr,
                         [("ps", banks[i])]) for fc in range(8)] for i, c in enumerate(cs)])
            for cs, banks in (((0, 1), (4, 5)), ((2, 3), (6, 7))):
                proj_pair(512, cs, banks)
                for i, c in enumerate(cs):
                    k.evac(kT[:, c, Q * 512:(Q + 1) * 512], ps[banks[i]][:], [("ps", banks[i])], [("kT", Q)],
                           partial=True)
            if Q < 8:
                for cs, banks in (((0, 1), (4, 5)), ((2, 3), (6, 7))):
                    proj_pair(0, cs, banks)
                    for i, c in enumerate(cs):
                        k.act(qT[:, c, Q * 512:(Q + 1) * 512], ps[banks[i]][:], AF.Copy, [("ps", banks[i])],
                              [("qT", Q)], scale=0.125, partial=True)
                for cs, banks in (((0, 1), (4, 5)), ((2, 3), (6, 7))):
                    proj_pair(1536, cs, banks)
                    for i, c in enumerate(cs):
                        k.evac(qmq[Q % 2][:, c, :], ps[banks[i]][:], [("ps", banks[i])], [("qmq", Q % 2)],
                               partial=True)
                k.dma("sync", dr["qmT_d"][Q], qmq[Q % 2], [("qmq", Q % 2)], [("qmT_d", Q)])
                k.dma("sync", dr["xT_d"][Q], xq, [("xTq", Q % 2)], [("xT_d", Q)])
            for tts, banks in (((0, 1), (4, 5)), ((2, 3), (6, 7))):
                k.mmi([[(ps[banks[i]][:], xq[:, fc, tt * 128:(tt + 1) * 128], W[:, fc, 1024:1536], xr,
                         [("ps", banks[i])]) for fc in range(8)] for i, tt in enumerate(tts)])
                for i, tt in enumerate(tts):
                    t = 4 * Q + tt
                    k.evac(Vext[:, t, :, 0:64], ps[banks[i]][:].rearrange("p (h d) -> p h d", h=8),
                           [("ps", banks[i])], [("Vext", t)])
        P.barrier()
        A.reset(b1_mark)

        bias_i = A.alloc([8, 768], BF16)
        bias_s = [A.alloc([8, 768], BF16) for _ in range(2)]
        eS = [A.alloc([768], BF16) for _ in range(4)]
        yna = [A.alloc([512], BF16) for _ in range(2)]
        rc = [A.alloc([8], F32) for _ in range(2)]
        ynaTq = [A.alloc([4, 512], BF16) for _ in range(2)]
        for hh in range(8):
            k.dma("gpsimd", bias_i[:, hh, :], dr["biasT"][0, hh], [], [("bias_i", hh)])
            k.act(bias_i[:, hh, :], bias_i[:, hh, :], AF.Exp, [("bias_i", hh)], [("bias_i", hh)])
        special = {0: 1, 1: 2, 30: 3, 31: 4}

        def tile_of_pair(lp):
            if 0 <= lp < 32:
                return lp
            return {-2: 32, -1: 33, 32: 34, 33: 35}[lp]
        nsp = 0

        def offs_of(qp):
            if qp == 0:
                return [-2, -1, 0, 1, 2, 3]
            if qp == 31:
                return [-3, -2, -1, 0, 1, 2]
            return [-2, -1, 0, 1, 2]
        bias_of = {}

        def na_finalize(qp):
            yq = yna[qp % 2]
            rcq = rc[qp % 2]
            for h2 in range(8):
                po2 = 4 + (qp % 2) * 2 + (h2 // 4)
                oc2 = (h2 % 4) * 65
                k.v("vector", "reciprocal", (rcq[:, h2:h2 + 1], ps[po2][:, oc2 + 64:oc2 + 65]),
                    [("ps", po2, h2 % 4)], [("rc", qp % 2, h2)])
                k.v("vector", "tensor_scalar", (yq[:, h2 * 64:(h2 + 1) * 64], ps[po2][:, oc2:oc2 + 64],
                                                 rcq[:, h2:h2 + 1], None, ALU.mult),
                    [("ps", po2, h2 % 4), ("rc", qp % 2, h2)], [("yna", qp % 2)], partial=True)

        def na_transposes(qp):
            yq = yna[qp % 2]
            Q = qp // 4
            pT = 4 + (qp % 2) * 2 + 1
            tv = psb[pT][:, 640:1024]
            pT2 = 4 + (qp % 2) * 2
            tv2 = psb[pT2][:, 640:768]
            k.tr(tv[:, 0:128], yq[:, 0:128], identb, [("yna", qp % 2), "identb"], [("ps", pT, "T")], partial=False)
            k.tr(tv2, yq[:, 384:512], identb, [("yna", qp % 2), "identb"], [("ps", pT2, "T")], partial=False)
            for cc in range(1, 3):
                k.tr(tv[:, cc * 128:(cc + 1) * 128], yq[:, cc * 128:(cc + 1) * 128], identb,
                     [("yna", qp % 2), "identb"], [("ps", pT, "T")], partial=True)
            yt = ynaTq[Q % 2]
            k.evac(yt[:, 0:3, (qp % 4) * 128:(qp % 4 + 1) * 128], tv.rearrange("p (a b) -> p a b", a=3),
                   [("ps", pT, "T")], [("ynaTq", Q % 2)], partial=True)
            k.evac(yt[:, 3, (qp % 4) * 128:(qp % 4 + 1) * 128], tv2, [("ps", pT2, "T")], [("ynaTq", Q % 2)],
                   partial=True)
            if qp % 4 == 3:
                k.dma("sync", dr["ynaT_d"][:, :, Q * 512:(Q + 1) * 512], yt, [("ynaTq", Q % 2)], [("ynaT_d", Q)])

        def pv_list(n):
            qp, h = n // 8, n % 8
            offs = offs_of(qp); ns = len(offs)
            es = eS[n % 4]; en = ("eS", n % 4)
            po = 4 + (qp % 2) * 2 + (h // 4)
            oc = (h % 4) * 65
            out = []
            for j, off in enumerate(offs):
                kt = tile_of_pair(qp + off)
                out.append((ps[po][:, oc:oc + 65], es[:, j * 128:(j + 1) * 128], Vext[:, kt, h, :], j == 0, j == ns - 1,
                            [en, ("Vext", kt), "Vext1"], [("ps", po, h % 4)]))
            return out
        NH = 256
        for n in range(NH + 2):
            qk = []
            if n < NH:
                qp, h = n // 8, n % 8
                offs = offs_of(qp); ns = len(offs)
                if h == 0:
                    if qp in special:
                        bt_ = bias_s[nsp % 2]; bname = ("bias_s", nsp % 2)
                        for hh in range(8):
                            k.dma("gpsimd", bt_[:, hh, :], dr["biasT"][special[qp], hh], [], [bname], key=bname,
                                  partial=(hh > 0))
                        for hh in range(8):
                            k.act(bt_[:, hh, :], bt_[:, hh, :], AF.Exp, [bname], [bname])
                        bias_of[qp] = (bt_, [bname])
                        nsp += 1
                    else:
                        bias_of[qp] = (bias_i, None)
                bt_, breads = bias_of[qp]
                c = h // 2; pb0 = 64 * (h % 2)
                sX = (n % 2) * 2; sY = sX + 1
                br_ = breads if breads is not None else [("bias_i", h)]
                for j, off in enumerate(offs):
                    kt = tile_of_pair(qp + off)
                    bank = sX if j < 4 else sY
                    first = (j == 0) or (j == 4)
                    last = (j == 3) or (j == ns - 1)
                    qk.append((ps[bank][:, (j % 4) * 128:(j % 4 + 1) * 128],
                               kT[pb0:pb0 + 64, c, kt * 128:(kt + 1) * 128],
                               qT[pb0:pb0 + 64, c, qp * 128:(qp + 1) * 128], first, last,
                               [("kT", kt // 4), ("qT", qp // 4)], [("ps", bank)]))
            m = n - 2
            pv = pv_list(m) if m >= 0 else []
            for i in range(max(len(qk), len(pv))):
                if i < len(qk):
                    k.mm(*qk[i])
                if i < len(pv):
                    k.mm(*pv[i])
            if n < NH:
                es = eS[n % 4]; en = ("eS", n % 4)
                k.act(es[:, 0:512], ps[sX][:], AF.Exp, [("ps", sX)], [en])
                k.act(es[:, 512:ns * 128], ps[sY][:, 0:(ns - 4) * 128], AF.Exp, [("ps", sY)], [en], partial=True)
                k.v("gpsimd", "tensor_tensor", (es[:, 0:ns * 128], es[:, 0:ns * 128], bt_[:, h, 0:ns * 128], ALU.mult),
                    [en] + br_, [en])
            if m >= 0 and m % 8 == 7:
                na_finalize(m // 8)
            if m >= 0 and m % 8 == 3 and m // 8 >= 1:
                na_transposes(m // 8 - 1)
        na_transposes(31)
        P.barrier()
        A.reset(base_mark)

        Atop = Arena(arena_t, ARENA)
        Atop.off = ARENA - (48 + 24 + 16) * 1024
        wg = Atop.alloc([8, 3072], BF16)
        wo3 = [Atop.alloc([4, 1024], BF16) for _ in range(3)]
        wout = Atop.alloc([8, 1024], BF16)
        top_limit = ARENA - (48 + 24 + 16) * 1024
        for cb in range(6):
            k.dma("gpsimd", wg[:, :, cb * 512:(cb + 1) * 512],
                  dr["w_gate"][:, cb * 512:(cb + 1) * 512].rearrange("(c p) n -> p c n", p=128), [], [("wg", cb)])
        for i, nm in enumerate(("w_fo", "w_nao", "w_memo")):
            k.dma("gpsimd", wo3[i], dr[nm].rearrange("(c p) n -> p c n", p=128), [], [("wo3", i)])
        for cb in range(2):
            k.dma("gpsimd", wout[:, :, cb * 512:(cb + 1) * 512],
                  dr["w_out"][:, cb * 512:(cb + 1) * 512].rearrange("(c p) n -> p c n", p=128), [], [("wout", cb)])
        wkv = A.alloc([8, 1024], BF16)
        memt = [A.alloc([1024], F32) for _ in range(2)]
        memT = A.alloc([8, 256], BF16)
        kmT = A.alloc([4, 256], BF16)
        Vm = A.alloc([2, 4, 129], BF16)
        qmq = [A.alloc([4, 512], BF16) for _ in range(2)]
        eSm = [A.alloc([512], BF16) for _ in range(4)]
        ym = [A.alloc([512], BF16) for _ in range(8)]
        rcm = [A.alloc([4], F32) for _ in range(8)]
        ymTq = [A.alloc([4, 512], BF16) for _ in range(2)]
        assert A.off <= top_limit, ("B1c arena overlaps prefetched weights", A.off, top_limit)
        for cb in range(2):
            k.dma("gpsimd", wkv[:, :, cb * 512:(cb + 1) * 512],
                  dr["w_kv"][:, cb * 512:(cb + 1) * 512].rearrange("(c p) n -> p c n", p=128), [], [("wkv", cb)])
        k.v("vector", "memset", (Vm[:, :, :, 128:129], 1.0), [], ["Vm1"])
        for mc in range(2):
            k.dma("sync", memt[mc], dr["mem"][mc * 128:(mc + 1) * 128, :], [], [("memt", mc)])
            for hb in range(2):
                bank = 2 * mc + hb
                for q in range(4):
                    fc = hb * 4 + q
                    k.tr(ps[bank][:, q * 128:(q + 1) * 128], memt[mc][:, fc * 128:(fc + 1) * 128], identf,
                         [("memt", mc), "identf"], [("ps", bank)], partial=(q > 0))
                k.evac(memT[:, hb * 4:(hb + 1) * 4, mc * 128:(mc + 1) * 128],
                       ps[bank][:].rearrange("p (a b) -> p a b", a=4), [("ps", bank)], ["memT"], partial=True)
        for h in range(4):
            pb_ = 4 + h % 2
            for fc in range(8):
                k.mm(ps[pb_][:, 0:256], wkv[:, fc, h * 128:(h + 1) * 128], memT[:, fc, :], fc == 0, fc == 7,
                     ["memT", ("wkv", 0)], [("ps", pb_)])
            k.evac(kmT[:, h, :], ps[pb_][:, 0:256], [("ps", pb_)], ["kmT"], partial=True)
        for mc in range(2):
            pb_ = 6 + mc
            for fc in range(8):
                k.mm(ps[pb_][:], memT[:, fc, mc * 128:(mc + 1) * 128], wkv[:, fc, 512:1024], fc == 0, fc == 7,
                     ["memT", ("wkv", 1)], [("ps", pb_)])
            k.evac(Vm[:, mc, :, 0:128], ps[pb_][:].rearrange("p (h d) -> p h d", h=4), [("ps", pb_)], ["Vm"],
                   partial=True)
        msc = 128.0 ** -0.5
        k.dma("sync", qmq[0], dr["qmT_d"][0], [], [("qmq", 0)])
        ecnt = 0
        for Q in range(8):
            if Q + 1 < 8:
                k.dma("sync", qmq[(Q + 1) % 2], dr["qmT_d"][Q + 1], [], [("qmq", (Q + 1) % 2)])
            qq = qmq[Q % 2]
            for h in range(4):
                ebufs = []
                for mc in range(2):
                    bank = (ecnt % 2) * 2 + mc
                    k.mm(ps[bank][:], kmT[:, h, mc * 128:(mc + 1) * 128], qq[:, h, :], True, True,
                         ["kmT", ("qmq", Q % 2)], [("ps", bank)])
                    eb = (ecnt % 2) * 2 + mc
                    k.act(eSm[eb], ps[bank][:], AF.Exp, [("ps", bank)], [("eSm", eb)], scale=msc)
                    ebufs.append(eb)
                for tts in ((0, 2), (1, 3)):
                    k.mmi([[(ps[4 + (tt // 2) + 2 * (ecnt % 2)][:, (tt % 2) * 129:(tt % 2) * 129 + 129],
                             eSm[ebufs[mc]][:, tt * 128:(tt + 1) * 128], Vm[:, mc, h, :],
                             [("eSm", ebufs[mc]), "Vm", "Vm1"], [("ps", 4 + (tt // 2) + 2 * (ecnt % 2), tt % 2)])
                            for mc in range(2)] for tt in tts])
                for tt in range(4):
                    po = 4 + (tt // 2) + 2 * (ecnt % 2)
                    oc = (tt % 2) * 129
                    yb_ = (Q % 2) * 4 + tt
                    k.v("vector", "reciprocal", (rcm[yb_][:, h:h + 1], ps[po][:, oc + 128:oc + 129]),
                        [("ps", po, tt % 2)], [("rcm", yb_, h)])
                    k.v("vector", "tensor_scalar", (ym[yb_][:, h * 128:(h + 1) * 128], ps[po][:, oc:oc + 128],
                                                     rcm[yb_][:, h:h + 1], None, ALU.mult),
                        [("ps", po, tt % 2), ("rcm", yb_, h)], [("ym", yb_)], partial=True)
                ecnt += 1
            yt = ymTq[Q % 2]
            for tt in range(4):
                yb_ = (Q % 2) * 4 + tt
                bank = (tt % 2) * 2 + (ecnt % 2)
                tvm = psb[bank][:, 0:512]
                for cc in range(4):
                    k.tr(tvm[:, cc * 128:(cc + 1) * 128], ym[yb_][:, cc * 128:(cc + 1) * 128], identb,
                         [("ym", yb_), "identb"], [("ps", bank)], partial=(cc > 0))
                k.evac(yt[:, :, tt * 128:(tt + 1) * 128], tvm.rearrange("p (a b) -> p a b", a=4), [("ps", bank)],
                       [("ymTq", Q % 2)], partial=True)
            k.dma("sync", dr["ymemT_d"][:, :, Q * 512:(Q + 1) * 512], yt, [("ymTq", Q % 2)], [("ymemT_d", Q)])
        P.barrier()
        A.reset(base_mark)

        wrt = A.alloc([8, 72], F32)
        bgt = A.alloc([24], F32)
        brt = A.alloc([72], F32); onesrow = A.alloc([128], F32)
        lng = A.alloc([1024], F32); lnb = A.alloc([1024], F32)
        ect = A.alloc([64], F32); mhalf = A.alloc([1], F32)
        atot = A.alloc([64], BF16)
        xTq = [A.alloc([8, 512], BF16) for _ in range(2)]
        ybq = [[A.alloc([4, 512], BF16) for _ in range(2)] for _ in range(3)]
        G = [[A.alloc([512], BF16) for _ in range(2)] for _ in range(3)]
        tm = [A.alloc([512], F32) for _ in range(4)]
        mT = A.alloc([8, 512], BF16)
        xt = [A.alloc([1024], F32) for _ in range(2)]
        rt = [A.alloc([1024], F32) for _ in range(2)]
        ht = [A.alloc([1024], F32) for _ in range(2)]
        hl = [A.alloc([1024], F32) for _ in range(2)]
        hb = [A.alloc([1024], BF16) for _ in range(2)]
        hT = A.alloc([8, 128], F32)
        sm = A.alloc([256], F32)
        sml = [A.alloc([16], F32) for _ in range(2)]
        k.A1 = A.alloc([64], F32); k.A2 = A.alloc([64], F32); k.Ab = A.alloc([64], BF16)
        assert A.off <= top_limit, ("B3 arena overlaps prefetched weights", A.off, top_limit)
        k.dma("sync", wrt, dr["wr"].rearrange("(c p) n -> p c n", p=128), [], ["wrt"])
        k.dma("sync", bgt, dr["bg"], [], ["bgt"])
        k.dma("sync", brt[0:1, :], dr["br"], [], ["brt"])
        k.dma("sync", onesrow[0:1, :], dr["c_onesrow"], [], ["onesrow"])
        k.dma("sync", lng, dr["ln1_g"].partition_broadcast(128)[:, 0, :], [], ["lng"])
        k.dma("sync", lnb, dr["ln1_b"].partition_broadcast(128)[:, 0, :], [], ["lnb"])
        k.dma("sync", ect, dr["c_ec"], [], ["ect"])
        k.dma("sync", mhalf, dr["c_mhalf"], [], ["mhalf"])
        k.v("vector", "memset", (atot, 0.0), [], ["atot"])
        ysrc = ("yfT_d", "ynaT_d", "ymemT_d")

        def c_load(Q):
            k.dma("sync", xTq[Q % 2], dr["xT_d"][Q], [], [("xTq", Q % 2)])
            for i in range(3):
                k.dma("sync", ybq[i][Q % 2], dr[ysrc[i]][:, :, Q * 512:(Q + 1) * 512], [], [("ybq", i, Q % 2)])
        c_load(0)
        wgr = [("wg", cb) for cb in range(6)]

        def r1_load(ti):
            b2 = ti % 2
            k.dma("sync", hl[b2], dr["h_d"][ti * 128:(ti + 1) * 128, :], [("h_d", ti)], [("hl", b2)])

        def r1(ti):
            b2 = ti % 2
            if ti % 4 >= 2:
                r1_load(ti)
            k.act(hb[b2], hl[b2], AF.Copy, [("hl", b2)], [("hb", b2)])
            k.tri([[(ps[6 + hh][:, q * 128:(q + 1) * 128], hl[b2][:, (hh * 4 + q) * 128:(hh * 4 + q + 1) * 128], identf,
                     [("hl", b2), "identf"], [("ps", 6 + hh)]) for q in range(4)] for hh in range(2)])
            for hh in range(2):
                k.evac(hT[:, hh * 4:(hh + 1) * 4, :], ps[6 + hh][:].rearrange("p (a b) -> p a b", a=4),
                       [("ps", 6 + hh)], [("hT", hh)])

        def r2(ti):
            for fc in range(8):
                k.mm(ps[7][:, 0:72], hT[:, fc, :], wrt[:, fc, :], fc == 0, False,
                     [("hT", 0), ("hT", 1), "wrt"], [("ps", 7)])
            k.mm(ps[7][:, 0:72], onesrow[0:1, :], brt[0:1, :], False, True, ["onesrow", "brt"], [("ps", 7)])

        def r3(ti):
            emit_route(k, ps, sm, ti, slots_i, rw, atot, ustrict, ones_b, ect, 7, 1)

        def r4(ti):
            b2 = ti % 2
            emit_route(k, ps, sm, ti, slots_i, rw, atot, ustrict, ones_b, ect, 7, 2)
            for kk in range(2):
                P.op("gpsimd", (lambda e, ti=ti, kk=kk, b2=b2: e.indirect_dma_start(
                    out=dr["xg_d"][:, :],
                    out_offset=bass.IndirectOffsetOnAxis(ap=slots_i[:, ti, kk:kk + 1], axis=0),
                    in_=hb[b2][:, :], in_offset=None, bounds_check=_bc_reg(e), oob_is_err=False)),
                    reads=[("hb", b2), ("slots", ti)], writes=["xg_d"], dma=True, key=("xg_sc", b2), partial=True)

        def route_sched(Qp):
            t = [Qp * 4 + i for i in range(4)]
            return [[lambda: r1(t[0])], [lambda: r2(t[0]), lambda: r3(t[0])],
                    [lambda: r4(t[0]), lambda: r1(t[1])], [lambda: r2(t[1]), lambda: r3(t[1])],
                    [lambda: r4(t[1]), lambda: r1(t[2])], [lambda: r2(t[2]), lambda: r3(t[2])],
                    [lambda: r4(t[2]), lambda: r1(t[3])], [lambda: r2(t[3]), lambda: r3(t[3])],
                    [lambda: r4(t[3])]]
        for Q in range(9):
            sched = route_sched(Q - 1) if Q >= 1 else [[] for _ in range(9)]
            if Q < 8:
                if Q + 1 < 8:
                    c_load(Q + 1)
                xq = xTq[Q % 2]
            for dc in range(8):
                if Q < 8:
                    k.mmi([[(ps[i][:], wg[:, fc, i * 1024 + dc * 128:i * 1024 + (dc + 1) * 128], xq[:, fc, :],
                             wgr + [("xTq", Q % 2)], [("ps", i)]) for fc in range(8)] for i in range(3)])
                    for i in range(3):
                        k.act(G[i][dc % 2], ps[i][:], AF.Sigmoid, [("ps", i), "bgt"], [("G", i, dc % 2)],
                              bias=bgt[:, i * 8 + dc:i * 8 + dc + 1])
                    k.mmi([[(ps[3 + i][:], wo3[i][:, cc, dc * 128:(dc + 1) * 128], ybq[i][Q % 2][:, cc, :],
                             [("wo3", i), ("ybq", i, Q % 2)], [("ps", 3 + i)]) for cc in range(4)] for i in range(3)])
                if Q < 8:
                    for i in range(3):
                        k.v("vector", "tensor_tensor", (tm[i], G[i][dc % 2], ps[3 + i][:], ALU.mult),
                            [("G", i, dc % 2), ("ps", 3 + i)], [("tm", i)])
                    k.v("gpsimd", "tensor_tensor", (tm[3], tm[0], tm[1], ALU.add), [("tm", 0), ("tm", 1)], [("tm", 3)])
                    k.v("gpsimd", "tensor_tensor", (mT[:, dc, :], tm[3], tm[2], ALU.add), [("tm", 3), ("tm", 2)],
                        [("mT", dc)])
                for f_ in sched[dc]:
                    f_()
            if Q == 8:
                for f_ in sched[8]:
                    f_()
                break
            mTr = [("mT", dc) for dc in range(8)]
            k.dma("sync", xt[0], xo_t[Q * 4], [], [("xt", 0)])

            def s1a(tt):
                ti = Q * 4 + tt
                b2 = ti % 2
                if tt + 1 < 4:
                    k.dma("sync", xt[(tt + 1) % 2], xo_t[ti + 1], [], [("xt", (tt + 1) % 2)])
                pbk = (6, 7) if tt % 2 == 0 else (4, 5)
                k.mmi([[(ps[pbk[half]][:], mT[:, dc, tt * 128:(tt + 1) * 128], wout[:, dc, half * 512:(half + 1) * 512],
                         mTr + [("wout", half)], [("ps", pbk[half])]) for dc in range(8)] for half in range(2)])
                for half in range(2):
                    pm = pbk[half]
                    k.v("vector", "scalar_tensor_tensor",
                        (rt[b2][:, half * 512:(half + 1) * 512], xt[tt % 2][:, half * 512:(half + 1) * 512], ALPHA,
                         ps[pm][:], ALU.mult, ALU.add),
                        [("xt", tt % 2), ("ps", pm)], [("rt", b2, half)])
                emit_ln_a(k, rt[b2], [("rt", b2, 0), ("rt", b2, 1)], sml[b2], mhalf, tag=b2)

            def s1b(tt):
                ti = Q * 4 + tt
                b2 = ti % 2
                emit_ln_b(k, rt[b2], [("rt", b2, 0), ("rt", b2, 1)], ht[b2], ("ht", b2), lng, lnb, ["lng", "lnb"],
                          sml[b2], tag=b2)
                k.dma("sync", dr["h_d"][ti * 128:(ti + 1) * 128, :], ht[b2], [("ht", b2)], [("h_d", ti)])
            s1a(0)
            s1a(1)
            s1b(0)
            r1_load(Q * 4)
            s1a(2)
            s1b(1)
            r1_load(Q * 4 + 1)
            s1a(3)
            s1b(2)
            s1b(3)
            for f_ in sched[8]:
                f_()
        P.barrier()
        A.reset(base_mark)
        if debug:
            dbgt = A.alloc([32, 4], F32)
            k.v("vector", "tensor_copy", (dbgt[:, :, 0:2], slots_i), [], ["dbgt0"])
            k.v("vector", "tensor_copy", (dbgt[:, :, 2:4], rw), [], ["dbgt1"])
            k.dma("sync", dr["dbg_rt"], dbgt, ["dbgt0", "dbgt1"], ["dbg_rt"])
            k.dma("sync", dr["dbg_h"], dr["h_d"], [], ["dbg_h"])
            for i_, nm_ in enumerate(("yfT_d", "ynaT_d", "ymemT_d")):
                k.dma("sync", dr["dbg_y3"][i_], dr[nm_], [], [("dbg_y3", i_)])

        weg = [A.alloc([8, 512], BF16) for _ in range(3)]
        weu = [A.alloc([8, 512], BF16) for _ in range(3)]
        wed = [A.alloc([4, 1024], BF16) for _ in range(3)]
        xg = [A.alloc([2, 1024], BF16) for _ in range(3)]
        xgT = [A.alloc([8, 256], BF16) for _ in range(2)]
        sg = [A.alloc([256], F32) for _ in range(2)]
        aT = [A.alloc([4, 256], BF16) for _ in range(2)]
        yo = [A.alloc([1024], BF16) for _ in range(2)]

        def e_load(e_):
            s = e_ % 3
            k.dma("gpsimd", weg[s], dr["w_eg"][e_].rearrange("(c p) n -> p c n", p=128), [], [("weg", s)])
            k.dma("gpsimd", weu[s], dr["w_eu"][e_].rearrange("(c p) n -> p c n", p=128), [], [("weu", s)])
            k.dma("gpsimd", wed[s], dr["w_ed"][e_].rearrange("(c p) n -> p c n", p=128), [], [("wed", s)])
            k.dma("sync", xg[e_ % 3], dr["xg_d"][e_ * CAP:(e_ + 1) * CAP, :].rearrange("(t p) f -> p t f", p=128),
                  ["xg_d"], [("xg", e_ % 3)])
        e_load(0); e_load(1)

        def e_transposes(ee):
            xgt_ = xgT[ee % 2]
            k.tri([[(psb[tt][:, fc * 128:(fc + 1) * 128], xg[ee % 3][:, tt, fc * 128:(fc + 1) * 128], identb,
                     [("xg", ee % 3), "identb"], [("ps", tt)]) for fc in range(8)] for tt in range(2)])
            for tt in range(2):
                k.evac(xgt_[:, :, tt * 128:(tt + 1) * 128], psb[tt][:].rearrange("p (a b) -> p a b", a=8),
                       [("ps", tt)], [("xgT", ee % 2)], partial=True)
        yc = 0
        for e_ in range(NEXP):
            if e_ + 2 < NEXP:
                e_load(e_ + 2)
            s = e_ % 3
            xgt = xgT[e_ % 2]
            if e_ == 0:
                e_transposes(0)
            at = aT[e_ % 2]
            for dcn in range(4):
                pg = 2 + (dcn % 2) * 2; pu = pg + 1
                k.mmi([[(ps[pg][:, 0:256], weg[s][:, fc, dcn * 128:(dcn + 1) * 128], xgt[:, fc, :],
                         [("weg", s), ("xgT", e_ % 2)], [("ps", pg)]) for fc in range(8)],
                       [(ps[pu][:, 0:256], weu[s][:, fc, dcn * 128:(dcn + 1) * 128], xgt[:, fc, :],
                         [("weu", s), ("xgT", e_ % 2)], [("ps", pu)]) for fc in range(8)]])
                k.act(sg[dcn % 2], ps[pg][:, 0:256], AF.Silu, [("ps", pg)], [("sg", dcn % 2)])
                k.v("vector", "tensor_tensor", (at[:, dcn, :], sg[dcn % 2], ps[pu][:, 0:256], ALU.mult),
                    [("sg", dcn % 2), ("ps", pu)], [("aT", e_ % 2, dcn)])
            atr = [("aT", e_ % 2, dcn) for dcn in range(4)]
            if e_ + 1 < NEXP:
                e_transposes(e_ + 1)
            for tt in range(2):
                yt = yo[yc % 2]
                k.mmi([[(ps[6 + half][:], at[:, dcn, tt * 128:(tt + 1) * 128], wed[s][:, dcn, half * 512:(half + 1) * 512],
                         atr + [("wed", s)], [("ps", 6 + half)]) for dcn in range(4)] for half in range(2)])
                for half in range(2):
                    pd = 6 + half
                    k.evac(yt[:, half * 512:(half + 1) * 512], ps[pd][:], [("ps", pd)], [("yo", yc % 2, half)])
                k.dma("sync", dr["yb_d"][e_ * CAP + tt * 128:e_ * CAP + (tt + 1) * 128, :], yt,
                      [("yo", yc % 2, 0), ("yo", yc % 2, 1)], ["yb_d"], key=("yo_st", yc % 2), partial=True)
                yc += 1
        P.barrier()
        A.reset(base_mark)

        lng = A.alloc([1024], F32); lnb = A.alloc([1024], F32); mhalf = A.alloc([1], F32)
        sm = A.alloc([256], F32)
        NB = 4
        hd = [A.alloc([1024], F32) for _ in range(NB)]
        g1 = [A.alloc([1024], BF16) for _ in range(NB)]
        g2 = [A.alloc([1024], BF16) for _ in range(NB)]
        acc = [A.alloc([1024], F32) for _ in range(NB)]
        ot = [A.alloc([1024], F32) for _ in range(NB)]
        smd = [A.alloc([32], F32) for _ in range(NB)]
        k.dma("sync", lng, dr["ln2_g"].partition_broadcast(128)[:, 0, :], [], ["lng"])
        k.dma("sync", lnb, dr["ln2_b"].partition_broadcast(128)[:, 0, :], [], ["lnb"])
        k.dma("sync", mhalf, dr["c_mhalf"], [], ["mhalf"])

        def d_load(ti):
            b2 = ti % NB
            k.dma("sync", hd[b2], dr["h_d"][ti * 128:(ti + 1) * 128, :], [], [("hd", b2)])
            for kk, gt in enumerate((g1, g2)):
                P.op("gpsimd", (lambda e, ti=ti, kk=kk, gt=gt, b2=b2: e.indirect_dma_start(
                    out=gt[b2][:, :], out_offset=None, in_=dr["yb_d"][:, :],
                    in_offset=bass.IndirectOffsetOnAxis(ap=slots_i[:, ti, kk:kk + 1], axis=0),
                    bounds_check=_bc_reg(e), oob_is_err=False)),
                    reads=[], writes=[("g", kk, b2)], dma=True)
        d_load(0); d_load(1)

        def da(ti):
            if ti + 2 < 32:
                d_load(ti + 2)
            b2 = ti % NB
            k.act(acc[b2], hd[b2], AF.Copy, [("hd", b2)], [("acc", b2)], scale=ALPHA)
            k.v("vector", "scalar_tensor_tensor", (acc[b2], g1[b2], rw[:, ti, 0:1], acc[b2], ALU.mult, ALU.add),
                [("g", 0, b2), ("acc", b2)], [("acc", b2)])
            k.v("vector", "scalar_tensor_tensor", (acc[b2], g2[b2], rw[:, ti, 1:2], acc[b2], ALU.mult, ALU.add),
                [("g", 1, b2), ("acc", b2)], [("acc", b2)])
            emit_ln_a(k, acc[b2], [("acc", b2)], smd[b2], mhalf, tag=b2)

        def db(ti):
            b2 = ti % NB
            emit_ln_b(k, acc[b2], [("acc", b2)], ot[b2], ("ot", b2), lng, lnb, ["lng", "lnb"], smd[b2], tag=b2)
            k.dma("sync", dr["out"][ti * 128:(ti + 1) * 128, :], ot[b2], [("ot", b2)], [("out", ti)])
        da(0)
        for ti in range(32):
            if ti + 1 < 32:
                da(ti + 1)
            db(ti)
        P.emit()
    return nc


def emit_ln_a(k, src, src_names, sm, mhalf, tag=0):
    st = sm[:, 0:12].rearrange("p (a b) -> p a b", a=2)
    mv = sm[:, 12:14]
    rs = sm[:, 14:15]
    nb = sm[:, 15:16]
    T = lambda x: (x, tag)
    for half in range(2):
        k.v("vector", "bn_stats", (st[:, half, :], src[:, half * 512:(half + 1) * 512]), src_names,
            [T("lnst%d" % half)])
    k.v("vector", "bn_aggr", (mv, st), [T("lnst0"), T("lnst1")], [T("lnmv")])
    k.v("gpsimd", "tensor_scalar", (rs, mv[:, 1:2], LN_EPS, 1.0, ALU.add, ALU.mult), [T("lnmv")], [T("lnrs0")])
    k.v("gpsimd", "tensor_tensor", (rs, rs, mhalf, ALU.pow), [T("lnrs0"), "mhalf"], [T("lnrs")])
    k.v("gpsimd", "tensor_scalar", (nb, mv[:, 0:1], rs, -1.0, ALU.mult, ALU.mult), [T("lnmv"), T("lnrs")],
        [T("lnnb")])


def emit_ln_b(k, src, src_names, dst, dst_name, g, b, gb_names, sm, tag=0):
    rs = sm[:, 14:15]
    nb = sm[:, 15:16]
    T = lambda x: (x, tag)
    k.act(dst, src, AF.Identity, src_names + [T("lnrs"), T("lnnb")], [dst_name + ("n",), dst_name], bias=nb, scale=rs)
    k.v("gpsimd", "tensor_tensor", (dst[:, 0:512], dst[:, 0:512], g[:, 0:512], ALU.mult),
        [dst_name + ("n",), gb_names[0]], [dst_name + ("g0",)])
    k.v("vector", "tensor_tensor", (dst[:, 512:1024], dst[:, 512:1024], g[:, 512:1024], ALU.mult),
        [dst_name + ("n",), gb_names[0]], [dst_name + ("g1",)])
    k.v("vector", "tensor_tensor", (dst, dst, b, ALU.add), [dst_name + ("g0",), dst_name + ("g1",), gb_names[1]],
        [dst_name])


def emit_route(k, ps, sm, ti, slots_i, rw, atot, ustrict, ones_b, ect, pl=2, stage=0):
    L = sm[:, 16:88]
    gm8 = sm[:, 88:96]
    ohg = sm[:, 96:104]
    dd = sm[:, 104:112]
    pg = sm[:, 112:113]
    el = sm[:, 113:121]
    tm8 = sm[:, 121:129]
    oh1 = sm[:, 129:137]
    oh2 = sm[:, 137:145]
    dv = sm[:, 145:146]
    t64 = sm[:, 146:210]
    sl = sm[:, 210:212]
    n = lambda s: ("rt_" + s,)
    V = lambda name, args, r, w: k.v("vector", name, args, r, w)
    if stage == 2:
        return _route_p2(k, ps, sm, ti, slots_i, atot, ustrict, ones_b, ect, pl)
    V("tensor_copy", (L, ps[pl][:, 0:72]), [("ps", pl)], [n("L")])
    V("max", (gm8, L[:, 0:8]), [n("L")], [n("gm8")])
    V("tensor_scalar", (ohg, L[:, 0:8], gm8[:, 0:1], None, ALU.is_equal), [n("L"), n("gm8")], [n("ohg")])
    V("tensor_scalar", (dd, L[:, 0:8], gm8[:, 0:1], None, ALU.subtract), [n("L"), n("gm8")], [n("dd")])
    k.act(dd, dd, AF.Sigmoid, [n("dd")], [n("dd")])
    V("tensor_scalar", (tm8, dd, -1.0, 1.0, ALU.mult, ALU.add), [n("dd")], [n("tm8")])
    V("reciprocal", (tm8, tm8), [n("tm8")], [n("tm8")])
    V("tensor_tensor", (dd, dd, tm8, ALU.mult), [n("dd"), n("tm8")], [n("dd")])
    V("tensor_reduce", (pg, dd, AX.X, ALU.add), [n("dd")], [n("pg")])
    V("reciprocal", (pg, pg), [n("pg")], [n("pg")])
    t3 = t64.rearrange("p (g e) -> p g e", g=8)
    L3 = L[:, 8:72].rearrange("p (g e) -> p g e", g=8)
    V("tensor_tensor", (t3, L3, ohg.unsqueeze(2).to_broadcast([128, 8, 8]), ALU.mult), [n("L"), n("ohg")], [n("t64")])
    V("tensor_reduce", (el, t64.rearrange("p (g e) -> p e g", g=8), AX.X, ALU.add), [n("t64")], [n("el")])
    V("max", (tm8, el), [n("el"), n("tm8")], [n("tm8")])
    V("tensor_scalar", (oh1, el, tm8[:, 0:1], None, ALU.is_equal), [n("el"), n("tm8")], [n("oh1")])
    V("tensor_scalar", (oh2, el, tm8[:, 1:2], None, ALU.is_equal), [n("el"), n("tm8")], [n("oh2")])
    V("tensor_tensor", (dv, tm8[:, 0:1], tm8[:, 1:2], ALU.subtract), [n("tm8")], [n("dv")])
    k.act(dv, dv, AF.Sigmoid, [n("dv")], [n("dv")])
    V("tensor_tensor", (rw[:, ti, 0:1], dv, pg, ALU.mult), [n("dv"), n("pg")], [("rw", ti, 0)])
    V("tensor_tensor", (rw[:, ti, 1:2], pg, rw[:, ti, 0:1], ALU.subtract), [n("pg"), ("rw", ti, 0)], [("rw", ti, 1)])
    A1 = k.A1
    A2 = k.A2
    Ab = k.Ab
    A13 = A1.rearrange("p (g e) -> p g e", g=8)
    A23 = A2.rearrange("p (g e) -> p g e", g=8)
    gb = ohg.unsqueeze(2).to_broadcast([128, 8, 8])
    V("tensor_tensor", (A13, gb, oh1.unsqueeze(1).to_broadcast([128, 8, 8]), ALU.mult), [n("ohg"), n("oh1")], [n("A1")])
    V("tensor_tensor", (A23, gb, oh2.unsqueeze(1).to_broadcast([128, 8, 8]), ALU.mult), [n("ohg"), n("oh2")], [n("A2")])
    V("tensor_tensor", (Ab, A1, A2, ALU.add), [n("A1"), n("A2")], [n("Ab")])
    if stage == 1:
        return
    _route_p2(k, ps, sm, ti, slots_i, atot, ustrict, ones_b, ect, pl)


def _route_p2(k, ps, sm, ti, slots_i, atot, ustrict, ones_b, ect, pl):
    t64 = sm[:, 146:210]
    sl = sm[:, 210:212]
    n = lambda s: ("rt_" + s,)
    V = lambda name, args, r, w: k.v("vector", name, args, r, w)
    A1 = k.A1
    A2 = k.A2
    Ab = k.Ab
    k.mm(ps[pl][:, 128:192], ustrict, Ab, True, False, ["ustrict", n("Ab")], [("ps", pl)])
    k.mm(ps[pl][:, 128:192], ones_b, atot, False, True, ["ones_b", "atot"], [("ps", pl)])
    V("tensor_tensor", (t64, ps[pl][:, 128:192], ect, ALU.add), [("ps", pl), "ect"], [n("t64")])
    V("tensor_tensor", (atot, atot, Ab, ALU.add), [n("Ab"), "atot"], ["atot"])
    V("tensor_tensor", (A1, A1, t64, ALU.mult), [n("A1"), n("t64")], [n("A1")])
    V("tensor_tensor", (A2, A2, t64, ALU.mult), [n("A2"), n("t64")], [n("A2")])
    V("tensor_reduce", (sl[:, 0:1], A1, AX.X, ALU.add), [n("A1")], [n("sl0")])
    V("tensor_reduce", (sl[:, 1:2], A2, AX.X, ALU.add), [n("A2")], [n("sl1")])
    V("tensor_copy", (slots_i[:, ti, :], sl), [n("sl0"), n("sl1")], [("slots", ti)])


_NC_CACHE = {}


def _consts(hf):
    bf = ml_dtypes.bfloat16
    c = {}
    c["c_identf"] = np.eye(128, dtype=np.float32)
    c["c_identb"] = np.eye(128, dtype=np.float32).astype(bf)
    c["c_ones"] = np.ones((128, 128), np.float32).astype(bf)
    c["c_ustrict"] = np.triu(np.ones((128, 128), np.float32), 1).astype(bf)
    row = np.arange(128)
    k2 = np.arange(128)
    ed = np.zeros((64, 128, 256), np.float32)
    for j in range(64):
        s = j + 64 * row
        th = 2.0 * np.pi * ((s[:, None] * k2[None, :]) % 8192) / 8192.0
        ed[j, :, 0:128] = np.cos(th) / 32.0
        ed[j, :, 128:256] = np.sin(th) / 32.0
    c["c_edft"] = ed.astype(bf)
    ph = 2.0 * np.pi * ((row[:, None] * row[None, :]) % 128) / 128.0
    C = np.cos(ph); S = np.sin(ph)
    c["c_fca"] = np.concatenate([C, -S], axis=1).astype(np.float32).astype(bf)
    c["c_fcb"] = np.concatenate([-S, -C], axis=1).astype(np.float32).astype(bf)
    s1 = np.arange(64)
    k1 = 32 * hf + np.arange(32)
    psi = 2.0 * np.pi * ((s1[:, None] * k1[None, :]) % 64) / 64.0
    bdc = np.zeros((128, 64), np.float32); bds = np.zeros((128, 64), np.float32)
    for a in range(2):
        bdc[a::2, a * 32:(a + 1) * 32] = np.cos(psi) / 32.0
        bds[a::2, a * 32:(a + 1) * 32] = np.sin(psi) / 32.0
    c["c_bdc"] = bdc.astype(bf); c["c_bds"] = bds.astype(bf)
    c["c_ec"] = np.tile((np.arange(64, dtype=np.float32) * CAP)[None, :], (128, 1)).astype(np.float32)
    c["c_mhalf"] = np.full((128, 1), -0.5, np.float32)
    c["c_onesrow"] = np.ones((1, 128), np.float32)
    return c


def _bias_tiles(rpb, hf):
    out = np.full((5, 8, 128, 768), MASKV, np.float32)
    specs = [(10, [-2, -1, 0, 1, 2]), (0, [-2, -1, 0, 1, 2, 3]), (1, [-2, -1, 0, 1, 2]),
             (30, [-2, -1, 0, 1, 2]), (31, [-3, -2, -1, 0, 1, 2])]
    qc = np.arange(64); kc = np.arange(64)
    cs = np.clip(qc - 8, 0, 48)
    colvalid = (kc[:, None] >= cs[None, :]) & (kc[:, None] < cs[None, :] + 16)
    dc = kc[:, None] - qc[None, :] + 15
    dcc = np.clip(dc, 0, 30)
    for ty, (qp, offs) in enumerate(specs):
        for j, off in enumerate(offs):
            kp = qp + off
            for aq in range(2):
                r = 64 * hf + 2 * qp + aq
                rs = min(max(r - 4, 0), 120)
                for ak in range(2):
                    kr = 64 * hf + 2 * kp + ak
                    if kr < 0 or kr > 127 or kr < rs or kr > rs + 7:
                        continue
                    drr = kr - r + 7
                    vals = rpb[:, drr, :][:, dcc]
                    blk = np.where(colvalid[None], vals, np.float32(MASKV))
                    out[ty, :, ak * 64:(ak + 1) * 64, j * 128 + aq * 64:j * 128 + (aq + 1) * 64] = blk
    return out


def kernel(x, mem, w_in, w_gate, b_gate, w_mem_kv, rpb, w_fourier_o, w_na_o, w_mem_o, w_out, ln1_g, ln1_b,
           w_router_group, b_router_group, w_router_expert, b_router_expert, w_exp_gate, w_exp_up, w_exp_down,
           ln2_g, ln2_b, _debug=False):
    f = lambda a: np.ascontiguousarray(np.asarray(a, dtype=np.float32))
    x = f(x); mem = f(mem)
    key = bool(_debug)
    if key not in _NC_CACHE:
        _NC_CACHE[key] = build_program(debug=_debug)
    nc = _NC_CACHE[key]
    shared = {
        "w_in": f(w_in[0]), "w_gate": f(w_gate[0]),
        "bg": f(np.asarray(b_gate[0]).reshape(24, 128).T),
        "w_kv": f(w_mem_kv[0]), "w_fo": f(w_fourier_o[0]), "w_nao": f(w_na_o[0]), "w_memo": f(w_mem_o[0]),
        "w_out": f(w_out[0]), "ln1_g": f(ln1_g), "ln1_b": f(ln1_b), "ln2_g": f(ln2_g), "ln2_b": f(ln2_b),
        "wr": f(np.concatenate([np.asarray(w_router_group[0]), np.asarray(w_router_expert[0])], axis=1)),
        "br": f(np.concatenate([np.asarray(b_router_group[0]), np.asarray(b_router_expert[0])])[None, :]),
        "w_eg": f(w_exp_gate[0]), "w_eu": f(w_exp_up[0]), "w_ed": f(w_exp_down[0]),
    }
    rp = f(rpb[0])
    in_maps = []
    for c in range(NCORES):
        b, hf = c // 2, c % 2
        m = dict(shared)
        m.update(_consts(hf))
        m["xf"] = x[b]
        xo = np.zeros((4608, D), np.float32)
        xo[0:4096] = x[b, 4096 * hf:4096 * hf + 4096]
        if hf == 1:
            xo[4096:4352] = x[b, 4096 - 256:4096]
        else:
            xo[4352:4608] = x[b, 4096:4096 + 256]
        m["xo"] = xo
        m["mem"] = mem[b]
        m["biasT"] = _bias_tiles(rp, hf)
        in_maps.append(m)
    res = run_bass_kernel_spmd(nc, in_maps, core_ids=list(range(NCORES)))
    out = np.zeros((4, 8192, D), np.float32)
    for c in range(NCORES):
        b, hf = c // 2, c % 2
        out[b, 4096 * hf:4096 * hf + 4096] = res.results[c]["out"]
    if _debug:
        return out, res
    return out
```
